# Optimizing a Trainium2 kernel written in Bass

```python
import jax, jax.numpy as jnp
from jax import lax
import numpy as np

D_MODEL = 1024
BATCH = 2
SEQ = 8192
DEPTH = 2

GRID_W = 64
CTX_LEN = 256
MIX = D_MODEL
D_A = MIX // 2
D_B = MIX // 2
D_C = MIX // 2
D_D = MIX // 2
CONV_A = 31
CONV_B = 4
CONV_C = 3
PAD_A = (CONV_A // 2, CONV_A // 2)
PAD_B = (2, 1)
PAD_C = (1, 1)
LRU_BLOCK = 128
N_LRU_BLOCKS = D_B // LRU_BLOCK
LRU_C = 8.0
HEAD_DIM = 64
N_HEADS_D = D_D // HEAD_DIM
WIN_H = 8
WIN_W = 16
N_EXPERTS = 16
N_GROUPS = 4
EXPERTS_PER_GROUP = N_EXPERTS // N_GROUPS
TOP_K = 2
D_FF = 512
EPS = 1e-6
N_EVEN = (DEPTH + 1) // 2
N_ODD = DEPTH // 2

kernel_name = 'hybrid_conv_lru_shortconv_natten_moe_dit'


def rms_norm(x, g):
    xf = x.astype(jnp.float32)
    y = xf * lax.rsqrt(jnp.mean(xf * xf, -1, keepdims=True) + EPS)
    return (y * g.astype(jnp.float32)).astype(x.dtype)


def layer_norm(x, g, b):
    xf = x.astype(jnp.float32)
    mu = jnp.mean(xf, -1, keepdims=True)
    var = jnp.mean(jnp.square(xf - mu), -1, keepdims=True)
    y = (xf - mu) * lax.rsqrt(var + EPS) * g.astype(jnp.float32) + b.astype(jnp.float32)
    return y.astype(x.dtype)


def depthwise_conv(x, w, pad):
    return lax.conv_general_dilated(x, w[:, None, :].astype(x.dtype), window_strides=(1,), padding=[pad],
                                    dimension_numbers=('NWC', 'WIO', 'NWC'), feature_group_count=x.shape[-1])


def conformer_conv(p, dw_w, dw_b, ln_g, ln_b):
    v, g = jnp.split(p, 2, -1)
    u = v * jax.nn.sigmoid(g)
    u = depthwise_conv(u, dw_w, PAD_A) + dw_b
    return jax.nn.silu(layer_norm(u, ln_g, ln_b))


def rglru_coeffs(v, w_gate, b_gate, lam):
    bsz, length, _ = v.shape
    vb = v.reshape(bsz, length, N_LRU_BLOCKS, LRU_BLOCK)
    gates = jnp.einsum('blnk,gnkj->gblnj', vb, w_gate).reshape(2, bsz, length, D_B) + b_gate[:, None, None, :]
    r = jax.nn.sigmoid(gates[0].astype(jnp.float32))
    i = jax.nn.sigmoid(gates[1].astype(jnp.float32))
    log_a = -LRU_C * r * jax.nn.softplus(-lam.astype(jnp.float32))
    a = jnp.exp(log_a)
    b = jnp.sqrt(-jnp.expm1(2.0 * log_a)) * (i * v.astype(jnp.float32))
    return a, b


def linear_scan(a, b, h0, reverse):
    def combine(left, right):
        a_l, b_l = left
        a_r, b_r = right
        return a_l * a_r, a_r * b_l + b_r
    a_cum, b_cum = lax.associative_scan(combine, (a, b), reverse=reverse, axis=1)
    return a_cum * h0[:, None, :] + b_cum


def rglru_bidir(u_lat, u_ctx, conv_w, conv_b, gate_w, gate_b, lam, with_ctx):
    v_lat = depthwise_conv(u_lat, conv_w, PAD_B) + conv_b
    v_ctx = depthwise_conv(u_ctx, conv_w, PAD_B) + conv_b
    y_lat = jnp.zeros(v_lat.shape, jnp.float32)
    y_ctx = jnp.zeros(v_ctx.shape, jnp.float32)
    for d, rev in enumerate((False, True)):
        a_c, b_c = rglru_coeffs(v_ctx, gate_w[d], gate_b[d], lam[d])
        h_c = linear_scan(a_c, b_c, jnp.zeros_like(b_c[:, 0]), rev)
        h_end = h_c[:, 0] if rev else h_c[:, -1]
        a_l, b_l = rglru_coeffs(v_lat, gate_w[d], gate_b[d], lam[d])
        y_lat = y_lat + linear_scan(a_l, b_l, h_end, rev)
        if with_ctx:
            y_ctx = y_ctx + h_c
    return y_lat.astype(u_lat.dtype), (y_ctx.astype(u_ctx.dtype) if with_ctx else None)


def short_gated_conv(p, conv_w):
    xin, bg, cg = jnp.split(p, 3, -1)
    return bg * depthwise_conv(cg * xin, conv_w, PAD_C)


def context_attention(q, k, v):
    s = jnp.einsum('bqhd,bkhd->bhqk', q, k).astype(jnp.float32) * (HEAD_DIM ** -0.5)
    p = jax.nn.softmax(s, -1).astype(v.dtype)
    o = jnp.einsum('bhqk,bkhd->bqhd', p, v)
    return o.reshape(o.shape[0], o.shape[1], D_D)


def neighbourhood_attention(q, k, v, k_ctx, v_ctx, rpb):
    bsz, seq = q.shape[0], q.shape[1]
    rows = seq // GRID_W
    kh = min(WIN_H, rows)
    kw = WIN_W
    scale = HEAD_DIM ** -0.5
    qg = q.reshape(bsz, rows, GRID_W, N_HEADS_D, HEAD_DIM)
    kg = k.reshape(bsz, rows, GRID_W, N_HEADS_D, HEAD_DIM)
    vg = v.reshape(bsz, rows, GRID_W, N_HEADS_D, HEAD_DIM)
    col = jnp.arange(GRID_W)
    col_start = jnp.clip(col - kw // 2, 0, GRID_W - kw)
    key_cols = col_start[:, None] + jnp.arange(kw)
    col_idx = key_cols - col[:, None] + (WIN_W - 1)
    rpb_f = rpb.astype(jnp.float32)

    def row_block(r):
        r_start = jnp.clip(r - kh // 2, 0, rows - kh)
        q_r = lax.dynamic_index_in_dim(qg, r, axis=1, keepdims=False)
        k_rows = lax.dynamic_slice_in_dim(kg, r_start, kh, axis=1)
        v_rows = lax.dynamic_slice_in_dim(vg, r_start, kh, axis=1)
        k_win = k_rows[:, :, key_cols]
        v_win = v_rows[:, :, key_cols]
        row_idx = r_start + jnp.arange(kh) - r + (WIN_H - 1)
        bias = rpb_f[:, row_idx[None, :, None], col_idx[:, None, :]]
        s_loc = jnp.einsum('bqhd,brqkhd->bhqrk', q_r, k_win).astype(jnp.float32) * scale + bias[None]
        s_ctx = jnp.einsum('bqhd,bchd->bhqc', q_r, k_ctx).astype(jnp.float32) * scale
        s = jnp.concatenate([s_loc.reshape(bsz, N_HEADS_D, GRID_W, kh * kw), s_ctx], -1)
        p = jax.nn.softmax(s, -1).astype(v.dtype)
        p_loc = p[..., :kh * kw].reshape(bsz, N_HEADS_D, GRID_W, kh, kw)
        p_ctx = p[..., kh * kw:]
        return (jnp.einsum('bhqrk,brqkhd->bqhd', p_loc, v_win)
                + jnp.einsum('bhqc,bchd->bqhd', p_ctx, v_ctx))

    out = lax.map(row_block, jnp.arange(rows))
    return out.transpose(1, 0, 2, 3, 4).reshape(bsz, seq, D_D)


def mixer_conv_lru(n_lat, n_ctx, w_in, dw_w, dw_b, ln_g, ln_b, conv_w, conv_b, gate_w, gate_b, lam, with_ctx):
    splits = [2 * D_A, 2 * D_A + D_B]
    a_l, u_l, g_l = jnp.split(n_lat @ w_in, splits, -1)
    a_c, u_c, g_c = jnp.split(n_ctx @ w_in, splits, -1)
    y_l, y_c = rglru_bidir(u_l, u_c, conv_w, conv_b, gate_w, gate_b, lam, with_ctx)
    out_lat = jnp.concatenate([conformer_conv(a_l, dw_w, dw_b, ln_g, ln_b), y_l * jax.nn.gelu(g_l)], -1)
    if not with_ctx:
        return out_lat, None
    out_ctx = jnp.concatenate([conformer_conv(a_c, dw_w, dw_b, ln_g, ln_b), y_c * jax.nn.gelu(g_c)], -1)
    return out_lat, out_ctx


def mixer_conv_natten(n_lat, n_ctx, w_in, conv_w, rpb, with_ctx):
    def heads(t):
        return t.reshape(t.shape[0], t.shape[1], N_HEADS_D, HEAD_DIM)
    p_l = n_lat @ w_in
    p_c = n_ctx @ w_in
    q_l, k_l, v_l = [heads(t) for t in jnp.split(p_l[..., 3 * D_C:], 3, -1)]
    q_c, k_c, v_c = [heads(t) for t in jnp.split(p_c[..., 3 * D_C:], 3, -1)]
    out_lat = jnp.concatenate([short_gated_conv(p_l[..., :3 * D_C], conv_w),
                               neighbourhood_attention(q_l, k_l, v_l, k_c, v_c, rpb)], -1)
    if not with_ctx:
        return out_lat, None
    out_ctx = jnp.concatenate([short_gated_conv(p_c[..., :3 * D_C], conv_w), context_attention(q_c, k_c, v_c)], -1)
    return out_lat, out_ctx


def moe(h, router_w, router_bias, w_gate, w_up, w_down):
    shp = h.shape
    t = h.reshape(-1, D_MODEL)
    score = jax.nn.sigmoid((t @ router_w).astype(jnp.float32))
    sel = (score + router_bias.astype(jnp.float32)).reshape(-1, N_GROUPS, EXPERTS_PER_GROUP)
    group_score = jnp.sum(lax.top_k(sel, TOP_K)[0], -1)
    in_group = jax.nn.one_hot(jnp.argmax(group_score, -1), N_GROUPS, dtype=jnp.bool_)
    masked = jnp.where(in_group[:, :, None], sel, -jnp.inf).reshape(-1, N_EXPERTS)
    _, idx = lax.top_k(masked, TOP_K)
    w = jnp.take_along_axis(score, idx, -1)
    w = w / jnp.sum(w, -1, keepdims=True)
    combine = jnp.sum(jax.nn.one_hot(idx, N_EXPERTS, dtype=jnp.float32) * w[..., None], 1).astype(t.dtype)
    out = jnp.zeros_like(t)
    for e in range(N_EXPERTS):
        hid = jax.nn.silu(t @ w_gate[e]) * (t @ w_up[e])
        out = out + combine[:, e:e + 1] * (hid @ w_down[e])
    return out.reshape(shp)


def setup_inputs(seed: int = 0) -> dict:
    key = jax.random.key(seed)
    ks = jax.random.split(key, 32)
    D = D_MODEL

    def nrm(k, shape, s):
        return jax.random.normal(k, shape, jnp.float32) * s

    a_base = jax.random.uniform(ks[17], (N_EVEN, 2, D_B), jnp.float32, 0.9, 0.999) ** (1.0 / LRU_C)
    b_lambda = jnp.log(a_base) - jnp.log1p(-a_base)
    return {
        'x': nrm(ks[0], (BATCH, SEQ, D), 1.0),
        'c': nrm(ks[1], (BATCH, D), 1.0),
        'ctx': nrm(ks[2], (BATCH, CTX_LEN, D), 1.0),
        'c_ctx': nrm(ks[3], (D,), 1.0),
        'ada_w': nrm(ks[4], (DEPTH, D, 6 * D), 0.5 * D ** -0.5),
        'ada_b': nrm(ks[5], (DEPTH, 6 * D), 0.01),
        'norm_mix_g': 1.0 + nrm(ks[6], (DEPTH, D), 0.05),
        'norm_ffn_g': 1.0 + nrm(ks[7], (DEPTH, D), 0.05),
        'w_out': nrm(ks[8], (DEPTH, MIX, D), MIX ** -0.5),
        'ab_w_in': nrm(ks[9], (N_EVEN, D, 2 * D_A + 2 * D_B), D ** -0.5),
        'a_dw_w': nrm(ks[10], (N_EVEN, CONV_A, D_A), CONV_A ** -0.5),
        'a_dw_b': nrm(ks[11], (N_EVEN, D_A), 0.01),
        'a_ln_g': 1.0 + nrm(ks[12], (N_EVEN, D_A), 0.05),
        'a_ln_b': nrm(ks[13], (N_EVEN, D_A), 0.01),
        'b_conv_w': nrm(ks[14], (N_EVEN, CONV_B, D_B), CONV_B ** -0.5),
        'b_conv_b': nrm(ks[15], (N_EVEN, D_B), 0.01),
        'b_gate_w': nrm(ks[16], (N_EVEN, 2, 2, N_LRU_BLOCKS, LRU_BLOCK, LRU_BLOCK), LRU_BLOCK ** -0.5),
        'b_gate_b': nrm(ks[18], (N_EVEN, 2, 2, D_B), 0.01),
        'b_lambda': b_lambda,
        'cd_w_in': nrm(ks[19], (N_ODD, D, 3 * D_C + 3 * D_D), D ** -0.5),
        'c_conv_w': nrm(ks[20], (N_ODD, CONV_C, D_C), CONV_C ** -0.5),
        'd_rpb': nrm(ks[21], (N_ODD, N_HEADS_D, 2 * WIN_H - 1, 2 * WIN_W - 1), 0.1),
        'router_w': nrm(ks[22], (D, N_EXPERTS), D ** -0.5),
        'router_bias': nrm(ks[23], (N_EXPERTS,), 0.01),
        'moe_w_gate': nrm(ks[24], (DEPTH, N_EXPERTS, D, D_FF), D ** -0.5),
        'moe_w_up': nrm(ks[25], (DEPTH, N_EXPERTS, D, D_FF), D ** -0.5),
        'moe_w_down': nrm(ks[26], (DEPTH, N_EXPERTS, D_FF, D), D_FF ** -0.5),
        'final_g': 1.0 + nrm(ks[27], (D,), 0.05),
    }


def reference(x, c, ctx, c_ctx, ada_w, ada_b, norm_mix_g, norm_ffn_g, w_out,
              ab_w_in, a_dw_w, a_dw_b, a_ln_g, a_ln_b, b_conv_w, b_conv_b, b_gate_w, b_gate_b, b_lambda,
              cd_w_in, c_conv_w, d_rpb, router_w, router_bias, moe_w_gate, moe_w_up, moe_w_down, final_g):
    h_lat, h_ctx = x, ctx
    cond_lat = jax.nn.silu(c)
    cond_ctx = jax.nn.silu(c_ctx)[None]
    for layer in range(DEPTH):
        with_ctx = layer < DEPTH - 1
        j = layer // 2
        mod_l = (cond_lat @ ada_w[layer] + ada_b[layer])[:, None, :]
        mod_c = (cond_ctx @ ada_w[layer] + ada_b[layer])[:, None, :]
        sh1_l, sc1_l, g1_l, sh2_l, sc2_l, g2_l = jnp.split(mod_l, 6, -1)
        sh1_c, sc1_c, g1_c, sh2_c, sc2_c, g2_c = jnp.split(mod_c, 6, -1)
        n_lat = rms_norm(h_lat, norm_mix_g[layer]) * (1.0 + sc1_l) + sh1_l
        n_ctx = rms_norm(h_ctx, norm_mix_g[layer]) * (1.0 + sc1_c) + sh1_c
        if layer % 2 == 0:
            m_lat, m_ctx = mixer_conv_lru(n_lat, n_ctx, ab_w_in[j], a_dw_w[j], a_dw_b[j], a_ln_g[j], a_ln_b[j],
                                          b_conv_w[j], b_conv_b[j], b_gate_w[j], b_gate_b[j], b_lambda[j], with_ctx)
        else:
            m_lat, m_ctx = mixer_conv_natten(n_lat, n_ctx, cd_w_in[j], c_conv_w[j], d_rpb[j], with_ctx)
        h_lat = h_lat + g1_l * (m_lat @ w_out[layer])
        f_lat = moe(rms_norm(h_lat, norm_ffn_g[layer]) * (1.0 + sc2_l) + sh2_l,
                    router_w, router_bias, moe_w_gate[layer], moe_w_up[layer], moe_w_down[layer])
        h_lat = h_lat + g2_l * f_lat
        if with_ctx:
            h_ctx = h_ctx + g1_c * (m_ctx @ w_out[layer])
            f_ctx = moe(rms_norm(h_ctx, norm_ffn_g[layer]) * (1.0 + sc2_c) + sh2_c,
                        router_w, router_bias, moe_w_gate[layer], moe_w_up[layer], moe_w_down[layer])
            h_ctx = h_ctx + g2_c * f_ctx
    return rms_norm(h_lat, final_g)
```

```python
import contextlib
import numpy as np
import concourse.bass as bass
import concourse.mybir as mybir
from concourse.bass_utils import run_bass_kernel_spmd

F32 = mybir.dt.float32
BF16 = mybir.dt.bfloat16
AF = mybir.ActivationFunctionType
ALU = mybir.AluOpType
AX = mybir.AxisListType

NCORES = 8
D = 1024
KD = 8
TOK = 2048
CTX = 256
HAL0 = 16
NL0 = TOK + 2 * HAL0
NT0 = NL0 + CTX
OWN0 = HAL0
CTX0 = NL0
UW = NL0 + 15 + CTX + 15
UCTX = NL0 + 15
EPS = 1e-6
BIG = 30000.0
DEBUG_SERIAL = False


class Res:
    __slots__ = ("name", "w", "rc", "rd")

    def __init__(self, name, init=()):
        self.name = name
        self.w = None
        self.rc = {}
        self.rd = []
        for o in init:
            self.addr(o)

    def addr(self, o):
        if o.kind == "d":
            self.rd.append(o)
        else:
            p = self.rc.get(o.eng)
            if p is None or p.idx < o.idx:
                self.rc[o.eng] = o

    def readers(self):
        return list(self.rc.values()) + self.rd


class Buf:
    def __init__(self, name, ap, lo=0, hi=0, init=()):
        self.name = name
        self.ap = ap
        self.lo = lo
        self.hi = hi
        self.init = list(init)
        self.subs = {}

    def r(self, key=0):
        s = self.subs.get(key)
        if s is None:
            s = Res(f"{self.name}.{key}", self.init)
            self.subs[key] = s
        return s

    def last_ops(self):
        d = {}
        for s in self.subs.values():
            if s.w is not None:
                d[s.w.idx] = s.w
            for x in s.readers():
                d[x.idx] = x
        for x in self.init:
            d[x.idx] = x
        best = {}
        out = []
        for x in d.values():
            if x.kind == "d":
                out.append(x)
            else:
                p = best.get(x.eng)
                if p is None or p.idx < x.idx:
                    best[x.eng] = x
        return out + list(best.values())


class Op:
    __slots__ = ("eng", "fn", "deps", "kind", "need", "sigsem", "sigval", "idx", "name")


ENGS = ["pe", "dve", "act", "pool", "sp"]


class Prog:
    def __init__(self, nc):
        self.nc = nc
        self.q = {e: [] for e in ENGS}
        self.n = 0

    def add(self, eng, fn, reads=(), writes=(), kind="c", name=""):
        o = Op()
        o.eng = eng
        o.fn = fn
        o.kind = kind
        o.need = False
        o.sigsem = None
        o.sigval = 0
        o.idx = self.n
        o.name = name
        self.n += 1
        deps = {}
        for r in reads:
            if r.w is not None:
                deps[r.w.idx] = r.w
        for w in writes:
            if w.w is not None:
                deps[w.w.idx] = w.w
            for x in w.readers():
                deps[x.idx] = x
        best = {}
        o.deps = []
        for d in deps.values():
            if d is o:
                continue
            if d.kind == "d":
                d.need = True
                o.deps.append(d)
                continue
            if d.eng == "pe" and eng == "pe" and kind == "c":
                continue
            p = best.get(d.eng)
            if p is None or p.idx < d.idx:
                best[d.eng] = d
        for d in best.values():
            d.need = True
            o.deps.append(d)
        for r in reads:
            r.addr(o)
        for w in writes:
            w.w = o
            w.rc = {}
            w.rd = []
        self.q[eng].append(o)
        return o

    def pe(self, fn, reads=(), writes=(), **k):
        return self.add("pe", fn, reads, writes, **k)

    def dve(self, fn, reads=(), writes=(), **k):
        return self.add("dve", fn, reads, writes, **k)

    def act(self, fn, reads=(), writes=(), **k):
        return self.add("act", fn, reads, writes, **k)

    def pool(self, fn, reads=(), writes=(), **k):
        return self.add("pool", fn, reads, writes, **k)

    def dma(self, q, fn, reads=(), writes=(), **k):
        return self.add(q, fn, reads, writes, kind="d", **k)

    def emit(self, final_waits):
        nc = self.nc
        NDS = 8
        with contextlib.ExitStack() as es:
            csem = {e: es.enter_context(nc.semaphore(f"c_{e}")) for e in ENGS}
            dsem = {e: [es.enter_context(nc.semaphore(f"d_{e}{i}")) for i in range(NDS)] for e in ENGS}
            for e in ENGS:
                cc = 0
                dcnt = [0] * NDS
                di = 0
                for o in self.q[e]:
                    if o.kind == "d":
                        s = di % NDS
                        di += 1
                        dcnt[s] += 16
                        o.sigsem = ("d", e, s)
                        o.sigval = dcnt[s]
                    else:
                        if o.need or o.kind == "cc":
                            cc += 1
                            o.sigsem = ("c", e, 0)
                            o.sigval = cc
            block = es.enter_context(nc.Block())

            def semof(key):
                return csem[key[1]] if key[0] == "c" else dsem[key[1]][key[2]]

            def run(ename, eng):
                waited = {}
                for o in self.q[ename]:
                    dl = list(o.deps)
                    if o.kind == "d":
                        prev = o.sigval - 16
                        if prev > 0:
                            key = o.sigsem
                            if waited.get(key, 0) < prev:
                                eng.wait_ge(semof(key), prev)
                                waited[key] = prev
                    for d in dl:
                        key = d.sigsem
                        if waited.get(key, 0) < d.sigval:
                            eng.wait_ge(semof(key), d.sigval)
                            waited[key] = d.sigval
                    ins = o.fn(eng)
                    if o.sigsem is not None:
                        ins.then_inc(semof(o.sigsem), 16 if o.kind == "d" else 1)
                for o in final_waits.get(ename, []):
                    eng.wait_ge(semof(o.sigsem), o.sigval)
                last = {}
                for o in self.q[ename]:
                    if o.kind == "d":
                        last[o.sigsem] = o.sigval
                for key, val in last.items():
                    if waited.get(key, 0) < val:
                        eng.wait_ge(semof(key), val)

            @block.tensor
            def _(eng):
                run("pe", eng)

            @block.vector
            def _(eng):
                run("dve", eng)

            @block.scalar
            def _(eng):
                run("act", eng)

            @block.gpsimd
            def _(eng):
                run("pool", eng)

            @block.sync
            def _(eng):
                run("sp", eng)


class Arena:
    def __init__(self, nc, nbytes, name="arena"):
        self.nbytes = nbytes
        self.t = nc.alloc_sbuf_tensor(name, [128, nbytes // 4], F32)
        self.live = []
        self.dead = []

    def alloc(self, name, shape, dtype, at=None):
        esz = 4 if dtype == F32 else 2
        n = 1
        for s in shape[1:]:
            n *= s
        nb = (n * esz + 63) // 64 * 64
        if at is None:
            pts = sorted([(lo, hi) for lo, hi, _ in self.live])
            cur = 0
            at = None
            for lo, hi in pts:
                if lo - cur >= nb:
                    at = cur
                    break
                cur = max(cur, hi)
            if at is None:
                if self.nbytes - cur >= nb:
                    at = cur
                else:
                    raise RuntimeError(f"arena full allocating {name} {nb}B; live={[(b.name, lo, hi) for lo, hi, b in self.live]}")
        lo, hi = at, at + nb
        assert hi <= self.nbytes, (name, lo, hi)
        for l2, h2, b2 in self.live:
            assert not (l2 < hi and lo < h2), f"arena overlap {name} [{lo},{hi}) with {b2.name} [{l2},{h2})"
        init = []
        keep = []
        for dl, dh, db in self.dead:
            if dl < hi and lo < dh:
                init.extend(db.last_ops())
                keep.append((dl, dh, db))
            else:
                keep.append((dl, dh, db))
        self.dead = keep
        ap = self.t[:, lo // 4: hi // 4]
        if dtype != F32:
            ap = ap.bitcast(dtype)
        ap = ap[:, 0:n]
        if len(shape) == 3:
            ap = ap.rearrange("p (a b) -> p a b", a=shape[1])
        elif len(shape) == 4:
            ap = ap.rearrange("p (a b c) -> p a b c", a=shape[1], b=shape[2])
        if shape[0] != 128:
            ap = ap[0:shape[0]]
        b = Buf(name, ap, lo, hi, init)
        self.live.append((lo, hi, b))
        return b

    def free(self, *bufs):
        for b in bufs:
            for i, (lo, hi, x) in enumerate(self.live):
                if x is b:
                    self.live.pop(i)
                    self.dead.append((lo, hi, b))
                    break
            else:
                raise RuntimeError(f"free of non-live {b.name}")
        if len(self.dead) > 400:
            self.dead = self.dead[-400:]


def I_mm(out, lhsT, rhs, start=True, stop=True, skip=False):
    if skip:
        return lambda e: e.matmul(out, lhsT, rhs, start=start, stop=stop, skip_group_check=True)
    return lambda e: e.matmul(out, lhsT, rhs, start=start, stop=stop)


def I_tr(out, in_, ident):
    return lambda e: e.transpose(out, in_, ident)


def I_act(out, in_, func, bias=None, scale=None, accum_out=None):
    def f(e):
        kw = {}
        if bias is not None:
            kw["bias"] = bias
        if scale is not None:
            kw["scale"] = scale
        if accum_out is not None:
            kw["accum_out"] = accum_out
        return e.activation(out=out, in_=in_, func=func, **kw)
    return f


def I_tt(out, in0, in1, op):
    return lambda e: e.tensor_tensor(out=out, in0=in0, in1=in1, op=op)


def I_ts(out, in0, s1, s2, op0, op1=None):
    if op1 is None:
        return lambda e: e.tensor_scalar(out=out, in0=in0, scalar1=s1, scalar2=None, op0=op0)
    return lambda e: e.tensor_scalar(out=out, in0=in0, scalar1=s1, scalar2=s2, op0=op0, op1=op1)


def I_stt(out, in0, scalar, in1, op0, op1):
    return lambda e: e.scalar_tensor_tensor(out=out, in0=in0, scalar=scalar, in1=in1, op0=op0, op1=op1)


def I_copy(out, in_):
    return lambda e: e.tensor_copy(out=out, in_=in_)


def I_dma(out, in_):
    return lambda e: e.dma_start(out=out, in_=in_)


def I_scan(out, d0, d1, init, op0=ALU.mult, op1=ALU.add):
    return lambda e: e.tensor_tensor_scan(out=out, data0=d0, data1=d1, initial=init, op0=op0, op1=op1)


def I_recip(out, in_):
    return lambda e: e.reciprocal(out=out, in_=in_)


def I_red(out, in_, op, axis=AX.X):
    return lambda e: e.tensor_reduce(out=out, in_=in_, axis=axis, op=op)


def I_memset(ap, v):
    return lambda e: e.memset(ap, v)


class PSum:
    def __init__(self, nc):
        self.t = nc.alloc_psum_tensor("ps", [128, 8, 512], F32)
        self.b = [Buf(f"ps{i}", self.t[:, i, :]) for i in range(8)]
        self.rr = {}

    def next(self, group, banks):
        i = self.rr.get(group, 0)
        self.rr[group] = i + 1
        return self.b[banks[i % len(banks)]]


VEC_LAYOUT = [
    ("cvec", 16), ("ada_b0", 48), ("ada_b1", 48), ("nmg0", 8), ("nmg1", 8), ("nfg0", 8), ("nfg1", 8), ("fing", 8),
    ("adw_w", 124), ("adw_b", 4), ("aln_g", 4), ("aln_b", 4), ("bcw", 16), ("bcb", 4), ("bgb", 16), ("blam", 8),
    ("ccw", 12), ("flags", 8), ("flags2", 8),
]
VOFF = {}
_o = 0
for _n, _c in VEC_LAYOUT:
    VOFF[_n] = _o
    _o += _c
NV = _o

ARENA_BYTES = 121 * 1024


class Rot:
    def __init__(self, AR, name, shape, dtype, n=2):
        self.bufs = [AR.alloc(f"{name}{i}", shape, dtype) for i in range(n)]
        self.i = 0
        self.AR = AR

    def next(self):
        b = self.bufs[self.i % len(self.bufs)]
        self.i += 1
        return b

    def free(self):
        self.AR.free(*self.bufs)


class _Done(Exception):
    pass


def build(stage="full"):
    nc = bass.Bass("TRN2", target_bir_lowering=False)
    try:
        _build(nc, stage)
    except _Done:
        pass
    return nc


def _build(nc, stage):
    dr = {}

    def din(name, shape):
        dr[name] = nc.dram_tensor(name, list(shape), F32, kind="ExternalInput").ap()

    din("xe", [NL0, D])
    din("ctxb", [CTX, D])
    din("vecT", [128, NV])
    din("ident", [128, 128])
    din("ada_w", [2, D, 6 * D])
    din("w_in0", [D, 2048])
    din("w_in1", [D, 3072])
    din("w_out", [2, D, D])
    din("gate_w", [16, 128, 128])
    din("router_w", [D, 16])
    din("rb18", [128, 288])
    din("selm", [16, 16 * 128])
    din("wg", [2, 16, D, 512])
    din("wu", [2, 16, D, 512])
    din("wd", [2, 16, 512, D])
    din("ug", [8, 128, 576])
    din("eg", [4, 8, 128, 768])
    yout = nc.dram_tensor("y", [TOK, D], F32, kind="ExternalOutput").ap()
    dbg = None
    if stage != "full":
        dbg = nc.dram_tensor("dbg", [128, KD, NT0], F32, kind="ExternalOutput").ap()
    abD = nc.dram_tensor("abD", [8, 2, 128, TOK], F32, kind="Internal").ap()
    cc_src = nc.dram_tensor("cc_src", [128, 16], F32, kind="Internal").ap()
    cc_dst = nc.dram_tensor("cc_dst", [4 * 128, 16], F32, kind="Internal").ap()
    groups = [[0, 1, 2, 3], [4, 5, 6, 7]]

    P = Prog(nc)
    AR = Arena(nc, ARENA_BYTES)
    PS = PSum(nc)

    def sb(name, shape, dt=F32):
        return Buf(name, nc.alloc_sbuf_tensor("sb_" + name, list(shape), dt)[:])

    hT = sb("hT", [128, KD, NT0])
    ident = sb("identf", [128, 128])
    identb = sb("identb", [128, 128], BF16)
    onesb = sb("onesb", [128, 128], BF16)
    vec = sb("vec", [128, NV])
    modT = [sb(f"mod{l}", [128, 48, 2]) for l in range(2)]
    scl = sb("scl", [128, 2, 6, 2, 8])
    condT = sb("condT", [128, 8, 2], BF16)
    lruc = sb("lruc", [128, 2, 8])
    lrut = sb("lrut", [128, 8, 8])
    rw = sb("rw", [128, 8, 16])
    rb18 = sb("rb18", [128, 288])
    selm = sb("selm", [16, 16, 128], BF16)
    zer = sb("zer", [128, 288])
    Sm = sb("Sm", [128, 4, 2, 5, 2])
    RS = sb("RS", [128, 4, 2, 4])
    hend = sb("hend", [128, 2, 4])
    summ = sb("summ", [128, 16])
    gath = sb("gath", [128, 4, 16])
    Hin = sb("Hin", [128, 2, 4])
    tiny = sb("tiny", [128, 8, 4])

    DB = {}

    def dres(name):
        if name not in DB:
            DB[name] = Buf(name, None)
        return DB[name]

    def hres(k, c0, c1):
        out = []
        if c1 > CTX0:
            out.append(hT.r((k, "x")))
        if c0 < CTX0:
            for c in range(4):
                lo = 0 if c == 0 else OWN0 + 512 * c
                hi = NL0 if c == 3 else OWN0 + 512 * (c + 1)
                if c0 < hi and lo < min(c1, CTX0):
                    out.append(hT.r((k, c)))
        return out

    V = lambda name, i=0, n=1: vec.ap[:, VOFF[name] + i: VOFF[name] + i + n]

    def cut(name, dumps=None):
        if stage != name:
            return
        fin = []
        rd = []
        for k in range(8):
            rd += hres(k, 0, NT0)
        if dumps:
            for (dst, src, reads) in dumps:
                fin.append(P.dma("sp", I_dma(dst, src), reads=reads, writes=[dres("dbgx").r()]))
        else:
            fin.append(P.dma("sp", I_dma(dbg[:, :, :], hT.ap), reads=rd, writes=[dres("dbg").r()]))
        P.emit({"sp": fin})
        raise _Done()

    P.dma("sp", I_dma(ident.ap, dr["ident"][:, :]), writes=[ident.r()])
    P.dma("sp", I_dma(vec.ap, dr["vecT"][:, :]), writes=[vec.r()])
    P.dma("sp", I_dma(rw.ap, dr["router_w"].rearrange("(k p) n -> p k n", p=128)), writes=[rw.r()])
    P.dma("sp", I_dma(rb18.ap, dr["rb18"][:, :]), writes=[rb18.r()])
    P.dma("pool", I_dma(selm.ap, dr["selm"].rearrange("p (e n) -> p e n", e=16)), writes=[selm.r()])
    P.dve(I_copy(identb.ap, ident.ap), reads=[ident.r()], writes=[identb.r()])
    P.dve(I_memset(onesb.ap, 1.0), writes=[onesb.r()])
    P.dve(I_memset(zer.ap, 0.0), writes=[zer.r()])
    P.act(I_act(condT.ap.rearrange("p k t -> p t k"), V("cvec", 0, 16).rearrange("p (t k) -> p t k", t=2), AF.Silu),
          reads=[vec.r()], writes=[condT.r()])

    cut("c0")
    psmod = PS.b[7]

    def emit_mod(l, pieces):
        for pc in pieces:
            wb = AR.alloc("adaw", [128, 8, 512], BF16)
            P.dma("pool", I_dma(wb.ap, dr["ada_w"][l, :, pc * 512:(pc + 1) * 512].rearrange("(k p) n -> p k n", p=128)),
                  writes=[wb.r()])
            pr = psmod.r()
            base = 320 + (l * 48 + pc * 4) * 2
            for o4 in range(4):
                pap = psmod.ap[:, base + o4 * 2:base + o4 * 2 + 2]
                for k in range(8):
                    P.pe(I_mm(pap, wb.ap[:, k, o4 * 128:(o4 + 1) * 128], condT.ap[:, k, :], k == 0, k == 7),
                         reads=[wb.r(), condT.r()], writes=[pr])
            pp = psmod.ap[:, base:base + 8].rearrange("p (o t) -> p o t", t=2)
            for t in range(2):
                P.dve(I_tt(modT[l].ap[:, pc * 4:pc * 4 + 4, t], pp[:, :, t], V(f"ada_b{l}", pc * 4, 4), ALU.add),
                      reads=[pr, vec.r()], writes=[modT[l].r(pc * 4 + o) for o in range(4)])
            AR.free(wb)

    def emit_scl(l, kinds):
        for t in range(2):
            m = modT[l].ap
            if "n1" in kinds:
                P.dve(I_stt(scl.ap[:, l, 0, t, :], m[:, 8:16, t], 1.0, V(f"nmg{l}", 0, 8), ALU.add, ALU.mult),
                      reads=[modT[l].r(o) for o in range(8, 16)] + [vec.r()], writes=[scl.r((l, 0, t))])
                P.dve(I_copy(scl.ap[:, l, 1, t, :], m[:, 0:8, t]), reads=[modT[l].r(o) for o in range(0, 8)], writes=[scl.r((l, 1, t))])
            if "g1" in kinds:
                P.dve(I_copy(scl.ap[:, l, 2, t, :], m[:, 16:24, t]), reads=[modT[l].r(o) for o in range(16, 24)], writes=[scl.r((l, 2, t))])
            if "n2" in kinds:
                P.dve(I_stt(scl.ap[:, l, 3, t, :], m[:, 32:40, t], 1.0, V(f"nfg{l}", 0, 8), ALU.add, ALU.mult),
                      reads=[modT[l].r(o) for o in range(32, 40)] + [vec.r()], writes=[scl.r((l, 3, t))])
                P.dve(I_copy(scl.ap[:, l, 4, t, :], m[:, 24:32, t]), reads=[modT[l].r(o) for o in range(24, 32)], writes=[scl.r((l, 4, t))])
            if "g2" in kinds:
                P.dve(I_copy(scl.ap[:, l, 5, t, :], m[:, 40:48, t]), reads=[modT[l].r(o) for o in range(40, 48)], writes=[scl.r((l, 5, t))])

    cut("c0b")
    xs = Rot(AR, "xs", [128, D], F32, 4)
    tiles = [("xe", t * 128, min(128, NL0 - t * 128), t * 128) for t in range(17)] + \
            [("ctxb", t * 128, 128, CTX0 + t * 128) for t in range(2)]
    for ti, (src, r0, rows, col) in enumerate(tiles):
        b = xs.next()
        P.dma("sp", I_dma(b.ap[0:rows, :], dr[src][r0:r0 + rows, :]), writes=[b.r()])
        for g in range(2):
            bank = PS.next("xt", [4, 5, 6])
            for kk in range(4):
                k = g * 4 + kk
                P.pe(I_tr(bank.ap[:, kk * 128:kk * 128 + rows], b.ap[0:rows, k * 128:(k + 1) * 128], ident.ap[0:rows, 0:rows]),
                     reads=[b.r(), ident.r()], writes=[bank.r()])
            srcap = bank.ap.rearrange("p (a b) -> p a b", a=4)[:, :, 0:rows]
            dst = hT.ap[:, 4 * g:4 * g + 4, col:col + rows]
            wr = []
            for k in range(4 * g, 4 * g + 4):
                wr += hres(k, col, col + rows)
            if (ti + g) % 2 == 0:
                P.dve(I_copy(dst, srcap), reads=[bank.r()], writes=wr)
            else:
                P.act(I_act(dst, srcap, AF.Copy), reads=[bank.r()], writes=wr)
            if ti == 0 and g == 0:
                cut("x0")
            if ti == 0 and g == 1:
                cut("x1")
            if ti == 1 and g == 1:
                cut("x2")
    xs.free()
    emit_mod(0, range(0, 4))
    cut("s0")
    emit_scl(0, ["n1"])

    cut("c1")

    def hchunk(c0, c1, nT, n0, t, ci):
        return dict(w=c1 - c0, src3=hT.ap[:, :, c0:c1], srck=lambda k: hT.ap[:, k, c0:c1], rres=lambda k: hres(k, c0, c1),
                    dst=lambda k: nT.ap[:, k, n0:n0 + (c1 - c0)], dres=lambda k: nT.r((k, ci)), t=t)

    def emit_norm(l, kind, chunks, router=None, gvec=None, wmax=512, nsq=2, ci0=0):
        gsk, shk = (0, 1) if kind == 1 else (3, 4)
        ncols_all = sum(ch["w"] for ch in chunks)
        sq = Rot(AR, "nsq", [128, 8, wmax], BF16, nsq)
        sd = Rot(AR, "nsd", [128, wmax], F32, 2)
        rstd = AR.alloc("nrstd", [128, ncols_all], F32)
        offs = []
        o_ = 0
        for ci, ch in enumerate(chunks):
            w = ch["w"]
            offs.append(o_)
            hr = []
            for k in range(8):
                hr += ch["rres"](k)
            sq_ = sq.next()
            sd_ = sd.next()
            P.act(I_act(sq_.ap[:, :, 0:w], ch["src3"], AF.Square), reads=hr, writes=[sq_.r()])
            bank = PS.next("nrm", [4, 5])
            for k in range(8):
                P.pe(I_mm(bank.ap[:, 0:w], onesb.ap, sq_.ap[:, k, 0:w], k == 0, k == 7), reads=[sq_.r(), onesb.r()], writes=[bank.r()])
            P.act(I_act(sd_.ap[:, 0:w], bank.ap[:, 0:w], AF.Sqrt, bias=EPS, scale=1.0 / D), reads=[bank.r()], writes=[sd_.r()])
            P.dve(I_recip(rstd.ap[:, o_:o_ + w], sd_.ap[:, 0:w]), reads=[sd_.r()], writes=[rstd.r(ci)])
            o_ += w
        sq.free()
        sd.free()
        tmp = Rot(AR, "ntmp", [128, wmax], F32, 3)
        n2f = Rot(AR, "n2f", [128, wmax], F32, 3) if router is not None else None
        cnt = 0
        for ci, ch in enumerate(chunks):
            w = ch["w"]
            t = ch["t"]
            rs_ap = rstd.ap[:, offs[ci]:offs[ci] + w]
            for k in range(8):
                tb = tmp.next()
                P.dve(I_tt(tb.ap[:, 0:w], ch["srck"](k), rs_ap, ALU.mult), reads=ch["rres"](k) + [rstd.r(ci)], writes=[tb.r()])
                if kind == 3:
                    P.act(I_act(ch["dst"](k), tb.ap[:, 0:w], AF.Identity, scale=gvec(k)), reads=[tb.r(), vec.r()], writes=[ch["dres"](k)])
                    continue
                gs = scl.ap[:, l, gsk, t, k:k + 1]
                sh = scl.ap[:, l, shk, t, k:k + 1]
                sr = [scl.r((l, gsk, t)), scl.r((l, shk, t))]
                if router is None:
                    P.act(I_act(ch["dst"](k), tb.ap[:, 0:w], AF.Identity, bias=sh, scale=gs), reads=[tb.r()] + sr, writes=[ch["dres"](k)])
                else:
                    fb = n2f.next()
                    P.act(I_act(fb.ap[:, 0:w], tb.ap[:, 0:w], AF.Identity, bias=sh, scale=gs), reads=[tb.r()] + sr, writes=[fb.r()])
                    cnt += 1
                    if cnt % 3 == 0:
                        P.act(I_act(ch["dst"](k), fb.ap[:, 0:w], AF.Copy), reads=[fb.r()], writes=[ch["dres"](k)])
                    elif cnt % 3 == 1:
                        P.dve(I_copy(ch["dst"](k), fb.ap[:, 0:w]), reads=[fb.r()], writes=[ch["dres"](k)])
                    else:
                        P.pool(I_copy(ch["dst"](k), fb.ap[:, 0:w]), reads=[fb.r()], writes=[ch["dres"](k)])
                    tile0 = router["tile0"][ci0 + ci]
                    for tt in range(w // 128):
                        P.pe(I_mm(router["ps"].ap[:, (tile0 + tt) * 16:(tile0 + tt) * 16 + 16], fb.ap[:, tt * 128:(tt + 1) * 128], rw.ap[:, k, :], False, k == 7, skip=True),
                             reads=[fb.r(), rw.r()], writes=[router["ps"].r()])
        AR.free(rstd)
        tmp.free()
        if n2f is not None:
            n2f.free()

    CH0 = [(i * 416, (i + 1) * 416) for i in range(5)] + [(CTX0, NT0)]
    OC = [(OWN0 + 512 * c, OWN0 + 512 * (c + 1)) for c in range(4)] + [(CTX0, NT0)]

    def ucol(c0):
        return c0 if c0 < CTX0 else UCTX

    def mcol(c0):
        return c0 - OWN0 if c0 < CTX0 else TOK

    nT = AR.alloc("nT", [128, 8, NT0], BF16)
    emit_norm(0, 1, [hchunk(c0, c1, nT, c0, 0 if c0 < CTX0 else 1, ci) for ci, (c0, c1) in enumerate(CH0)])

    TOP = ARENA_BYTES
    gB = AR.alloc("gB", [128, 4, NT0], BF16, at=TOP - 18688)
    uA = AR.alloc("uA", [128, 4, UW], BF16, at=TOP - 18688 - 2048 - 18944)
    uB = AR.alloc("uB", [128, 4, UW], BF16, at=TOP - 18688 - 2048 - 2 * 18944)
    for ub in (uA, uB):
        P.pool(I_memset(ub.ap[:, :, NL0:NL0 + 15], 0.0), writes=[ub.r((j, 5)) for j in range(4)])
        P.pool(I_memset(ub.ap[:, :, UCTX + CTX:UW], 0.0), writes=[ub.r((j, 5)) for j in range(4)])
    w0 = dr["w_in0"]

    def load_w(name, segs, src):
        ncols = sum(n for _, n in segs)
        wb = AR.alloc(name, [128, 8, ncols], BF16)
        o = 0
        for si, (c0, n) in enumerate(segs):
            P.dma("pool", I_dma(wb.ap[:, :, o:o + n], src[:, c0:c0 + n].rearrange("(k p) n -> p k n", p=128)), writes=[wb.r(si)])
            o += n
        return wb

    tA = Rot(AR, "tA", [128, 416], F32, 3)

    def proj(wb, seg, wc0, ci, c0, c1):
        bank = PS.next("proj", [0, 1, 2, 3])
        w = c1 - c0
        for k in range(8):
            P.pe(I_mm(bank.ap[:, 0:w], wb.ap[:, k, wc0:wc0 + 128], nT.ap[:, k, c0:c1], k == 0, k == 7),
                 reads=[wb.r(seg), nT.r((k, ci))], writes=[bank.r()])
        return bank

    for p in range(2):
        wb = load_w("wA", [(2 * p * 128, 256), (512 + 2 * p * 128, 256)], w0)
        for ci, (c0, c1) in enumerate(CH0):
            w = c1 - c0
            for jj in range(2):
                j = 2 * p + jj
                pv = proj(wb, 0, jj * 128, ci, c0, c1)
                pg = proj(wb, 1, 256 + jj * 128, ci, c0, c1)
                sg = tA.next()
                P.act(I_act(sg.ap[:, 0:w], pg.ap[:, 0:w], AF.Sigmoid), reads=[pg.r()], writes=[sg.r()])
                P.dve(I_tt(uA.ap[:, j, ucol(c0):ucol(c0) + w], pv.ap[:, 0:w], sg.ap[:, 0:w], ALU.mult),
                      reads=[pv.r(), sg.r()], writes=[uA.r((j, ci))])
        AR.free(wb)
    wb = load_w("wBu", [(1024, 512)], w0)
    for j in range(4):
        for ci, (c0, c1) in enumerate(CH0):
            w = c1 - c0
            pu = proj(wb, 0, j * 128, ci, c0, c1)
            P.act(I_act(uB.ap[:, j, ucol(c0):ucol(c0) + w], pu.ap[:, 0:w], AF.Copy), reads=[pu.r()], writes=[uB.r((j, ci))])
    AR.free(wb)
    for ub in (uA, uB):
        for j in range(4):
            P.dve(I_ts(ub.ap[:, j, 0:HAL0], ub.ap[:, j, 0:HAL0], V("flags", 0), None, ALU.mult), reads=[ub.r((j, 0)), vec.r()], writes=[ub.r((j, 0))])
            P.dve(I_ts(ub.ap[:, j, NL0 - HAL0:NL0], ub.ap[:, j, NL0 - HAL0:NL0], V("flags", 1), None, ALU.mult), reads=[ub.r((j, 4)), vec.r()], writes=[ub.r((j, 4))])
    wb = load_w("wBg", [(1536, 512)], w0)
    for j in range(4):
        for ci, (c0, c1) in enumerate(CH0):
            w = c1 - c0
            pg = proj(wb, 0, j * 128, ci, c0, c1)
            x2 = tA.next()
            P.act(I_act(x2.ap[:, 0:w], pg.ap[:, 0:w], AF.Square), reads=[pg.r()], writes=[x2.r()])
            P.dve(I_ts(x2.ap[:, 0:w], x2.ap[:, 0:w], 0.044715, 1.0, ALU.mult, ALU.add), reads=[x2.r()], writes=[x2.r()])
            t2 = tA.next()
            P.dve(I_tt(t2.ap[:, 0:w], pg.ap[:, 0:w], x2.ap[:, 0:w], ALU.mult), reads=[pg.r(), x2.r()], writes=[t2.r()])
            P.act(I_act(t2.ap[:, 0:w], t2.ap[:, 0:w], AF.Sigmoid, scale=1.5957691216), reads=[t2.r()], writes=[t2.r()])
            P.dve(I_tt(gB.ap[:, j, c0:c1], pg.ap[:, 0:w], t2.ap[:, 0:w], ALU.mult), reads=[pg.r(), t2.r()], writes=[gB.r((j, ci))])
    AR.free(wb)
    tA.free()
    AR.free(nT)

    def allres(buf, j, n=6):
        return [buf.r((j, ci)) for ci in range(n)]

    emit_mod(0, range(4, 6))
    emit_scl(0, ["g1"])
    diagB = AR.alloc("diagB", [128, 16, 128], BF16)
    for k in range(4):
        for j in range(4):
            P.dve(I_ts(diagB.ap[:, k * 4 + j, :], identb.ap, V("bcw", k * 4 + j), None, ALU.mult),
                  reads=[identb.r(), vec.r()], writes=[diagB.r(k * 4 + j)])
    gw = AR.alloc("gw", [128, 16, 128], BF16)
    P.dma("pool", I_dma(gw.ap, dr["gate_w"].rearrange("g k j -> k g j")), writes=[gw.r()])
    L = lambda i: lrut.ap[:, i, :]
    lr = lrut.r()
    P.dve(I_ts(L(0), V("blam", 0, 8), -1.0, None, ALU.mult), reads=[vec.r()], writes=[lr])
    P.dve(I_tt(L(1), L(0), V("blam", 0, 8), ALU.max), reads=[lr, vec.r()], writes=[lr])
    P.act(I_act(L(2), L(1), AF.Exp, scale=-1.0), reads=[lr], writes=[lr])
    P.dve(I_ts(L(3), L(2), 2.0, None, ALU.add), reads=[lr], writes=[lr])
    P.dve(I_recip(L(3), L(3)), reads=[lr], writes=[lr])
    P.dve(I_tt(L(3), L(3), L(2), ALU.mult), reads=[lr], writes=[lr])
    P.dve(I_tt(L(4), L(3), L(3), ALU.mult), reads=[lr], writes=[lr])
    P.dve(I_ts(L(5), L(4), 1.0 / 13, 1.0 / 11, ALU.mult, ALU.add), reads=[lr], writes=[lr])
    for cf in (1.0 / 9, 1.0 / 7, 1.0 / 5, 1.0 / 3, 1.0):
        P.dve(I_tt(L(5), L(5), L(4), ALU.mult), reads=[lr], writes=[lr])
        P.dve(I_ts(L(5), L(5), cf, None, ALU.add), reads=[lr], writes=[lr])
    P.dve(I_tt(L(5), L(5), L(3), ALU.mult), reads=[lr], writes=[lr])
    P.dve(I_ts(L(6), L(0), 0.0, None, ALU.max), reads=[lr], writes=[lr])
    P.dve(I_stt(L(6), L(5), 2.0, L(6), ALU.mult, ALU.add), reads=[lr], writes=[lr])
    P.dve(I_ts(lruc.ap[:, 0, :], L(6), -8.0, None, ALU.mult), reads=[lr], writes=[lruc.r()])
    P.dve(I_ts(lruc.ap[:, 1, :], L(6), -16.0, None, ALU.mult), reads=[lr], writes=[lruc.r()])

    mBc = AR.alloc("mBc", [128, 4, CTX], BF16, at=TOP - 18688 - 2048)
    hbg = sb("hbg", [128, 16])
    lrc2 = sb("lrc2", [128, 2, 8])
    P.dve(I_ts(hbg.ap, V("bgb", 0, 16), 0.5, None, ALU.mult), reads=[vec.r()], writes=[hbg.r()])
    P.dve(I_ts(lrc2.ap[:, 0, :], lruc.ap[:, 0, :], 0.5, None, ALU.mult), reads=[lruc.r()], writes=[lrc2.r()])
    P.dve(I_ts(lrc2.ap[:, 1, :], lruc.ap[:, 0, :], 256.0, None, ALU.mult), reads=[lruc.r()], writes=[lrc2.r()])
    vFs = [AR.alloc(f"vF{i}", [128, 512], F32) for i in range(5)]
    vbs = [AR.alloc(f"vb{i}", [128, 512], BF16) for i in range(5)]
    aR = Rot(AR, "lra", [128, 512], F32, 5)
    sR = Rot(AR, "lrs", [128, 512], F32, 5)
    ivR = Rot(AR, "lriv", [128, 512], F32, 5)
    hlR = Rot(AR, "lrhl", [128, 512], F32, 2)
    hcf = AR.alloc("hcf", [128, CTX], F32)
    hcr = AR.alloc("hcr", [128, CTX], F32)
    for j in range(4):
        for ci, (c0, c1) in enumerate(OC):
            w = c1 - c0
            bank = PS.next("cv", [0, 1])
            for k in range(4):
                o = ucol(c0) + k - 2
                P.pe(I_mm(bank.ap[:, 0:w], diagB.ap[:, k * 4 + j, :], uB.ap[:, j, o:o + w], k == 0, k == 3),
                     reads=[diagB.r(k * 4 + j)] + allres(uB, j), writes=[bank.r()])
            P.act(I_act(vFs[ci].ap[:, 0:w], bank.ap[:, 0:w], AF.Identity, bias=V("bcb", j)), reads=[bank.r(), vec.r()], writes=[vFs[ci].r()])
            P.pool(I_copy(vbs[ci].ap[:, 0:w], vFs[ci].ap[:, 0:w]), reads=[vFs[ci].r()], writes=[vbs[ci].r()])
        for d in range(2):
            c1h = lrc2.ap[:, 0, d * 4 + j:d * 4 + j + 1]
            c1f = lruc.ap[:, 0, d * 4 + j:d * 4 + j + 1]
            bufs = []
            for ci, (c0, c1) in enumerate(OC):
                w = c1 - c0
                W = slice(0, w)
                pr = PS.next("gt", [2, 3, 4, 5])
                pi = PS.next("gt", [2, 3, 4, 5])
                P.pe(I_mm(pr.ap[:, W], gw.ap[:, (d * 2 + 0) * 4 + j, :], vbs[ci].ap[:, W]), reads=[gw.r(), vbs[ci].r()], writes=[pr.r()])
                P.pe(I_mm(pi.ap[:, W], gw.ap[:, (d * 2 + 1) * 4 + j, :], vbs[ci].ap[:, W]), reads=[gw.r(), vbs[ci].r()], writes=[pi.r()])
                a_, s_, iv = aR.next(), sR.next(), ivR.next()
                bufs.append((a_, s_, iv))
                gi = (d * 2 + 0) * 4 + j
                if ci < 4:
                    P.act(I_act(a_.ap[:, W], pr.ap[:, W], AF.Tanh, bias=hbg.ap[:, gi:gi + 1], scale=0.5, accum_out=RS.ap[:, j, d, ci:ci + 1]),
                          reads=[pr.r(), hbg.r()], writes=[a_.r(), RS.r((j, d))])
                else:
                    P.act(I_act(a_.ap[:, W], pr.ap[:, W], AF.Tanh, bias=hbg.ap[:, gi:gi + 1], scale=0.5), reads=[pr.r(), hbg.r()], writes=[a_.r()])
                gi = (d * 2 + 1) * 4 + j
                P.act(I_act(iv.ap[:, W], pi.ap[:, W], AF.Tanh, bias=hbg.ap[:, gi:gi + 1], scale=0.5), reads=[pi.r(), hbg.r()], writes=[iv.r()])
                P.act(I_act(a_.ap[:, W], a_.ap[:, W], AF.Exp, bias=c1h, scale=c1h), reads=[a_.r(), lrc2.r()], writes=[a_.r()])
                P.pool(I_tt(s_.ap[:, W], a_.ap[:, W], a_.ap[:, W], ALU.mult), reads=[a_.r()], writes=[s_.r()])
                P.dve(I_stt(iv.ap[:, W], iv.ap[:, W], 1.0, vFs[ci].ap[:, W], ALU.add, ALU.mult), reads=[iv.r(), vFs[ci].r()], writes=[iv.r()])
            for ci, (c0, c1) in enumerate(OC):
                w = c1 - c0
                W = slice(0, w)
                a_, s_, iv = bufs[ci]
                P.act(I_act(s_.ap[:, W], s_.ap[:, W], AF.Sqrt, bias=0.25, scale=-0.25), reads=[s_.r()], writes=[s_.r()])
                b_ = iv
                P.dve(I_tt(b_.ap[:, W], iv.ap[:, W], s_.ap[:, W], ALU.mult), reads=[iv.r(), s_.r()], writes=[iv.r()])
                if ci < 4:
                    hl = hlR.next()
                    if d == 0:
                        P.dve(I_scan(hl.ap[:, W], a_.ap[:, W], b_.ap[:, W], 0.0), reads=[a_.r(), b_.r()], writes=[hl.r()])
                        end = hl.ap[:, w - 1:w]
                    else:
                        P.dve(I_scan(hl.ap[:, W][:, ::-1], a_.ap[:, W][:, ::-1], b_.ap[:, W][:, ::-1], 0.0), reads=[a_.r(), b_.r()], writes=[hl.r()])
                        end = hl.ap[:, 0:1]
                    P.dve(I_copy(Sm.ap[:, j, d, ci, 1:2], end), reads=[hl.r()], writes=[Sm.r((j, d, "h"))])
                    P.dma("sp", I_dma(abD[d * 4 + j, 0, :, ci * 512:(ci + 1) * 512], a_.ap[:, W]), reads=[a_.r()], writes=[dres(f"ab{j}{d}").r(("a", ci))])
                    P.dma("sp", I_dma(abD[d * 4 + j, 1, :, ci * 512:(ci + 1) * 512], b_.ap[:, W]), reads=[b_.r()], writes=[dres(f"ab{j}{d}").r(("b", ci))])
                else:
                    hc = hcf if d == 0 else hcr
                    if d == 0:
                        P.dve(I_scan(hc.ap, a_.ap[:, W], b_.ap[:, W], 0.0), reads=[a_.r(), b_.r()], writes=[hc.r()])
                        P.dve(I_copy(hend.ap[:, 0, j:j + 1], hc.ap[:, CTX - 1:CTX]), reads=[hc.r()], writes=[hend.r()])
                    else:
                        P.dve(I_scan(hc.ap[:, ::-1], a_.ap[:, W][:, ::-1], b_.ap[:, W][:, ::-1], 0.0), reads=[a_.r(), b_.r()], writes=[hc.r()])
                        P.dve(I_copy(hend.ap[:, 1, j:j + 1], hc.ap[:, 0:1]), reads=[hc.r()], writes=[hend.r()])
            P.act(I_act(Sm.ap[:, j, d, 0:4, 0], RS.ap[:, j, d, :], AF.Exp, bias=lrc2.ap[:, 1, d * 4 + j:d * 4 + j + 1], scale=c1h),
                  reads=[RS.r((j, d)), lrc2.r()], writes=[Sm.r((j, d, "p"))])
        P.dve(I_tt(hcf.ap, hcf.ap, hcr.ap, ALU.add), reads=[hcf.r(), hcr.r()], writes=[hcf.r()])
        P.dve(I_tt(mBc.ap[:, j, :], hcf.ap, gB.ap[:, j, CTX0:NT0], ALU.mult), reads=[hcf.r()] + allres(gB, j), writes=[mBc.r(j)])
    aR.free()
    sR.free()
    ivR.free()
    hlR.free()
    AR.free(*vFs)
    AR.free(*vbs)
    AR.free(hcf, hcr, diagB, gw, uB)

    smr = [Sm.r((j, d, x)) for j in range(4) for d in range(2) for x in ("p", "h")]
    tr_ = tiny.r()
    TV = lambda i: tiny.ap[:, i, :]
    for d in range(2):
        order = [0, 1, 2, 3] if d == 0 else [3, 2, 1, 0]
        Pc = lambda c: Sm.ap[:, :, d, c, 0]
        Hc = lambda c: Sm.ap[:, :, d, c, 1]
        Hs = summ.ap[:, d * 8 + 4:d * 8 + 8]
        Ps = summ.ap[:, d * 8:d * 8 + 4]
        P.dve(I_copy(Hs, Hc(order[0])), reads=smr, writes=[summ.r()])
        P.dve(I_copy(Ps, Pc(order[0])), reads=smr, writes=[summ.r()])
        for c in order[1:]:
            P.dve(I_tt(Hs, Hs, Pc(c), ALU.mult), reads=smr + [summ.r()], writes=[summ.r()])
            P.dve(I_tt(Hs, Hs, Hc(c), ALU.add), reads=smr + [summ.r()], writes=[summ.r()])
            P.dve(I_tt(Ps, Ps, Pc(c), ALU.mult), reads=smr + [summ.r()], writes=[summ.r()])
    P.dma("sp", I_dma(cc_src[:, :], summ.ap), reads=[summ.r()], writes=[dres("ccs").r()])
    P.add("pool", lambda e: e.collective_compute("AllGather", ALU.bypass, replica_groups=groups, ins=[cc_src[:, :]], outs=[cc_dst[:, :]]),
          reads=[dres("ccs").r()], writes=[dres("ccd").r()], kind="cc")
    P.dma("sp", I_dma(gath.ap, cc_dst.rearrange("(r p) n -> p r n", p=128)), reads=[dres("ccd").r()], writes=[gath.r()])
    diagA = AR.alloc("diagA", [128, 124, 128], BF16)
    for k in range(31):
        for j in range(4):
            P.dve(I_ts(diagA.ap[:, k * 4 + j, :], identb.ap, V("adw_w", k * 4 + j), None, ALU.mult),
                  reads=[identb.r(), vec.r()], writes=[diagA.r(k * 4 + j)])
    mA = AR.alloc("mA", [128, 4, TOK + CTX], BF16, at=TOP - 18688 - 2048 - 18944 - 18432)
    cf = AR.alloc("cf", [128, 4, 512], F32)
    cb = AR.alloc("cb", [128, 4, 512], BF16)
    csq = AR.alloc("csq", [128, 4, 512], BF16)
    mean = AR.alloc("mean", [128, 512], F32)
    m2 = AR.alloc("m2", [128, 512], F32)
    rstdA = AR.alloc("rstdA", [128, 512], F32)
    tln = Rot(AR, "tln", [128, 512], F32, 2)
    for ci, (c0, c1) in enumerate(OC):
        w = c1 - c0
        W = slice(0, w)
        for j in range(4):
            bank = PS.b[j]
            for k in range(31):
                o = ucol(c0) + k - 15
                P.pe(I_mm(bank.ap[:, W], diagA.ap[:, k * 4 + j, :], uA.ap[:, j, o:o + w], k == 0, k == 30),
                     reads=[diagA.r(k * 4 + j)] + allres(uA, j), writes=[bank.r()])
            P.act(I_act(cf.ap[:, j, W], bank.ap[:, W], AF.Identity, bias=V("adw_b", j)), reads=[bank.r(), vec.r()], writes=[cf.r(j)])
            P.dve(I_copy(cb.ap[:, j, W], cf.ap[:, j, W]), reads=[cf.r(j)], writes=[cb.r(j)])
            P.act(I_act(csq.ap[:, j, W], cf.ap[:, j, W], AF.Square), reads=[cf.r(j)], writes=[csq.r(j)])
        bs, bq = PS.b[4], PS.b[5]
        for j in range(4):
            P.pe(I_mm(bs.ap[:, W], onesb.ap, cb.ap[:, j, W], j == 0, j == 3), reads=[cb.r(j), onesb.r()], writes=[bs.r()])
        for j in range(4):
            P.pe(I_mm(bq.ap[:, W], onesb.ap, csq.ap[:, j, W], j == 0, j == 3), reads=[csq.r(j), onesb.r()], writes=[bq.r()])
        P.act(I_act(mean.ap[:, W], bs.ap[:, W], AF.Copy, scale=1.0 / 512), reads=[bs.r()], writes=[mean.r()])
        P.dve(I_tt(m2.ap[:, W], mean.ap[:, W], mean.ap[:, W], ALU.mult), reads=[mean.r()], writes=[m2.r()])
        P.dve(I_stt(m2.ap[:, W], bq.ap[:, W], 1.0 / 512, m2.ap[:, W], ALU.mult, ALU.subtract), reads=[bq.r(), m2.r()], writes=[m2.r()])
        P.dve(I_ts(m2.ap[:, W], m2.ap[:, W], 0.0, None, ALU.max), reads=[m2.r()], writes=[m2.r()])
        P.act(I_act(m2.ap[:, W], m2.ap[:, W], AF.Sqrt, bias=EPS), reads=[m2.r()], writes=[m2.r()])
        P.dve(I_recip(rstdA.ap[:, W], m2.ap[:, W]), reads=[m2.r()], writes=[rstdA.r()])
        for j in range(4):
            t = tln.next()
            P.dve(I_tt(t.ap[:, W], cf.ap[:, j, W], mean.ap[:, W], ALU.subtract), reads=[cf.r(j), mean.r()], writes=[t.r()])
            P.dve(I_tt(t.ap[:, W], t.ap[:, W], rstdA.ap[:, W], ALU.mult), reads=[t.r(), rstdA.r()], writes=[t.r()])
            P.act(I_act(mA.ap[:, j, mcol(c0):mcol(c0) + w], t.ap[:, W], AF.Silu, bias=V("aln_b", j), scale=V("aln_g", j)),
                  reads=[t.r(), vec.r()], writes=[mA.r((j, ci))])
    tln.free()
    AR.free(diagA, cf, cb, csq, mean, m2, rstdA, uA)

    hr_ = Hin.r()
    for d in range(2):
        qs = [0, 1, 2, 3] if d == 0 else [3, 2, 1, 0]
        Hc_ = TV(d)
        Hi = Hin.ap[:, d, :]
        Pq = lambda q: gath.ap[:, q, d * 8:d * 8 + 4]
        Hq = lambda q: gath.ap[:, q, d * 8 + 4:d * 8 + 8]
        P.dve(I_copy(Hc_, hend.ap[:, d, :]), reads=[hend.r()], writes=[tr_])
        P.dve(I_ts(Hi, Hc_, V("flags", 2 + qs[0]), None, ALU.mult), reads=[tr_, vec.r()], writes=[hr_])
        for n in range(3):
            q = qs[n]
            P.dve(I_tt(Hc_, Hc_, Pq(q), ALU.mult), reads=[tr_, gath.r()], writes=[tr_])
            P.dve(I_tt(Hc_, Hc_, Hq(q), ALU.add), reads=[tr_, gath.r()], writes=[tr_])
            P.dve(I_stt(Hi, Hc_, V("flags", 2 + qs[n + 1]), Hi, ALU.mult, ALU.add), reads=[tr_, hr_, vec.r()], writes=[hr_])

    wo = AR.alloc("wo", [128, 8, D], BF16, at=TOP - 18688 - 2048 - 18944)
    P.dma("pool", I_dma(wo.ap, dr["w_out"][0].rearrange("(k p) n -> p k n", p=128)), writes=[wo.r()])
    mB = AR.alloc("mB", [128, 4, TOK], BF16)
    lar = Rot(AR, "la", [128, TOK], F32, 2)
    lbr = Rot(AR, "lb", [128, TOK], F32, 2)
    yb = AR.alloc("yb", [128, TOK], F32)
    hs = AR.alloc("hs", [128, TOK], F32)
    for j in range(4):
        for d in range(2):
            la = lar.next()
            lb = lbr.next()
            abr = dres(f"ab{j}{d}")
            P.dma("sp", I_dma(la.ap, abD[d * 4 + j, 0, :, :]), reads=[abr.r(("a", c)) for c in range(4)], writes=[la.r()])
            P.dma("sp", I_dma(lb.ap, abD[d * 4 + j, 1, :, :]), reads=[abr.r(("b", c)) for c in range(4)], writes=[lb.r()])
            init = Hin.ap[:, d, j:j + 1]
            if d == 0:
                P.dve(I_scan(yb.ap, la.ap, lb.ap, init), reads=[la.r(), lb.r(), hr_], writes=[yb.r()])
            else:
                P.dve(I_scan(hs.ap[:, ::-1], la.ap[:, ::-1], lb.ap[:, ::-1], init), reads=[la.r(), lb.r(), hr_], writes=[hs.r()])
        P.pool(I_tt(yb.ap, yb.ap, hs.ap, ALU.add), reads=[yb.r(), hs.r()], writes=[yb.r()])
        P.dve(I_tt(mB.ap[:, j, :], yb.ap, gB.ap[:, j, OWN0:OWN0 + TOK], ALU.mult), reads=[yb.r()] + allres(gB, j), writes=[mB.r(j)])
    lar.free()
    lbr.free()
    AR.free(yb, hs, gB)

    emit_mod(0, range(6, 10))
    emit_scl(0, ["n2"])

    def out_proj(l, wo, rhs_of, chunks, chunk_hook=None):
        for ci, (c0, c1, t) in enumerate(chunks):
            if chunk_hook is not None and ci >= 1:
                chunk_hook(ci - 1)
            for dk in range(8):
                w = c1 - c0
                bank = PS.next("op", [0, 1, 2, 3])
                for mk in range(8):
                    rap, rres = rhs_of(mk, ci)
                    P.pe(I_mm(bank.ap[:, 0:w], wo.ap[:, mk, dk * 128:(dk + 1) * 128], rap, mk == 0, mk == 7), reads=[wo.r()] + rres, writes=[bank.r()])
                P.dve(I_stt(hT.ap[:, dk, c0:c1], bank.ap[:, 0:w], scl.ap[:, l, 2, t, dk:dk + 1], hT.ap[:, dk, c0:c1], ALU.mult, ALU.add),
                      reads=[bank.r(), scl.r((l, 2, t))] + hres(dk, c0, c1), writes=hres(dk, c0, c1))
        if chunk_hook is not None:
            chunk_hook(len(chunks) - 1)

    def rhs0(mk, ci):
        c0, c1 = OC[ci]
        w = c1 - c0
        if mk < 4:
            return mA.ap[:, mk, mcol(c0):mcol(c0) + w], [mA.r((mk, ci))]
        if ci < 4:
            return mB.ap[:, mk - 4, mcol(c0):mcol(c0) + w], [mB.r(mk - 4)]
        return mBc.ap[:, mk - 4, :], [mBc.r(mk - 4)]

    def l0_outproj_then(norm_chunk):
        out_proj(0, wo, rhs0, [(c0, c1, 0 if c0 < CTX0 else 1) for (c0, c1) in OC], chunk_hook=norm_chunk)
        AR.free(wo, mA, mB, mBc)

    def emit_moe(l, chunks, mod_hook=None, expert_hook=None, chunk_done_hook=None, pre_norm=None):
        ncol = sum(c1 - c0 for c0, c1, _ in chunks)
        ntile = ncol // 128
        n2T = AR.alloc("n2T", [128, 8, ncol], BF16)
        rl = PS.b[6]
        P.pe(I_mm(rl.ap[:, 0:288], zer.ap[:, 0:128], zer.ap[:, 0:288], True, False, skip=True), reads=[zer.r()], writes=[rl.r()])
        nch = []
        tile0 = []
        n0 = 0
        for (c0, c1, t) in chunks:
            nch.append((c0, c1, n0, t))
            tile0.append(n0 // 128)
            n0 += c1 - c0
        descs = [hchunk(c0, c1, n2T, n0_, t, ci) for ci, (c0, c1, n0_, t) in enumerate(nch)]
        rt = dict(ps=rl, tile0=tile0)
        yield_ = lambda ci: emit_norm(l, 2, [descs[ci]], router=rt, nsq=1, ci0=ci)
        if pre_norm is not None:
            pre_norm(yield_)
        else:
            emit_norm(l, 2, descs, router=rt)
        wslots = [None, None]

        def load_expert(e):
            g = AR.alloc(f"wg{e % 2}", [128, 8, 512], BF16)
            u = AR.alloc(f"wu{e % 2}", [128, 8, 512], BF16)
            dn = AR.alloc(f"wd{e % 2}", [128, 4, D], BF16)
            P.dma("pool", I_dma(g.ap, dr["wg"][l, e].rearrange("(k p) n -> p k n", p=128)), writes=[g.r()])
            P.dma("pool", I_dma(u.ap, dr["wu"][l, e].rearrange("(k p) n -> p k n", p=128)), writes=[u.r()])
            P.dma("pool", I_dma(dn.ap, dr["wd"][l, e].rearrange("(k p) n -> p k n", p=128)), writes=[dn.r()])
            wslots[e % 2] = (g, u, dn)

        load_expert(0)
        load_expert(1)
        NT_ = ntile
        R = lambda name: AR.alloc(name, [128, 288], F32)
        sc, sel, tA_, tB_, gsc, keep = R("r_sc"), R("r_sel"), R("r_ta"), R("r_tb"), R("r_gs"), R("r_keep")
        NE = NT_ * 16
        NG = NT_ * 4
        P.act(I_act(sc.ap[:, 0:NE], rl.ap[:, 0:NE], AF.Sigmoid), reads=[rl.r()], writes=[sc.r()])
        P.dve(I_tt(sel.ap[:, 0:NE], sc.ap[:, 0:NE], rb18.ap[:, 0:NE], ALU.add), reads=[sc.r(), rb18.r()], writes=[sel.r()])
        X = lambda e_: sel.ap[:, 0:NE].rearrange("p (g e) -> p g e", e=4)[:, :, e_]
        pairs = [(0, 1), (0, 2), (0, 3), (1, 2), (1, 3), (2, 3)]
        ta = tA_.ap[:, 0:NG]
        tb = tB_.ap[:, 0:NG]
        gs_ = gsc.ap[:, 0:NG]
        thr = gsc.ap[:, NG:2 * NG]
        for pi_, (a, b) in enumerate(pairs):
            P.dve(I_tt(ta, X(a), X(b), ALU.add), reads=[sel.r()], writes=[tA_.r()])
            P.dve(I_tt(tb, X(a), X(b), ALU.min), reads=[sel.r()], writes=[tB_.r()])
            if pi_ == 0:
                P.dve(I_copy(gs_, ta), reads=[tA_.r()], writes=[gsc.r("g")])
                P.dve(I_copy(thr, tb), reads=[tB_.r()], writes=[gsc.r("t")])
            else:
                P.dve(I_tt(gs_, gs_, ta, ALU.max), reads=[tA_.r(), gsc.r("g")], writes=[gsc.r("g")])
                P.dve(I_tt(thr, thr, tb, ALU.max), reads=[tB_.r(), gsc.r("t")], writes=[gsc.r("t")])
        G = lambda g_: gsc.ap[:, 0:NG].rearrange("p (t g) -> p t g", g=4)[:, :, g_]
        gm = tA_.ap[:, 0:NT_]
        P.dve(I_tt(gm, G(0), G(1), ALU.max), reads=[gsc.r("g")], writes=[tA_.r()])
        P.dve(I_tt(gm, gm, G(2), ALU.max), reads=[gsc.r("g"), tA_.r()], writes=[tA_.r()])
        P.dve(I_tt(gm, gm, G(3), ALU.max), reads=[gsc.r("g"), tA_.r()], writes=[tA_.r()])
        ing = tB_.ap[:, 0:NG]
        for g_ in range(4):
            P.dve(I_tt(ing.rearrange("p (t g) -> p t g", g=4)[:, :, g_], G(g_), gm, ALU.is_equal), reads=[gsc.r("g"), tA_.r()], writes=[tB_.r()])
        K = lambda e_: keep.ap[:, 0:NE].rearrange("p (g e) -> p g e", e=4)[:, :, e_]
        for e_ in range(4):
            P.dve(I_tt(K(e_), X(e_), thr, ALU.is_ge), reads=[sel.r(), gsc.r("t")], writes=[keep.r()])
            P.dve(I_tt(K(e_), K(e_), ing, ALU.mult), reads=[keep.r(), tB_.r()], writes=[keep.r()])
        P.dve(I_tt(keep.ap[:, 0:NE], keep.ap[:, 0:NE], sc.ap[:, 0:NE], ALU.mult), reads=[keep.r(), sc.r()], writes=[keep.r()])
        den = tA_.ap[:, 64:64 + NT_]
        P.dve(I_red(den, keep.ap[:, 0:NE].rearrange("p (t e) -> p t e", e=16), ALU.add), reads=[keep.r()], writes=[tA_.r()])
        P.dve(I_recip(den, den), reads=[tA_.r()], writes=[tA_.r()])
        comb = sel
        for t_ in range(NT_):
            P.dve(I_ts(comb.ap[:, t_ * 16:(t_ + 1) * 16], keep.ap[:, t_ * 16:(t_ + 1) * 16], tA_.ap[:, 64 + t_:65 + t_], None, ALU.mult),
                  reads=[keep.r(), tA_.r()], writes=[sel.r()])
        chi = AR.alloc("chi", [128, 288], BF16)
        clo = AR.alloc("clo", [128, 288], BF16)
        P.dve(I_copy(chi.ap[:, 0:NE], comb.ap[:, 0:NE]), reads=[sel.r()], writes=[chi.r()])
        P.dve(I_tt(clo.ap[:, 0:NE], comb.ap[:, 0:NE], chi.ap[:, 0:NE], ALU.subtract), reads=[sel.r(), chi.r()], writes=[clo.r()])
        cT = [AR.alloc("cThi", [128, ncol], BF16), AR.alloc("cTlo", [128, ncol], BF16)]
        for hi_, src in enumerate((chi, clo)):
            t_ = 0
            while t_ < NT_:
                nb = min(8, NT_ - t_)
                bank = PS.next("ct", [4, 5])
                bap = bank.ap.bitcast(BF16)
                for q in range(nb):
                    P.pe(I_tr(bap[0:16, q * 128:(q + 1) * 128], src.ap[:, (t_ + q) * 16:(t_ + q + 1) * 16], identb.ap),
                         reads=[src.r(), identb.r()], writes=[bank.r()])
                P.act(I_act(cT[hi_].ap[0:16, t_ * 128:(t_ + nb) * 128], bap[0:16, 0:nb * 128], AF.Copy), reads=[bank.r()], writes=[cT[hi_].r()])
                t_ += nb
        AR.free(sc, sel, tA_, tB_, gsc, keep, chi, clo)
        if mod_hook is not None:
            mod_hook()
        sgr = Rot(AR, "sg", [128, 512], F32, 2)
        t1r = Rot(AR, "t1", [128, 512], F32, 2)
        hidr = Rot(AR, "hid", [128, 4, 512], BF16, 2)
        for e in range(16):
            g, u, dn = wslots[e % 2]
            n0s = []
            n0 = 0
            for (c0, c1, t) in chunks:
                n0s.append(n0)
                n0 += c1 - c0
            corder = list(range(len(chunks)))
            if e == 15 and len(chunks) >= 4:
                corder = [0, 3, 1, 2] + corder[4:]
            for cidx in corder:
                c0, c1, t = chunks[cidx]
                n0 = n0s[cidx]
                w = c1 - c0
                W = slice(0, w)
                cwb = PS.b[6]
                P.pe(I_mm(cwb.ap[:, W], selm.ap[:, e, :], cT[0].ap[0:16, n0:n0 + w], True, False), reads=[selm.r(), cT[0].r()], writes=[cwb.r()])
                P.pe(I_mm(cwb.ap[:, W], selm.ap[:, e, :], cT[1].ap[0:16, n0:n0 + w], False, True), reads=[selm.r(), cT[1].r()], writes=[cwb.r()])
                hid = hidr.next()
                for f in range(4):
                    pg = PS.next("gu", [0, 1, 2, 3])
                    pu = PS.next("gu", [0, 1, 2, 3])
                    for k in range(8):
                        P.pe(I_mm(pg.ap[:, W], g.ap[:, k, f * 128:(f + 1) * 128], n2T.ap[:, k, n0:n0 + w], k == 0, k == 7),
                             reads=[g.r(), n2T.r((k, chunks.index((c0, c1, t))))], writes=[pg.r()])
                    for k in range(8):
                        P.pe(I_mm(pu.ap[:, W], u.ap[:, k, f * 128:(f + 1) * 128], n2T.ap[:, k, n0:n0 + w], k == 0, k == 7),
                             reads=[u.r(), n2T.r((k, chunks.index((c0, c1, t))))], writes=[pu.r()])
                    sg = sgr.next()
                    t1 = t1r.next()
                    P.act(I_act(sg.ap[:, W], pg.ap[:, W], AF.Silu), reads=[pg.r()], writes=[sg.r()])
                    P.dve(I_tt(t1.ap[:, W], pu.ap[:, W], sg.ap[:, W], ALU.mult), reads=[pu.r(), sg.r()], writes=[t1.r()])
                    P.dve(I_tt(hid.ap[:, f, W], t1.ap[:, W], cwb.ap[:, W], ALU.mult), reads=[t1.r(), cwb.r()], writes=[hid.r(f)])
                for dk in range(8):
                    pd = PS.next("dn", [4, 5])
                    for f in range(4):
                        P.pe(I_mm(pd.ap[:, W], dn.ap[:, f, dk * 128:(dk + 1) * 128], hid.ap[:, f, W], f == 0, f == 3),
                             reads=[dn.r(), hid.r(f)], writes=[pd.r()])
                    P.dve(I_stt(hT.ap[:, dk, c0:c1], pd.ap[:, W], scl.ap[:, l, 5, t, dk:dk + 1], hT.ap[:, dk, c0:c1], ALU.mult, ALU.add),
                          reads=[pd.r(), scl.r((l, 5, t))] + hres(dk, c0, c1), writes=hres(dk, c0, c1))
                if e == 15 and chunk_done_hook is not None:
                    chunk_done_hook(cidx)
            AR.free(g, u, dn)
            if e + 2 < 16:
                load_expert(e + 2)
            if expert_hook is not None:
                expert_hook(e)
        sgr.free()
        t1r.free()
        hidr.free()
        AR.free(n2T, cT[0], cT[1])


    def hook0():
        emit_mod(0, range(10, 12))
        emit_scl(0, ["g2"])

    def ehook0(e):
        if 1 <= e <= 12:
            emit_mod(1, [e - 1])

    emit_moe(0, [(c0, c1, 0 if c0 < CTX0 else 1) for (c0, c1) in OC], mod_hook=hook0, expert_hook=ehook0, pre_norm=l0_outproj_then)
    emit_scl(1, ["n1", "g1", "n2", "g2"])
    cut("l0")

    NH = 448
    w1 = dr["w_in1"]

    def load_w_at(name, segs, src, at):
        ncols = sum(n for _, n in segs)
        wb = AR.alloc(name, [128, 8, ncols], BF16, at=at)
        o = 0
        for si, (c0, n) in enumerate(segs):
            P.dma("pool", I_dma(wb.ap[:, :, o:o + n], src[:, c0:c0 + n].rearrange("(k p) n -> p k n", p=128)), writes=[wb.r(si)])
            o += n
        return wb

    wbs = [load_w_at(f"wC{p}", [(2 * p * 128, 256), (1024 + 2 * p * 128, 256)], w1, ARENA_BYTES - 16384 - 16384 + 8192 * p) for p in range(2)]
    hxd_views = []
    for half in range(2):
        hx_src = nc.dram_tensor(f"hx_src{half}", [128, 4 * NH], F32, kind="Internal").ap()
        hx_dst = nc.dram_tensor(f"hx_dst{half}", [4 * 128, 4 * NH], F32, kind="Internal").ap()
        rdh0, rdh1 = [], []
        for k in range(4 * half, 4 * half + 4):
            rdh0 += hres(k, OWN0, OWN0 + 192)
            rdh1 += hres(k, OWN0 + TOK - 256, OWN0 + TOK)
        hxs = hx_src.rearrange("p (k n) -> p k n", k=4)
        ks = slice(4 * half, 4 * half + 4)
        P.dma("sp", I_dma(hxs[:, :, 0:192], hT.ap[:, ks, OWN0:OWN0 + 192]), reads=rdh0, writes=[dres(f"hxs{half}").r(0)])
        P.dma("sp", I_dma(hxs[:, :, 192:448], hT.ap[:, ks, OWN0 + TOK - 256:OWN0 + TOK]), reads=rdh1, writes=[dres(f"hxs{half}").r(1)])
        P.add("pool", (lambda e, a_=hx_src, b_=hx_dst: e.collective_compute("AllGather", ALU.bypass, replica_groups=groups, ins=[a_[:, :]], outs=[b_[:, :]])),
              reads=[dres(f"hxs{half}").r(0), dres(f"hxs{half}").r(1)], writes=[dres(f"hxd{half}").r()], kind="cc")
        hxd_views.append(hx_dst.rearrange("(r p) (k n) -> p r k n", r=4, k=4))

    NT1 = 2752
    nTo = AR.alloc("nTo", [128, 8, TOK], BF16, at=0)
    nTx = AR.alloc("nTx", [128, 8, 704], BF16, at=32768)
    hH = AR.alloc("hH", [128, 8, NH], F32)

    def ncol(c):
        if c < 256:
            return nTx, c
        if c < 2304:
            return nTo, c - 256
        if c < 2496:
            return nTx, 256 + (c - 2304)
        return nTx, 448 + (c - 2496)

    def nslice(k, c0, c1):
        b, o = ncol(c0)
        return b.ap[:, k, o:o + (c1 - c0)], b.r((k, c0))

    L1CH = [(256 + 512 * c, 256 + 512 * (c + 1)) for c in range(4)] + [(2496, 2752)]
    chs = []
    for (c0, c1) in L1CH:
        if c0 < 2496:
            h0 = OWN0 + (c0 - 256)
            tt = 0
        else:
            h0 = CTX0
            tt = 1
        b, o = ncol(c0)
        chs.append(dict(w=c1 - c0, src3=hT.ap[:, :, h0:h0 + (c1 - c0)], srck=(lambda k, h0=h0, w=c1 - c0: hT.ap[:, k, h0:h0 + w]),
                        rres=(lambda k, h0=h0, w=c1 - c0: hres(k, h0, h0 + w)),
                        dst=(lambda k, b=b, o=o, w=c1 - c0: b.ap[:, k, o:o + w]), dres=(lambda k, b=b, c0=c0: b.r((k, c0))), t=tt))
    emit_norm(1, 1, chs)
    def emit_halo():
        cand = Rot(AR, "cand", [128, 2, NH], F32, 2)
        for k in range(8):
            top = hH.ap[:, k, 0:256]
            bot = hH.ap[:, k, 256:448]
            for hf in range(2):
                cb_ = cand.next()
                P.dma("sp", I_dma(cb_.ap, hxd_views[k // 4][:, 2 * hf:2 * hf + 2, k % 4, :]), reads=[dres(f"hxd{k // 4}").r()], writes=[cb_.r()])
                for rr in range(2):
                    r = 2 * hf + rr
                    if r == 0:
                        P.dve(I_ts(top, cb_.ap[:, rr, 192:448], V("flags2", r), None, ALU.mult), reads=[cb_.r(), vec.r()], writes=[hH.r(k)])
                        P.dve(I_ts(bot, cb_.ap[:, rr, 0:192], V("flags2", 4 + r), None, ALU.mult), reads=[cb_.r(), vec.r()], writes=[hH.r(k)])
                    else:
                        P.dve(I_stt(top, cb_.ap[:, rr, 192:448], V("flags2", r), top, ALU.mult, ALU.add), reads=[cb_.r(), vec.r(), hH.r(k)], writes=[hH.r(k)])
                        P.dve(I_stt(bot, cb_.ap[:, rr, 0:192], V("flags2", 4 + r), bot, ALU.mult, ALU.add), reads=[cb_.r(), vec.r(), hH.r(k)], writes=[hH.r(k)])
        cand.free()
        cut("hx")
        chs = []
        for (c0, c1, s0) in [(0, 256, 0), (2304, 2496, 256)]:
            b, o = ncol(c0)
            chs.append(dict(w=c1 - c0, src3=hH.ap[:, :, s0:s0 + (c1 - c0)], srck=(lambda k, s0=s0, w=c1 - c0: hH.ap[:, k, s0:s0 + w]),
                            rres=(lambda k: [hH.r(k)]),
                            dst=(lambda k, b=b, o=o, w=c1 - c0: b.ap[:, k, o:o + w]), dres=(lambda k, b=b, c0=c0: b.r((k, c0))), t=0))
        emit_norm(1, 1, chs, wmax=256)
        AR.free(hH)

    ALLCH = L1CH[:4] + [(2496, 2752), (0, 256), (2304, 2496)]
    def proj1(wb, seg, wc0, c0, c1, group="proj", banks=(0, 1, 2, 3)):
        bank = PS.next(group, list(banks))
        w = c1 - c0
        for k in range(8):
            ap, rs = nslice(k, c0, c1)
            P.pe(I_mm(bank.ap[:, 0:w], wb.ap[:, k, wc0:wc0 + 128], ap, k == 0, k == 7), reads=[wb.r(seg), rs], writes=[bank.r()])
        return bank

    tC = AR.alloc("tC", [128, 4, 2496], BF16)
    tmpC = Rot(AR, "tmpC", [128, 512], F32, 3)
    CCH = L1CH[:4] + [(0, 256), (2304, 2496)]
    for part in range(2):
        if part == 1:
            emit_halo()
        for p in range(2):
            wb = wbs[p]
            for (c0, c1) in (CCH[:4] if part == 0 else CCH[4:]):
                w = c1 - c0
                for jj in range(2):
                    j = 2 * p + jj
                    px = proj1(wb, 0, jj * 128, c0, c1)
                    pc = proj1(wb, 1, 256 + jj * 128, c0, c1)
                    sg = tmpC.next()
                    P.act(I_act(sg.ap[:, 0:w], pc.ap[:, 0:w], AF.Copy), reads=[pc.r()], writes=[sg.r()])
                    P.dve(I_tt(tC.ap[:, j, c0:c1], px.ap[:, 0:w], sg.ap[:, 0:w], ALU.mult), reads=[px.r(), sg.r()], writes=[tC.r((j, c0))])
    AR.free(*wbs)
    for j in range(4):
        P.dve(I_ts(tC.ap[:, j, 255:256], tC.ap[:, j, 255:256], V("flags", 0), None, ALU.mult), reads=[tC.r((j, 0)), vec.r()], writes=[tC.r((j, 0))])
        P.dve(I_ts(tC.ap[:, j, 2304:2305], tC.ap[:, j, 2304:2305], V("flags", 1), None, ALU.mult), reads=[tC.r((j, 2304)), vec.r()], writes=[tC.r((j, 2304))])
    mC = AR.alloc("mC", [128, 4, TOK], BF16, at=ARENA_BYTES - 16384)
    wb = load_w("wCb", [(512, 512)], w1)
    tcall = lambda j: [tC.r((j, c0)) for (c0, _) in CCH]
    for j in range(4):
        for (c0, c1) in L1CH[:4]:
            w = c1 - c0
            pb = proj1(wb, 0, j * 128, c0, c1)
            cv = tmpC.next()
            P.dve(I_ts(cv.ap[:, 0:w], tC.ap[:, j, c0 - 1:c1 - 1], V("ccw", 0 * 4 + j), None, ALU.mult), reads=tcall(j) + [vec.r()], writes=[cv.r()])
            P.dve(I_stt(cv.ap[:, 0:w], tC.ap[:, j, c0:c1], V("ccw", 1 * 4 + j), cv.ap[:, 0:w], ALU.mult, ALU.add), reads=tcall(j) + [vec.r(), cv.r()], writes=[cv.r()])
            P.dve(I_stt(cv.ap[:, 0:w], tC.ap[:, j, c0 + 1:c1 + 1], V("ccw", 2 * 4 + j), cv.ap[:, 0:w], ALU.mult, ALU.add), reads=tcall(j) + [vec.r(), cv.r()], writes=[cv.r()])
            P.dve(I_tt(mC.ap[:, j, c0 - 256:c1 - 256], pb.ap[:, 0:w], cv.ap[:, 0:w], ALU.mult), reads=[pb.r(), cv.r()], writes=[mC.r((j, c0))])
    AR.free(wb, tC)
    tmpC.free()

    KT = AR.alloc("KT", [128, 4, NT1], BF16)
    Vt = AR.alloc("Vt", [128, 22, 512], BF16)
    wb = load_w("wK", [(2048, 512)], w1)
    for j in range(4):
        for (c0, c1) in ALLCH:
            w = c1 - c0
            pk = proj1(wb, 0, j * 128, c0, c1)
            if (j + c0 // 256) % 2 == 0:
                P.act(I_act(KT.ap[:, j, c0:c1], pk.ap[:, 0:w], AF.Copy), reads=[pk.r()], writes=[KT.r((j, c0))])
            else:
                P.dve(I_copy(KT.ap[:, j, c0:c1], pk.ap[:, 0:w]), reads=[pk.r()], writes=[KT.r((j, c0))])
    AR.free(wb)
    wb = load_w("wV", [(2560, 512)], w1)
    vtiles = [(128 * m, min(128, 2496 - 128 * m)) for m in range(20)] + [(2496, 128), (2624, 128)]

    def chunk_of(c):
        for (c0, c1) in ALLCH:
            if c0 <= c < c1:
                return c0
        raise ValueError(c)

    vorder = [ti for ti, (c0, n) in enumerate(vtiles) if 256 <= c0 < 2304 or c0 >= 2496] + \
             [ti for ti, (c0, n) in enumerate(vtiles) if not (256 <= c0 < 2304 or c0 >= 2496)]
    for ti in vorder:
        c0, n = vtiles[ti]
        bank = PS.next("proj", [0, 1, 2, 3])
        for k in range(8):
            ap, _ = nslice(k, c0, c0 + n)
            b_, _o = ncol(c0)
            P.pe(I_mm(bank.ap[0:n, 0:512], ap, wb.ap[:, k, 0:512], k == 0, k == 7), reads=[wb.r(0), b_.r((k, chunk_of(c0)))], writes=[bank.r()])
        if ti % 2 == 0:
            P.act(I_act(Vt.ap[0:n, ti, :], bank.ap[0:n, 0:512], AF.Copy), reads=[bank.r()], writes=[Vt.r(ti)])
        else:
            P.dve(I_copy(Vt.ap[0:n, ti, :], bank.ap[0:n, 0:512]), reads=[bank.r()], writes=[Vt.r(ti)])
    AR.free(wb, nTx)
    QT = AR.alloc("QT", [128, 4, TOK], BF16)
    wb = load_w("wQ", [(1536, 512)], w1)
    for j in range(4):
        for (c0, c1) in L1CH[:4]:
            w = c1 - c0
            pq = proj1(wb, 0, j * 128, c0, c1)
            if (j + c0 // 512) % 2 == 0:
                P.act(I_act(QT.ap[:, j, c0 - 256:c1 - 256], pq.ap[:, 0:w], AF.Copy), reads=[pq.r()], writes=[QT.r((j, (c0 - 256) // 128 + mm)) for mm in range(4)])
            else:
                P.dve(I_copy(QT.ap[:, j, c0 - 256:c1 - 256], pq.ap[:, 0:w]), reads=[pq.r()], writes=[QT.r((j, (c0 - 256) // 128 + mm)) for mm in range(4)])
    AR.free(wb, nTo)
    cut("l1a")

    din_ug = dr["ug"]
    din_eg = dr["eg"]
    UG = AR.alloc("UG", [128, 8, 576], F32)
    P.dma("sp", I_dma(UG.ap, din_ug.rearrange("h p n -> p h n")), writes=[UG.r()])
    Sbr = Rot(AR, "Sb", [128, 1024], F32, 2)
    Pbr = Rot(AR, "Pb", [128, 1024], BF16, 2)
    PTr = Rot(AR, "PT", [128, 1024], BF16, 2)
    EGr = Rot(AR, "EGb", [128, 768], F32, 2)
    mdt = Rot(AR, "mdt", [128, 512], BF16, 2)
    stat = Rot(AR, "stat", [128, 8, 4], F32, 2)
    special = {0: (0, 768, 0), 1: (128, 640, 1), 15: (1792, 704, 3)}
    units = [(m, h) for m in range(16) for h in range(8)]
    ctxs = {}
    ctxa = {}
    pair_st = {}

    def pair_info(m):
        if m in special:
            return special[m]
        return 128 * m, 576, (2 if m == 14 else None)

    def stage_a(u):
        m, h = units[u]
        kc0, nloc, egi = pair_info(m)
        ntot = nloc + 256
        if m not in pair_st:
            pair_st[m] = stat.next()
        st = pair_st[m]
        jh, hp = h // 2, h % 2
        prt = slice(hp * 64, hp * 64 + 64)
        bi = 2 * (u % 2)
        sb0, sb1 = PS.b[bi], PS.b[bi + 1]
        sres = [sb0.r(), sb1.r()]
        S2 = PS.t[:, bi:bi + 2, :].rearrange("p a b -> p (a b)")
        q_ap = QT.ap[prt, jh, 128 * m:128 * m + 128]
        qr = QT.r((jh, m))
        kr = [KT.r((jh, c0)) for (c0, c1) in ALLCH if c0 < kc0 + nloc and kc0 < c1]
        P.pe(I_mm(sb0.ap[:, 0:512], q_ap, KT.ap[prt, jh, kc0:kc0 + 512]), reads=[qr] + kr, writes=[sres[0]])
        P.pe(I_mm(sb1.ap[:, 0:nloc - 512], q_ap, KT.ap[prt, jh, kc0 + 512:kc0 + nloc]), reads=[qr] + kr, writes=[sres[1]])
        P.pe(I_mm(sb1.ap[:, nloc - 512:nloc - 512 + 256], q_ap, KT.ap[prt, jh, 2496:2752]), reads=[qr, KT.r((jh, 2496))], writes=[sres[1]])
        Sb = Sbr.next()
        if egi is None:
            bias_ap, bias_r = UG.ap[:, h, 0:nloc], [UG.r()]
        else:
            eb = EGr.next()
            P.dma("sp", I_dma(eb.ap[:, 0:nloc], din_eg[egi, h, :, 0:nloc]), writes=[eb.r()])
            bias_ap, bias_r = eb.ap[:, 0:nloc], [eb.r()]
        P.dve(I_stt(Sb.ap[:, 0:nloc], S2[:, 0:nloc], 0.125, bias_ap, ALU.mult, ALU.add), reads=sres + bias_r, writes=[Sb.r()])
        P.act(I_act(Sb.ap[:, nloc:ntot], S2[:, nloc:ntot], AF.Copy, scale=0.125), reads=sres, writes=[Sb.r()])
        P.dve(lambda e, o=st.ap[:, h, 0:1], i=Sb.ap[:, 0:ntot]: e.tensor_reduce(out=o, in_=i, axis=AX.X, op=ALU.max, negate=True),
              reads=[Sb.r()], writes=[st.r(h)])
        ctxa[u] = (Sb, st, ntot)

    def stage_a2(u):
        m, h = units[u]
        Sb, st, ntot = ctxa.pop(u)
        Pb = Pbr.next()
        P.act(I_act(Pb.ap[:, 0:ntot], Sb.ap[:, 0:ntot], AF.Exp, bias=st.ap[:, h, 0:1], accum_out=st.ap[:, h, 1:2]),
              reads=[Sb.r(), st.r(h)], writes=[Pb.r(), st.r(h)])
        ctxs[u] = Pb

    ctxt = {}

    def stage_t(u):
        m, h = units[u]
        kc0, nloc, egi = pair_info(m)
        Pb = ctxs.pop(u)
        chunks_ = []
        off = 0
        while off < nloc:
            cw = min(128, nloc - off)
            chunks_.append((off, cw, (kc0 + off) // 128))
            off += cw
        chunks_.append((nloc, 128, 20))
        chunks_.append((nloc + 128, 128, 21))
        ptb = PS.next("pt", [4, 5])
        ptap = ptb.ap.bitcast(BF16)
        for i_, (off, cw, vt) in enumerate(chunks_):
            P.pe(I_tr(ptap[0:cw, i_ * 128:(i_ + 1) * 128], Pb.ap[:, off:off + cw], identb.ap), reads=[Pb.r(), identb.r()], writes=[ptb.r()])
        PT = PTr.next()
        ncols_ = len(chunks_) * 128
        if h % 2 == 0:
            P.act(I_act(PT.ap[:, 0:ncols_], ptap[:, 0:ncols_], AF.Copy), reads=[ptb.r()], writes=[PT.r()])
        else:
            P.dve(I_copy(PT.ap[:, 0:ncols_], ptap[:, 0:ncols_]), reads=[ptb.r()], writes=[PT.r()])
        ctxt[u] = (PT, chunks_)

    def stage_pv(u):
        m, h = units[u]
        PT, chunks_ = ctxt.pop(u)
        pvb = PS.b[6 + (m % 2)]
        for i_, (off, cw, vt) in enumerate(chunks_):
            P.pe(I_mm(pvb.ap[:, h * 64:(h + 1) * 64], PT.ap[0:cw, i_ * 128:(i_ + 1) * 128], Vt.ap[0:cw, vt, h * 64:(h + 1) * 64], i_ == 0, i_ == len(chunks_) - 1),
                 reads=[PT.r(), Vt.r(vt)], writes=[pvb.r()])

    def stage_e(m):
        st = pair_st[m]
        pvb = PS.b[6 + (m % 2)]
        md = mdt.next()
        allst = [st.r(h) for h in range(8)]
        P.dve(I_recip(st.ap[:, :, 2], st.ap[:, :, 1]), reads=allst, writes=allst)
        for h in range(8):
            P.dve(I_ts(md.ap[:, h * 64:(h + 1) * 64], pvb.ap[:, h * 64:(h + 1) * 64], st.ap[:, h, 2:3], None, ALU.mult), reads=[pvb.r(), st.r(h)], writes=[md.r()])
        tb_ = PS.next("pt", [4, 5])
        tbap = tb_.ap.bitcast(BF16)
        for j in range(4):
            P.pe(I_tr(tbap[:, j * 128:(j + 1) * 128], md.ap[:, j * 128:(j + 1) * 128], identb.ap), reads=[md.r(), identb.r()], writes=[tb_.r()])
        P.act(I_act(QT.ap[:, :, 128 * m:128 * m + 128], tbap[:, 0:512].rearrange("p (j n) -> p j n", j=4), AF.Copy),
              reads=[tb_.r()], writes=[QT.r((j, m)) for j in range(4)])

    NU = len(units)
    stage_a(0)
    stage_a2(0)
    stage_a(1)
    stage_a2(1)
    stage_t(0)
    for u in range(NU):
        if u + 2 < NU:
            stage_a(u + 2)
        if u + 1 < NU:
            stage_t(u + 1)
        if u + 2 < NU:
            stage_a2(u + 2)
        stage_pv(u)
        if units[u][1] == 7:
            stage_e(units[u][0])
    for r_ in (Sbr, Pbr, PTr, EGr, mdt, stat):
        r_.free()
    AR.free(UG, KT, Vt)

    wo = AR.alloc("wo", [128, 8, D], BF16)
    P.dma("pool", I_dma(wo.ap, dr["w_out"][1].rearrange("(k p) n -> p k n", p=128)), writes=[wo.r()])
    OC1 = OC[:4]

    def rhs1(mk, ci):
        c0, c1 = OC1[ci]
        if mk < 4:
            return mC.ap[:, mk, c0 - OWN0:c1 - OWN0], [mC.r((mk, 256 + 512 * ci))]
        return QT.ap[:, mk - 4, c0 - OWN0:c1 - OWN0], [QT.r((mk - 4, 4 * ci + mm)) for mm in range(4)]

    def l1_outproj_then(norm_chunk):
        out_proj(1, wo, rhs1, [(c0, c1, 0) for (c0, c1) in OC1], chunk_hook=norm_chunk)
        AR.free(wo, mC, QT)

    finals = []
    emit_moe(1, [(c0, c1, 0) for (c0, c1) in OC1], pre_norm=l1_outproj_then)
    frs = AR.alloc("frs", [128, TOK], F32)
    fsq = Rot(AR, "fsq", [128, 8, 512], BF16, 2)
    fsd = Rot(AR, "fsd", [128, 512], F32, 2)
    for ci, (c0, c1) in enumerate(OC1):
        hr = []
        for k in range(8):
            hr += hres(k, c0, c1)
        q_ = fsq.next()
        d_ = fsd.next()
        P.act(I_act(q_.ap, hT.ap[:, :, c0:c1], AF.Square), reads=hr, writes=[q_.r()])
        bank = PS.next("nrm", [4, 5])
        for k in range(8):
            P.pe(I_mm(bank.ap, onesb.ap, q_.ap[:, k, :], k == 0, k == 7), reads=[q_.r(), onesb.r()], writes=[bank.r()])
        P.act(I_act(d_.ap, bank.ap, AF.Sqrt, bias=EPS, scale=1.0 / D), reads=[bank.r()], writes=[d_.r()])
        P.dve(I_recip(frs.ap[:, ci * 512:(ci + 1) * 512], d_.ap), reads=[d_.r()], writes=[frs.r(ci)])
    fsq.free()
    fsd.free()
    ofs = [AR.alloc(f"of{i}", [128, 8, 512], F32) for i in range(2)]
    ftm = Rot(AR, "ftm", [128, 512], F32, 3)
    yt = Rot(AR, "yt", [128, D], F32, 3)

    def f_pass2(ci):
        c0, c1 = OC1[ci]
        of = ofs[ci % 2]
        for k in range(8):
            tb = ftm.next()
            P.dve(I_tt(tb.ap, hT.ap[:, k, c0:c1], frs.ap[:, ci * 512:(ci + 1) * 512], ALU.mult), reads=hres(k, c0, c1) + [frs.r(ci)], writes=[tb.r()])
            P.act(I_act(of.ap[:, k, :], tb.ap, AF.Identity, scale=V("fing", k)), reads=[tb.r(), vec.r()], writes=[of.r(k)])

    def f_out(ci):
        c0, c1 = OC1[ci]
        of = ofs[ci % 2]
        for tt in range(4):
            y_ = yt.next()
            for g in range(2):
                bank = PS.next("xt", [0, 1, 2, 3])
                for kk in range(4):
                    k = g * 4 + kk
                    P.pe(I_tr(bank.ap[:, kk * 128:(kk + 1) * 128], of.ap[:, k, tt * 128:(tt + 1) * 128], ident.ap), reads=[of.r(k), ident.r()], writes=[bank.r()])
                if g == 0:
                    P.dve(I_copy(y_.ap[:, 0:512], bank.ap), reads=[bank.r()], writes=[y_.r()])
                else:
                    P.act(I_act(y_.ap[:, 512:1024], bank.ap, AF.Copy), reads=[bank.r()], writes=[y_.r()])
            r0 = (c0 - OWN0) + tt * 128
            finals.append(P.dma("sp", I_dma(yout[r0:r0 + 128, :], y_.ap), reads=[y_.r()], writes=[dres("y").r(r0)]))

    finals = []
    for ci in range(4):
        f_pass2(ci)
        if ci >= 1:
            f_out(ci - 1)
    f_out(3)
    P.emit({"sp": finals})


def _fm(v):
    v = np.asarray(v, np.float32).reshape(-1, 128)
    return np.ascontiguousarray(v.T)


def _bias_table(rpb, w0, nrows, m, qcore):
    qc = np.arange(64)
    kc = np.arange(64)
    col_start = np.clip(qc - 8, 0, 48)
    colvalid = (kc[None, :] >= col_start[:, None]) & (kc[None, :] < col_start[:, None] + 16)
    colidx = np.clip(kc[None, :] - qc[:, None] + 15, 0, 30)
    T = np.full((8, 2, 64, nrows, 64), -BIG, np.float32)
    for rho in range(2):
        R = 32 * qcore + 2 * m + rho
        r_start = min(max(R - 4, 0), 120)
        for wp in range(nrows):
            keyrow = 32 * qcore + (w0 + wp) - 4
            if keyrow < r_start or keyrow >= r_start + 8:
                continue
            ridx = keyrow - R + 7
            vals = rpb[:, ridx, :][:, colidx]
            T[:, rho, :, wp, :] = np.where(colvalid[None], vals, np.float32(-BIG))
    return np.ascontiguousarray(T.reshape(8, 128, nrows * 64))


def prep_inputs(inp):
    f32 = lambda a: np.ascontiguousarray(np.asarray(a, np.float32))
    x, c, ctx, c_ctx = f32(inp["x"]), f32(inp["c"]), f32(inp["ctx"]), f32(inp["c_ctx"])
    shared = {
        "ident": np.eye(128, dtype=np.float32),
        "ada_w": f32(inp["ada_w"]),
        "w_in0": f32(inp["ab_w_in"][0]),
        "w_in1": f32(inp["cd_w_in"][0]),
        "w_out": f32(inp["w_out"]),
        "gate_w": f32(inp["b_gate_w"]).reshape(16, 128, 128),
        "router_w": f32(inp["router_w"]),
        "rb18": np.ascontiguousarray(np.broadcast_to(np.tile(f32(inp["router_bias"]), 18)[None, :], (128, 288))),
        "selm": np.ascontiguousarray(np.repeat(np.eye(16, dtype=np.float32)[:, :, None], 128, axis=2).reshape(16, 16 * 128)),
        "wg": f32(inp["moe_w_gate"]),
        "wu": f32(inp["moe_w_up"]),
        "wd": f32(inp["moe_w_down"]),
    }
    common = {
        "ada_b0": _fm(inp["ada_b"][0]), "ada_b1": _fm(inp["ada_b"][1]),
        "nmg0": _fm(inp["norm_mix_g"][0]), "nmg1": _fm(inp["norm_mix_g"][1]),
        "nfg0": _fm(inp["norm_ffn_g"][0]), "nfg1": _fm(inp["norm_ffn_g"][1]),
        "fing": _fm(inp["final_g"]),
        "adw_w": np.ascontiguousarray(f32(inp["a_dw_w"][0]).reshape(31, 4, 128).transpose(2, 0, 1).reshape(128, 124)),
        "adw_b": _fm(inp["a_dw_b"][0]), "aln_g": _fm(inp["a_ln_g"][0]), "aln_b": _fm(inp["a_ln_b"][0]),
        "bcw": np.ascontiguousarray(f32(inp["b_conv_w"][0]).reshape(4, 4, 128).transpose(2, 0, 1).reshape(128, 16)),
        "bcb": _fm(inp["b_conv_b"][0]),
        "bgb": np.ascontiguousarray(f32(inp["b_gate_b"][0]).reshape(4, 4, 128).transpose(2, 0, 1).reshape(128, 16)),
        "blam": np.ascontiguousarray(f32(inp["b_lambda"][0]).reshape(2, 4, 128).transpose(2, 0, 1).reshape(128, 8)),
        "ccw": np.ascontiguousarray(f32(inp["c_conv_w"][0]).reshape(3, 4, 128).transpose(2, 0, 1).reshape(128, 12)),
    }
    rpb = f32(inp["d_rpb"][0])
    shared["ug"] = _bias_table(rpb, 8, 9, 4, 1)
    maps = []
    for core in range(NCORES):
        b, q = core // 4, core % 4
        xe = np.zeros((NL0, D), np.float32)
        lo, hi = q * TOK - HAL0, (q + 1) * TOK + HAL0
        slo, shi = max(lo, 0), min(hi, 8192)
        xe[slo - lo:shi - lo] = x[b, slo:shi]
        vecT = np.zeros((128, NV), np.float32)
        for name, n in VEC_LAYOUT:
            o = VOFF[name]
            if name == "cvec":
                vecT[:, o:o + 8] = _fm(c[b])
                vecT[:, o + 8:o + 16] = _fm(c_ctx)
            elif name == "flags":
                fl = np.zeros(8, np.float32)
                fl[0] = 1.0 if q > 0 else 0.0
                fl[1] = 1.0 if q < 3 else 0.0
                fl[2 + q] = 1.0
                vecT[:, o:o + 8] = fl[None, :]
            elif name == "flags2":
                fl = np.zeros(8, np.float32)
                if q > 0:
                    fl[q - 1] = 1.0
                if q < 3:
                    fl[4 + q + 1] = 1.0
                vecT[:, o:o + 8] = fl[None, :]
            else:
                vecT[:, o:o + n] = common[name]
        m = dict(shared)
        eg = np.full((4, 8, 128, 768), -BIG, np.float32)
        for i, (mm, w0, nr) in enumerate([(0, 0, 12), (1, 2, 10), (14, 28, 9), (15, 28, 11)]):
            eg[i, :, :, 0:nr * 64] = _bias_table(rpb, w0, nr, mm, q)
        m["eg"] = eg
        m["xe"] = xe
        m["ctxb"] = np.ascontiguousarray(ctx[b])
        m["vecT"] = vecT
        maps.append(m)
    return maps


def run_stage(stage, inp):
    nc = build(stage)
    maps = prep_inputs(inp)
    res = run_bass_kernel_spmd(nc, maps, core_ids=list(range(NCORES)))
    return res


def kernel(**inputs):
    res = run_stage("full", inputs)
    out = np.zeros((2, 8192, D), np.float32)
    for core in range(NCORES):
        b, q = core // 4, core % 4
        out[b, q * TOK:(q + 1) * TOK] = res.results[core]["y"]
    return out
```

```python
import contextlib
import numpy as np
import concourse.bass as bass
import concourse.mybir as mybir
from concourse.bass_utils import run_bass_kernel_spmd

F32 = mybir.dt.float32
BF16 = mybir.dt.bfloat16
AF = mybir.ActivationFunctionType
ALU = mybir.AluOpType
AX = mybir.AxisListType

NCORES = 8
D = 1024
KD = 8
TOK = 2048
CTX = 256
HAL0 = 16
NL0 = TOK + 2 * HAL0
NT0 = NL0 + CTX
OWN0 = HAL0
CTX0 = NL0
UW = NL0 + 15 + CTX + 15
UCTX = NL0 + 15
EPS = 1e-6
BIG = 30000.0
DEBUG_SERIAL = False


class Res:
    __slots__ = ("name", "w", "rc", "rd")

    def __init__(self, name, init=()):
        self.name = name
        self.w = None
        self.rc = {}
        self.rd = []
        for o in init:
            self.addr(o)

    def addr(self, o):
        if o.kind == "d":
            self.rd.append(o)
        else:
            p = self.rc.get(o.eng)
            if p is None or p.idx < o.idx:
                self.rc[o.eng] = o

    def readers(self):
        return list(self.rc.values()) + self.rd


class Buf:
    def __init__(self, name, ap, lo=0, hi=0, init=()):
        self.name = name
        self.ap = ap
        self.lo = lo
        self.hi = hi
        self.init = list(init)
        self.subs = {}

    def r(self, key=0):
        s = self.subs.get(key)
        if s is None:
            s = Res(f"{self.name}.{key}", self.init)
            self.subs[key] = s
        return s

    def last_ops(self):
        d = {}
        for s in self.subs.values():
            if s.w is not None:
                d[s.w.idx] = s.w
            for x in s.readers():
                d[x.idx] = x
        for x in self.init:
            d[x.idx] = x
        best = {}
        out = []
        for x in d.values():
            if x.kind == "d":
                out.append(x)
            else:
                p = best.get(x.eng)
                if p is None or p.idx < x.idx:
                    best[x.eng] = x
        return out + list(best.values())


class Op:
    __slots__ = ("eng", "fn", "deps", "kind", "need", "sigsem", "sigval", "idx", "name")


ENGS = ["pe", "dve", "act", "pool", "sp"]


class Prog:
    def __init__(self, nc):
        self.nc = nc
        self.q = {e: [] for e in ENGS}
        self.n = 0

    def add(self, eng, fn, reads=(), writes=(), kind="c", name=""):
        o = Op()
        o.eng = eng
        o.fn = fn
        o.kind = kind
        o.need = False
        o.sigsem = None
        o.sigval = 0
        o.idx = self.n
        o.name = name
        self.n += 1
        deps = {}
        for r in reads:
            if r.w is not None:
                deps[r.w.idx] = r.w
        for w in writes:
            if w.w is not None:
                deps[w.w.idx] = w.w
            for x in w.readers():
                deps[x.idx] = x
        best = {}
        o.deps = []
        for d in deps.values():
            if d is o:
                continue
            if d.kind == "d":
                d.need = True
                o.deps.append(d)
                continue
            if d.eng == "pe" and eng == "pe" and kind == "c":
                continue
            p = best.get(d.eng)
            if p is None or p.idx < d.idx:
                best[d.eng] = d
        for d in best.values():
            d.need = True
            o.deps.append(d)
        for r in reads:
            r.addr(o)
        for w in writes:
            w.w = o
            w.rc = {}
            w.rd = []
        self.q[eng].append(o)
        return o

    def pe(self, fn, reads=(), writes=(), **k):
        return self.add("pe", fn, reads, writes, **k)

    def dve(self, fn, reads=(), writes=(), **k):
        return self.add("dve", fn, reads, writes, **k)

    def act(self, fn, reads=(), writes=(), **k):
        return self.add("act", fn, reads, writes, **k)

    def pool(self, fn, reads=(), writes=(), **k):
        return self.add("pool", fn, reads, writes, **k)

    def dma(self, q, fn, reads=(), writes=(), **k):
        return self.add(q, fn, reads, writes, kind="d", **k)

    def emit(self, final_waits):
        nc = self.nc
        NDS = 8
        with contextlib.ExitStack() as es:
            csem = {e: es.enter_context(nc.semaphore(f"c_{e}")) for e in ENGS}
            dsem = {e: [es.enter_context(nc.semaphore(f"d_{e}{i}")) for i in range(NDS)] for e in ENGS}
            for e in ENGS:
                cc = 0
                dcnt = [0] * NDS
                di = 0
                for o in self.q[e]:
                    if o.kind == "d":
                        s = di % NDS
                        di += 1
                        dcnt[s] += 16
                        o.sigsem = ("d", e, s)
                        o.sigval = dcnt[s]
                    else:
                        if o.need or o.kind == "cc":
                            cc += 1
                            o.sigsem = ("c", e, 0)
                            o.sigval = cc
            block = es.enter_context(nc.Block())

            def semof(key):
                return csem[key[1]] if key[0] == "c" else dsem[key[1]][key[2]]

            def run(ename, eng):
                waited = {}
                for o in self.q[ename]:
                    dl = list(o.deps)
                    if o.kind == "d":
                        prev = o.sigval - 16
                        if prev > 0:
                            key = o.sigsem
                            if waited.get(key, 0) < prev:
                                eng.wait_ge(semof(key), prev)
                                waited[key] = prev
                    for d in dl:
                        key = d.sigsem
                        if waited.get(key, 0) < d.sigval:
                            eng.wait_ge(semof(key), d.sigval)
                            waited[key] = d.sigval
                    ins = o.fn(eng)
                    if o.sigsem is not None:
                        ins.then_inc(semof(o.sigsem), 16 if o.kind == "d" else 1)
                for o in final_waits.get(ename, []):
                    eng.wait_ge(semof(o.sigsem), o.sigval)
                last = {}
                for o in self.q[ename]:
                    if o.kind == "d":
                        last[o.sigsem] = o.sigval
                for key, val in last.items():
                    if waited.get(key, 0) < val:
                        eng.wait_ge(semof(key), val)

            @block.tensor
            def _(eng):
                run("pe", eng)

            @block.vector
            def _(eng):
                run("dve", eng)

            @block.scalar
            def _(eng):
                run("act", eng)

            @block.gpsimd
            def _(eng):
                run("pool", eng)

            @block.sync
            def _(eng):
                run("sp", eng)


class Arena:
    def __init__(self, nc, nbytes, name="arena"):
        self.nbytes = nbytes
        self.t = nc.alloc_sbuf_tensor(name, [128, nbytes // 4], F32)
        self.live = []
        self.dead = []

    def alloc(self, name, shape, dtype, at=None):
        esz = 4 if dtype == F32 else 2
        n = 1
        for s in shape[1:]:
            n *= s
        nb = (n * esz + 63) // 64 * 64
        if at is None:
            pts = sorted([(lo, hi) for lo, hi, _ in self.live])
            cur = 0
            at = None
            for lo, hi in pts:
                if lo - cur >= nb:
                    at = cur
                    break
                cur = max(cur, hi)
            if at is None:
                if self.nbytes - cur >= nb:
                    at = cur
                else:
                    raise RuntimeError(f"arena full allocating {name} {nb}B; live={[(b.name, lo, hi) for lo, hi, b in self.live]}")
        lo, hi = at, at + nb
        assert hi <= self.nbytes, (name, lo, hi)
        for l2, h2, b2 in self.live:
            assert not (l2 < hi and lo < h2), f"arena overlap {name} [{lo},{hi}) with {b2.name} [{l2},{h2})"
        init = []
        keep = []
        for dl, dh, db in self.dead:
            if dl < hi and lo < dh:
                init.extend(db.last_ops())
                keep.append((dl, dh, db))
            else:
                keep.append((dl, dh, db))
        self.dead = keep
        ap = self.t[:, lo // 4: hi // 4]
        if dtype != F32:
            ap = ap.bitcast(dtype)
        ap = ap[:, 0:n]
        if len(shape) == 3:
            ap = ap.rearrange("p (a b) -> p a b", a=shape[1])
        elif len(shape) == 4:
            ap = ap.rearrange("p (a b c) -> p a b c", a=shape[1], b=shape[2])
        if shape[0] != 128:
            ap = ap[0:shape[0]]
        b = Buf(name, ap, lo, hi, init)
        self.live.append((lo, hi, b))
        return b

    def free(self, *bufs):
        for b in bufs:
            for i, (lo, hi, x) in enumerate(self.live):
                if x is b:
                    self.live.pop(i)
                    self.dead.append((lo, hi, b))
                    break
            else:
                raise RuntimeError(f"free of non-live {b.name}")
        if len(self.dead) > 400:
            self.dead = self.dead[-400:]


def I_mm(out, lhsT, rhs, start=True, stop=True, skip=False):
    if skip:
        return lambda e: e.matmul(out, lhsT, rhs, start=start, stop=stop, skip_group_check=True)
    return lambda e: e.matmul(out, lhsT, rhs, start=start, stop=stop)


def I_tr(out, in_, ident):
    return lambda e: e.transpose(out, in_, ident)


def I_act(out, in_, func, bias=None, scale=None, accum_out=None):
    def f(e):
        kw = {}
        if bias is not None:
            kw["bias"] = bias
        if scale is not None:
            kw["scale"] = scale
        if accum_out is not None:
            kw["accum_out"] = accum_out
        return e.activation(out=out, in_=in_, func=func, **kw)
    return f


def I_tt(out, in0, in1, op):
    return lambda e: e.tensor_tensor(out=out, in0=in0, in1=in1, op=op)


def I_ts(out, in0, s1, s2, op0, op1=None):
    if op1 is None:
        return lambda e: e.tensor_scalar(out=out, in0=in0, scalar1=s1, scalar2=None, op0=op0)
    return lambda e: e.tensor_scalar(out=out, in0=in0, scalar1=s1, scalar2=s2, op0=op0, op1=op1)


def I_stt(out, in0, scalar, in1, op0, op1):
    return lambda e: e.scalar_tensor_tensor(out=out, in0=in0, scalar=scalar, in1=in1, op0=op0, op1=op1)


def I_copy(out, in_):
    return lambda e: e.tensor_copy(out=out, in_=in_)


def I_dma(out, in_):
    return lambda e: e.dma_start(out=out, in_=in_)


def I_scan(out, d0, d1, init, op0=ALU.mult, op1=ALU.add):
    return lambda e: e.tensor_tensor_scan(out=out, data0=d0, data1=d1, initial=init, op0=op0, op1=op1)


def I_recip(out, in_):
    return lambda e: e.reciprocal(out=out, in_=in_)


def I_red(out, in_, op, axis=AX.X):
    return lambda e: e.tensor_reduce(out=out, in_=in_, axis=axis, op=op)


def I_memset(ap, v):
    return lambda e: e.memset(ap, v)


class PSum:
    def __init__(self, nc):
        self.t = nc.alloc_psum_tensor("ps", [128, 8, 512], F32)
        self.b = [Buf(f"ps{i}", self.t[:, i, :]) for i in range(8)]
        self.rr = {}

    def next(self, group, banks):
        i = self.rr.get(group, 0)
        self.rr[group] = i + 1
        return self.b[banks[i % len(banks)]]


VEC_LAYOUT = [
    ("cvec", 16), ("ada_b0", 48), ("ada_b1", 48), ("nmg0", 8), ("nmg1", 8), ("nfg0", 8), ("nfg1", 8), ("fing", 8),
    ("adw_w", 124), ("adw_b", 4), ("aln_g", 4), ("aln_b", 4), ("bcw", 16), ("bcb", 4), ("bgb", 16), ("blam", 8),
    ("ccw", 12), ("flags", 8), ("flags2", 8),
]
VOFF = {}
_o = 0
for _n, _c in VEC_LAYOUT:
    VOFF[_n] = _o
    _o += _c
NV = _o

ARENA_BYTES = 121 * 1024


class Rot:
    def __init__(self, AR, name, shape, dtype, n=2):
        self.bufs = [AR.alloc(f"{name}{i}", shape, dtype) for i in range(n)]
        self.i = 0
        self.AR = AR

    def next(self):
        b = self.bufs[self.i % len(self.bufs)]
        self.i += 1
        return b

    def free(self):
        self.AR.free(*self.bufs)


class _Done(Exception):
    pass


def build(stage="full"):
    nc = bass.Bass("TRN2", target_bir_lowering=False)
    try:
        _build(nc, stage)
    except _Done:
        pass
    return nc


def _build(nc, stage):
    dr = {}

    def din(name, shape):
        dr[name] = nc.dram_tensor(name, list(shape), F32, kind="ExternalInput").ap()

    din("xe", [NL0, D])
    din("ctxb", [CTX, D])
    din("vecT", [128, NV])
    din("ident", [128, 128])
    din("ada_w", [2, D, 6 * D])
    din("w_in0", [D, 2048])
    din("w_in1", [D, 3072])
    din("w_out", [2, D, D])
    din("gate_w", [16, 128, 128])
    din("router_w", [D, 16])
    din("rb18", [128, 288])
    din("selm", [16, 16 * 128])
    din("wg", [2, 16, D, 512])
    din("wu", [2, 16, D, 512])
    din("wd", [2, 16, 512, D])
    din("ug", [8, 128, 576])
    din("eg", [4, 8, 128, 768])
    yout = nc.dram_tensor("y", [TOK, D], F32, kind="ExternalOutput").ap()
    dbg = None
    if stage != "full":
        dbg = nc.dram_tensor("dbg", [128, KD, NT0], F32, kind="ExternalOutput").ap()
    abD = nc.dram_tensor("abD", [8, 2, 128, TOK], F32, kind="Internal").ap()
    cc_src = nc.dram_tensor("cc_src", [128, 16], F32, kind="Internal").ap()
    cc_dst = nc.dram_tensor("cc_dst", [4 * 128, 16], F32, kind="Internal").ap()
    groups = [[0, 1, 2, 3], [4, 5, 6, 7]]

    P = Prog(nc)
    AR = Arena(nc, ARENA_BYTES)
    PS = PSum(nc)

    def sb(name, shape, dt=F32):
        return Buf(name, nc.alloc_sbuf_tensor("sb_" + name, list(shape), dt)[:])

    hT = sb("hT", [128, KD, NT0])
    ident = sb("identf", [128, 128])
    identb = sb("identb", [128, 128], BF16)
    onesb = sb("onesb", [128, 128], BF16)
    vec = sb("vec", [128, NV])
    modT = [sb(f"mod{l}", [128, 48, 2]) for l in range(2)]
    scl = sb("scl", [128, 2, 6, 2, 8])
    condT = sb("condT", [128, 8, 2], BF16)
    lruc = sb("lruc", [128, 2, 8])
    lrut = sb("lrut", [128, 8, 8])
    rw = sb("rw", [128, 8, 16])
    rb18 = sb("rb18", [128, 288])
    selm = sb("selm", [16, 16, 128], BF16)
    zer = sb("zer", [128, 288])
    Sm = sb("Sm", [128, 4, 2, 5, 2])
    RS = sb("RS", [128, 4, 2, 4])
    hend = sb("hend", [128, 2, 4])
    summ = sb("summ", [128, 16])
    gath = sb("gath", [128, 4, 16])
    Hin = sb("Hin", [128, 2, 4])
    tiny = sb("tiny", [128, 8, 4])

    DB = {}

    def dres(name):
        if name not in DB:
            DB[name] = Buf(name, None)
        return DB[name]

    def hres(k, c0, c1):
        out = []
        if c1 > CTX0:
            out.append(hT.r((k, "x")))
        if c0 < CTX0:
            for c in range(4):
                lo = 0 if c == 0 else OWN0 + 512 * c
                hi = NL0 if c == 3 else OWN0 + 512 * (c + 1)
                if c0 < hi and lo < min(c1, CTX0):
                    out.append(hT.r((k, c)))
        return out

    V = lambda name, i=0, n=1: vec.ap[:, VOFF[name] + i: VOFF[name] + i + n]

    def cut(name, dumps=None):
        if stage != name:
            return
        fin = []
        rd = []
        for k in range(8):
            rd += hres(k, 0, NT0)
        if dumps:
            for (dst, src, reads) in dumps:
                fin.append(P.dma("sp", I_dma(dst, src), reads=reads, writes=[dres("dbgx").r()]))
        else:
            fin.append(P.dma("sp", I_dma(dbg[:, :, :], hT.ap), reads=rd, writes=[dres("dbg").r()]))
        P.emit({"sp": fin})
        raise _Done()

    P.dma("sp", I_dma(ident.ap, dr["ident"][:, :]), writes=[ident.r()])
    P.dma("sp", I_dma(vec.ap, dr["vecT"][:, :]), writes=[vec.r()])
    P.dma("sp", I_dma(rw.ap, dr["router_w"].rearrange("(k p) n -> p k n", p=128)), writes=[rw.r()])
    P.dma("sp", I_dma(rb18.ap, dr["rb18"][:, :]), writes=[rb18.r()])
    P.dma("pool", I_dma(selm.ap, dr["selm"].rearrange("p (e n) -> p e n", e=16)), writes=[selm.r()])
    P.dve(I_copy(identb.ap, ident.ap), reads=[ident.r()], writes=[identb.r()])
    P.dve(I_memset(onesb.ap, 1.0), writes=[onesb.r()])
    P.dve(I_memset(zer.ap, 0.0), writes=[zer.r()])
    P.act(I_act(condT.ap.rearrange("p k t -> p t k"), V("cvec", 0, 16).rearrange("p (t k) -> p t k", t=2), AF.Silu),
          reads=[vec.r()], writes=[condT.r()])

    cut("c0")
    psmod = PS.b[7]

    def emit_mod(l, pieces):
        for pc in pieces:
            wb = AR.alloc("adaw", [128, 8, 512], BF16)
            P.dma("pool", I_dma(wb.ap, dr["ada_w"][l, :, pc * 512:(pc + 1) * 512].rearrange("(k p) n -> p k n", p=128)),
                  writes=[wb.r()])
            pr = psmod.r()
            base = 320 + (l * 48 + pc * 4) * 2
            for o4 in range(4):
                pap = psmod.ap[:, base + o4 * 2:base + o4 * 2 + 2]
                for k in range(8):
                    P.pe(I_mm(pap, wb.ap[:, k, o4 * 128:(o4 + 1) * 128], condT.ap[:, k, :], k == 0, k == 7),
                         reads=[wb.r(), condT.r()], writes=[pr])
            pp = psmod.ap[:, base:base + 8].rearrange("p (o t) -> p o t", t=2)
            for t in range(2):
                P.dve(I_tt(modT[l].ap[:, pc * 4:pc * 4 + 4, t], pp[:, :, t], V(f"ada_b{l}", pc * 4, 4), ALU.add),
                      reads=[pr, vec.r()], writes=[modT[l].r(pc * 4 + o) for o in range(4)])
            AR.free(wb)

    def emit_scl(l, kinds):
        for t in range(2):
            m = modT[l].ap
            if "n1" in kinds:
                P.dve(I_stt(scl.ap[:, l, 0, t, :], m[:, 8:16, t], 1.0, V(f"nmg{l}", 0, 8), ALU.add, ALU.mult),
                      reads=[modT[l].r(o) for o in range(8, 16)] + [vec.r()], writes=[scl.r((l, 0, t))])
                P.dve(I_copy(scl.ap[:, l, 1, t, :], m[:, 0:8, t]), reads=[modT[l].r(o) for o in range(0, 8)], writes=[scl.r((l, 1, t))])
            if "g1" in kinds:
                P.dve(I_copy(scl.ap[:, l, 2, t, :], m[:, 16:24, t]), reads=[modT[l].r(o) for o in range(16, 24)], writes=[scl.r((l, 2, t))])
            if "n2" in kinds:
                P.dve(I_stt(scl.ap[:, l, 3, t, :], m[:, 32:40, t], 1.0, V(f"nfg{l}", 0, 8), ALU.add, ALU.mult),
                      reads=[modT[l].r(o) for o in range(32, 40)] + [vec.r()], writes=[scl.r((l, 3, t))])
                P.dve(I_copy(scl.ap[:, l, 4, t, :], m[:, 24:32, t]), reads=[modT[l].r(o) for o in range(24, 32)], writes=[scl.r((l, 4, t))])
            if "g2" in kinds:
                P.dve(I_copy(scl.ap[:, l, 5, t, :], m[:, 40:48, t]), reads=[modT[l].r(o) for o in range(40, 48)], writes=[scl.r((l, 5, t))])

    cut("c0b")
    xs = Rot(AR, "xs", [128, D], F32, 4)
    tiles = [("xe", t * 128, min(128, NL0 - t * 128), t * 128) for t in range(17)] + \
            [("ctxb", t * 128, 128, CTX0 + t * 128) for t in range(2)]
    for ti, (src, r0, rows, col) in enumerate(tiles):
        b = xs.next()
        P.dma("sp", I_dma(b.ap[0:rows, :], dr[src][r0:r0 + rows, :]), writes=[b.r()])
        for g in range(2):
            bank = PS.next("xt", [4, 5, 6])
            for kk in range(4):
                k = g * 4 + kk
                P.pe(I_tr(bank.ap[:, kk * 128:kk * 128 + rows], b.ap[0:rows, k * 128:(k + 1) * 128], ident.ap[0:rows, 0:rows]),
                     reads=[b.r(), ident.r()], writes=[bank.r()])
            srcap = bank.ap.rearrange("p (a b) -> p a b", a=4)[:, :, 0:rows]
            dst = hT.ap[:, 4 * g:4 * g + 4, col:col + rows]
            wr = []
            for k in range(4 * g, 4 * g + 4):
                wr += hres(k, col, col + rows)
            if (ti + g) % 2 == 0:
                P.dve(I_copy(dst, srcap), reads=[bank.r()], writes=wr)
            else:
                P.act(I_act(dst, srcap, AF.Copy), reads=[bank.r()], writes=wr)
            if ti == 0 and g == 0:
                cut("x0")
            if ti == 0 and g == 1:
                cut("x1")
            if ti == 1 and g == 1:
                cut("x2")
    xs.free()
    emit_mod(0, range(0, 4))
    cut("s0")
    emit_scl(0, ["n1"])

    cut("c1")

    def hchunk(c0, c1, nT, n0, t, ci):
        return dict(w=c1 - c0, src3=hT.ap[:, :, c0:c1], srck=lambda k: hT.ap[:, k, c0:c1], rres=lambda k: hres(k, c0, c1),
                    dst=lambda k: nT.ap[:, k, n0:n0 + (c1 - c0)], dres=lambda k: nT.r((k, ci)), t=t)

    def emit_norm(l, kind, chunks, router=None, gvec=None, wmax=512, nsq=2, ci0=0):
        gsk, shk = (0, 1) if kind == 1 else (3, 4)
        ncols_all = sum(ch["w"] for ch in chunks)
        sq = Rot(AR, "nsq", [128, 8, wmax], BF16, nsq)
        sd = Rot(AR, "nsd", [128, wmax], F32, 2)
        rstd = AR.alloc("nrstd", [128, ncols_all], F32)
        offs = []
        o_ = 0
        for ci, ch in enumerate(chunks):
            w = ch["w"]
            offs.append(o_)
            hr = []
            for k in range(8):
                hr += ch["rres"](k)
            sq_ = sq.next()
            sd_ = sd.next()
            P.act(I_act(sq_.ap[:, :, 0:w], ch["src3"], AF.Square), reads=hr, writes=[sq_.r()])
            bank = PS.next("nrm", [4, 5])
            for k in range(8):
                P.pe(I_mm(bank.ap[:, 0:w], onesb.ap, sq_.ap[:, k, 0:w], k == 0, k == 7), reads=[sq_.r(), onesb.r()], writes=[bank.r()])
            P.act(I_act(sd_.ap[:, 0:w], bank.ap[:, 0:w], AF.Sqrt, bias=EPS, scale=1.0 / D), reads=[bank.r()], writes=[sd_.r()])
            P.dve(I_recip(rstd.ap[:, o_:o_ + w], sd_.ap[:, 0:w]), reads=[sd_.r()], writes=[rstd.r(ci)])
            o_ += w
        sq.free()
        sd.free()
        tmp = Rot(AR, "ntmp", [128, wmax], F32, 3)
        n2f = Rot(AR, "n2f", [128, wmax], F32, 3) if router is not None else None
        cnt = 0
        for ci, ch in enumerate(chunks):
            w = ch["w"]
            t = ch["t"]
            rs_ap = rstd.ap[:, offs[ci]:offs[ci] + w]
            for k in range(8):
                tb = tmp.next()
                P.dve(I_tt(tb.ap[:, 0:w], ch["srck"](k), rs_ap, ALU.mult), reads=ch["rres"](k) + [rstd.r(ci)], writes=[tb.r()])
                if kind == 3:
                    P.act(I_act(ch["dst"](k), tb.ap[:, 0:w], AF.Identity, scale=gvec(k)), reads=[tb.r(), vec.r()], writes=[ch["dres"](k)])
                    continue
                gs = scl.ap[:, l, gsk, t, k:k + 1]
                sh = scl.ap[:, l, shk, t, k:k + 1]
                sr = [scl.r((l, gsk, t)), scl.r((l, shk, t))]
                if router is None:
                    P.act(I_act(ch["dst"](k), tb.ap[:, 0:w], AF.Identity, bias=sh, scale=gs), reads=[tb.r()] + sr, writes=[ch["dres"](k)])
                else:
                    fb = n2f.next()
                    P.act(I_act(fb.ap[:, 0:w], tb.ap[:, 0:w], AF.Identity, bias=sh, scale=gs), reads=[tb.r()] + sr, writes=[fb.r()])
                    cnt += 1
                    if cnt % 3 == 0:
                        P.act(I_act(ch["dst"](k), fb.ap[:, 0:w], AF.Copy), reads=[fb.r()], writes=[ch["dres"](k)])
                    elif cnt % 3 == 1:
                        P.dve(I_copy(ch["dst"](k), fb.ap[:, 0:w]), reads=[fb.r()], writes=[ch["dres"](k)])
                    else:
                        P.pool(I_copy(ch["dst"](k), fb.ap[:, 0:w]), reads=[fb.r()], writes=[ch["dres"](k)])
                    tile0 = router["tile0"][ci0 + ci]
                    for tt in range(w // 128):
                        P.pe(I_mm(router["ps"].ap[:, (tile0 + tt) * 16:(tile0 + tt) * 16 + 16], fb.ap[:, tt * 128:(tt + 1) * 128], rw.ap[:, k, :], False, k == 7, skip=True),
                             reads=[fb.r(), rw.r()], writes=[router["ps"].r()])
        AR.free(rstd)
        tmp.free()
        if n2f is not None:
            n2f.free()

    CH0 = [(i * 416, (i + 1) * 416) for i in range(5)] + [(CTX0, NT0)]
    OC = [(OWN0 + 512 * c, OWN0 + 512 * (c + 1)) for c in range(4)] + [(CTX0, NT0)]

    def ucol(c0):
        return c0 if c0 < CTX0 else UCTX

    def mcol(c0):
        return c0 - OWN0 if c0 < CTX0 else TOK

    nT = AR.alloc("nT", [128, 8, NT0], BF16)
    emit_norm(0, 1, [hchunk(c0, c1, nT, c0, 0 if c0 < CTX0 else 1, ci) for ci, (c0, c1) in enumerate(CH0)])

    TOP = ARENA_BYTES
    gB = AR.alloc("gB", [128, 4, NT0], BF16, at=TOP - 18688)
    uA = AR.alloc("uA", [128, 4, UW], BF16, at=TOP - 18688 - 2048 - 18944)
    uB = AR.alloc("uB", [128, 4, UW], BF16, at=TOP - 18688 - 2048 - 2 * 18944)
    for ub in (uA, uB):
        P.pool(I_memset(ub.ap[:, :, NL0:NL0 + 15], 0.0), writes=[ub.r((j, 5)) for j in range(4)])
        P.pool(I_memset(ub.ap[:, :, UCTX + CTX:UW], 0.0), writes=[ub.r((j, 5)) for j in range(4)])
    w0 = dr["w_in0"]

    def load_w(name, segs, src):
        ncols = sum(n for _, n in segs)
        wb = AR.alloc(name, [128, 8, ncols], BF16)
        o = 0
        for si, (c0, n) in enumerate(segs):
            P.dma("pool", I_dma(wb.ap[:, :, o:o + n], src[:, c0:c0 + n].rearrange("(k p) n -> p k n", p=128)), writes=[wb.r(si)])
            o += n
        return wb

    tA = Rot(AR, "tA", [128, 416], F32, 3)

    def proj(wb, seg, wc0, ci, c0, c1):
        bank = PS.next("proj", [0, 1, 2, 3])
        w = c1 - c0
        for k in range(8):
            P.pe(I_mm(bank.ap[:, 0:w], wb.ap[:, k, wc0:wc0 + 128], nT.ap[:, k, c0:c1], k == 0, k == 7),
                 reads=[wb.r(seg), nT.r((k, ci))], writes=[bank.r()])
        return bank

    for p in range(2):
        wb = load_w("wA", [(2 * p * 128, 256), (512 + 2 * p * 128, 256)], w0)
        for ci, (c0, c1) in enumerate(CH0):
            w = c1 - c0
            for jj in range(2):
                j = 2 * p + jj
                pv = proj(wb, 0, jj * 128, ci, c0, c1)
                pg = proj(wb, 1, 256 + jj * 128, ci, c0, c1)
                sg = tA.next()
                P.act(I_act(sg.ap[:, 0:w], pg.ap[:, 0:w], AF.Sigmoid), reads=[pg.r()], writes=[sg.r()])
                P.dve(I_tt(uA.ap[:, j, ucol(c0):ucol(c0) + w], pv.ap[:, 0:w], sg.ap[:, 0:w], ALU.mult),
                      reads=[pv.r(), sg.r()], writes=[uA.r((j, ci))])
        AR.free(wb)
    wb = load_w("wBu", [(1024, 512)], w0)
    for j in range(4):
        for ci, (c0, c1) in enumerate(CH0):
            w = c1 - c0
            pu = proj(wb, 0, j * 128, ci, c0, c1)
            P.act(I_act(uB.ap[:, j, ucol(c0):ucol(c0) + w], pu.ap[:, 0:w], AF.Copy), reads=[pu.r()], writes=[uB.r((j, ci))])
    AR.free(wb)
    for ub in (uA, uB):
        for j in range(4):
            P.dve(I_ts(ub.ap[:, j, 0:HAL0], ub.ap[:, j, 0:HAL0], V("flags", 0), None, ALU.mult), reads=[ub.r((j, 0)), vec.r()], writes=[ub.r((j, 0))])
            P.dve(I_ts(ub.ap[:, j, NL0 - HAL0:NL0], ub.ap[:, j, NL0 - HAL0:NL0], V("flags", 1), None, ALU.mult), reads=[ub.r((j, 4)), vec.r()], writes=[ub.r((j, 4))])
    wb = load_w("wBg", [(1536, 512)], w0)
    for j in range(4):
        for ci, (c0, c1) in enumerate(CH0):
            w = c1 - c0
            pg = proj(wb, 0, j * 128, ci, c0, c1)
            x2 = tA.next()
            P.act(I_act(x2.ap[:, 0:w], pg.ap[:, 0:w], AF.Square), reads=[pg.r()], writes=[x2.r()])
            P.dve(I_ts(x2.ap[:, 0:w], x2.ap[:, 0:w], 0.044715, 1.0, ALU.mult, ALU.add), reads=[x2.r()], writes=[x2.r()])
            t2 = tA.next()
            P.dve(I_tt(t2.ap[:, 0:w], pg.ap[:, 0:w], x2.ap[:, 0:w], ALU.mult), reads=[pg.r(), x2.r()], writes=[t2.r()])
            P.act(I_act(t2.ap[:, 0:w], t2.ap[:, 0:w], AF.Sigmoid, scale=1.5957691216), reads=[t2.r()], writes=[t2.r()])
            P.dve(I_tt(gB.ap[:, j, c0:c1], pg.ap[:, 0:w], t2.ap[:, 0:w], ALU.mult), reads=[pg.r(), t2.r()], writes=[gB.r((j, ci))])
    AR.free(wb)
    tA.free()
    AR.free(nT)

    def allres(buf, j, n=6):
        return [buf.r((j, ci)) for ci in range(n)]

    emit_mod(0, range(4, 6))
    emit_scl(0, ["g1"])
    diagB = AR.alloc("diagB", [128, 16, 128], BF16)
    for k in range(4):
        for j in range(4):
            P.dve(I_ts(diagB.ap[:, k * 4 + j, :], identb.ap, V("bcw", k * 4 + j), None, ALU.mult),
                  reads=[identb.r(), vec.r()], writes=[diagB.r(k * 4 + j)])
    gw = AR.alloc("gw", [128, 16, 128], BF16)
    P.dma("pool", I_dma(gw.ap, dr["gate_w"].rearrange("g k j -> k g j")), writes=[gw.r()])
    L = lambda i: lrut.ap[:, i, :]
    lr = lrut.r()
    P.dve(I_ts(L(0), V("blam", 0, 8), -1.0, None, ALU.mult), reads=[vec.r()], writes=[lr])
    P.dve(I_tt(L(1), L(0), V("blam", 0, 8), ALU.max), reads=[lr, vec.r()], writes=[lr])
    P.act(I_act(L(2), L(1), AF.Exp, scale=-1.0), reads=[lr], writes=[lr])
    P.dve(I_ts(L(3), L(2), 2.0, None, ALU.add), reads=[lr], writes=[lr])
    P.dve(I_recip(L(3), L(3)), reads=[lr], writes=[lr])
    P.dve(I_tt(L(3), L(3), L(2), ALU.mult), reads=[lr], writes=[lr])
    P.dve(I_tt(L(4), L(3), L(3), ALU.mult), reads=[lr], writes=[lr])
    P.dve(I_ts(L(5), L(4), 1.0 / 13, 1.0 / 11, ALU.mult, ALU.add), reads=[lr], writes=[lr])
    for cf in (1.0 / 9, 1.0 / 7, 1.0 / 5, 1.0 / 3, 1.0):
        P.dve(I_tt(L(5), L(5), L(4), ALU.mult), reads=[lr], writes=[lr])
        P.dve(I_ts(L(5), L(5), cf, None, ALU.add), reads=[lr], writes=[lr])
    P.dve(I_tt(L(5), L(5), L(3), ALU.mult), reads=[lr], writes=[lr])
    P.dve(I_ts(L(6), L(0), 0.0, None, ALU.max), reads=[lr], writes=[lr])
    P.dve(I_stt(L(6), L(5), 2.0, L(6), ALU.mult, ALU.add), reads=[lr], writes=[lr])
    P.dve(I_ts(lruc.ap[:, 0, :], L(6), -8.0, None, ALU.mult), reads=[lr], writes=[lruc.r()])
    P.dve(I_ts(lruc.ap[:, 1, :], L(6), -16.0, None, ALU.mult), reads=[lr], writes=[lruc.r()])

    mBc = AR.alloc("mBc", [128, 4, CTX], BF16, at=TOP - 18688 - 2048)
    hbg = sb("hbg", [128, 16])
    lrc2 = sb("lrc2", [128, 2, 8])
    P.dve(I_ts(hbg.ap, V("bgb", 0, 16), 0.5, None, ALU.mult), reads=[vec.r()], writes=[hbg.r()])
    P.dve(I_ts(lrc2.ap[:, 0, :], lruc.ap[:, 0, :], 0.5, None, ALU.mult), reads=[lruc.r()], writes=[lrc2.r()])
    P.dve(I_ts(lrc2.ap[:, 1, :], lruc.ap[:, 0, :], 256.0, None, ALU.mult), reads=[lruc.r()], writes=[lrc2.r()])
    vFs = [AR.alloc(f"vF{i}", [128, 512], F32) for i in range(5)]
    vbs = [AR.alloc(f"vb{i}", [128, 512], BF16) for i in range(5)]
    aR = Rot(AR, "lra", [128, 512], F32, 5)
    sR = Rot(AR, "lrs", [128, 512], F32, 5)
    ivR = Rot(AR, "lriv", [128, 512], F32, 5)
    hlR = Rot(AR, "lrhl", [128, 512], F32, 2)
    hcf = AR.alloc("hcf", [128, CTX], F32)
    hcr = AR.alloc("hcr", [128, CTX], F32)
    for j in range(4):
        for ci, (c0, c1) in enumerate(OC):
            w = c1 - c0
            bank = PS.next("cv", [0, 1])
            for k in range(4):
                o = ucol(c0) + k - 2
                P.pe(I_mm(bank.ap[:, 0:w], diagB.ap[:, k * 4 + j, :], uB.ap[:, j, o:o + w], k == 0, k == 3),
                     reads=[diagB.r(k * 4 + j)] + allres(uB, j), writes=[bank.r()])
            P.act(I_act(vFs[ci].ap[:, 0:w], bank.ap[:, 0:w], AF.Identity, bias=V("bcb", j)), reads=[bank.r(), vec.r()], writes=[vFs[ci].r()])
            P.pool(I_copy(vbs[ci].ap[:, 0:w], vFs[ci].ap[:, 0:w]), reads=[vFs[ci].r()], writes=[vbs[ci].r()])
        for d in range(2):
            c1h = lrc2.ap[:, 0, d * 4 + j:d * 4 + j + 1]
            c1f = lruc.ap[:, 0, d * 4 + j:d * 4 + j + 1]
            bufs = []
            for ci, (c0, c1) in enumerate(OC):
                w = c1 - c0
                W = slice(0, w)
                pr = PS.next("gt", [2, 3, 4, 5])
                pi = PS.next("gt", [2, 3, 4, 5])
                P.pe(I_mm(pr.ap[:, W], gw.ap[:, (d * 2 + 0) * 4 + j, :], vbs[ci].ap[:, W]), reads=[gw.r(), vbs[ci].r()], writes=[pr.r()])
                P.pe(I_mm(pi.ap[:, W], gw.ap[:, (d * 2 + 1) * 4 + j, :], vbs[ci].ap[:, W]), reads=[gw.r(), vbs[ci].r()], writes=[pi.r()])
                a_, s_, iv = aR.next(), sR.next(), ivR.next()
                bufs.append((a_, s_, iv))
                gi = (d * 2 + 0) * 4 + j
                if ci < 4:
                    P.act(I_act(a_.ap[:, W], pr.ap[:, W], AF.Tanh, bias=hbg.ap[:, gi:gi + 1], scale=0.5, accum_out=RS.ap[:, j, d, ci:ci + 1]),
                          reads=[pr.r(), hbg.r()], writes=[a_.r(), RS.r((j, d))])
                else:
                    P.act(I_act(a_.ap[:, W], pr.ap[:, W], AF.Tanh, bias=hbg.ap[:, gi:gi + 1], scale=0.5), reads=[pr.r(), hbg.r()], writes=[a_.r()])
                gi = (d * 2 + 1) * 4 + j
                P.act(I_act(iv.ap[:, W], pi.ap[:, W], AF.Tanh, bias=hbg.ap[:, gi:gi + 1], scale=0.5), reads=[pi.r(), hbg.r()], writes=[iv.r()])
                P.act(I_act(a_.ap[:, W], a_.ap[:, W], AF.Exp, bias=c1h, scale=c1h), reads=[a_.r(), lrc2.r()], writes=[a_.r()])
                P.pool(I_tt(s_.ap[:, W], a_.ap[:, W], a_.ap[:, W], ALU.mult), reads=[a_.r()], writes=[s_.r()])
                P.dve(I_stt(iv.ap[:, W], iv.ap[:, W], 1.0, vFs[ci].ap[:, W], ALU.add, ALU.mult), reads=[iv.r(), vFs[ci].r()], writes=[iv.r()])
            for ci, (c0, c1) in enumerate(OC):
                w = c1 - c0
                W = slice(0, w)
                a_, s_, iv = bufs[ci]
                P.act(I_act(s_.ap[:, W], s_.ap[:, W], AF.Sqrt, bias=0.25, scale=-0.25), reads=[s_.r()], writes=[s_.r()])
                b_ = iv
                P.dve(I_tt(b_.ap[:, W], iv.ap[:, W], s_.ap[:, W], ALU.mult), reads=[iv.r(), s_.r()], writes=[iv.r()])
                if ci < 4:
                    hl = hlR.next()
                    if d == 0:
                        P.dve(I_scan(hl.ap[:, W], a_.ap[:, W], b_.ap[:, W], 0.0), reads=[a_.r(), b_.r()], writes=[hl.r()])
                        end = hl.ap[:, w - 1:w]
                    else:
                        P.dve(I_scan(hl.ap[:, W][:, ::-1], a_.ap[:, W][:, ::-1], b_.ap[:, W][:, ::-1], 0.0), reads=[a_.r(), b_.r()], writes=[hl.r()])
                        end = hl.ap[:, 0:1]
                    P.dve(I_copy(Sm.ap[:, j, d, ci, 1:2], end), reads=[hl.r()], writes=[Sm.r((j, d, "h"))])
                    P.dma("sp", I_dma(abD[d * 4 + j, 0, :, ci * 512:(ci + 1) * 512], a_.ap[:, W]), reads=[a_.r()], writes=[dres(f"ab{j}{d}").r(("a", ci))])
                    P.dma("sp", I_dma(abD[d * 4 + j, 1, :, ci * 512:(ci + 1) * 512], b_.ap[:, W]), reads=[b_.r()], writes=[dres(f"ab{j}{d}").r(("b", ci))])
                else:
                    hc = hcf if d == 0 else hcr
                    if d == 0:
                        P.dve(I_scan(hc.ap, a_.ap[:, W], b_.ap[:, W], 0.0), reads=[a_.r(), b_.r()], writes=[hc.r()])
                        P.dve(I_copy(hend.ap[:, 0, j:j + 1], hc.ap[:, CTX - 1:CTX]), reads=[hc.r()], writes=[hend.r()])
                    else:
                        P.dve(I_scan(hc.ap[:, ::-1], a_.ap[:, W][:, ::-1], b_.ap[:, W][:, ::-1], 0.0), reads=[a_.r(), b_.r()], writes=[hc.r()])
                        P.dve(I_copy(hend.ap[:, 1, j:j + 1], hc.ap[:, 0:1]), reads=[hc.r()], writes=[hend.r()])
            P.act(I_act(Sm.ap[:, j, d, 0:4, 0], RS.ap[:, j, d, :], AF.Exp, bias=lrc2.ap[:, 1, d * 4 + j:d * 4 + j + 1], scale=c1h),
                  reads=[RS.r((j, d)), lrc2.r()], writes=[Sm.r((j, d, "p"))])
        P.dve(I_tt(hcf.ap, hcf.ap, hcr.ap, ALU.add), reads=[hcf.r(), hcr.r()], writes=[hcf.r()])
        P.dve(I_tt(mBc.ap[:, j, :], hcf.ap, gB.ap[:, j, CTX0:NT0], ALU.mult), reads=[hcf.r()] + allres(gB, j), writes=[mBc.r(j)])
    aR.free()
    sR.free()
    ivR.free()
    hlR.free()
    AR.free(*vFs)
    AR.free(*vbs)
    AR.free(hcf, hcr, diagB, gw, uB)

    smr = [Sm.r((j, d, x)) for j in range(4) for d in range(2) for x in ("p", "h")]
    tr_ = tiny.r()
    TV = lambda i: tiny.ap[:, i, :]
    for d in range(2):
        order = [0, 1, 2, 3] if d == 0 else [3, 2, 1, 0]
        Pc = lambda c: Sm.ap[:, :, d, c, 0]
        Hc = lambda c: Sm.ap[:, :, d, c, 1]
        Hs = summ.ap[:, d * 8 + 4:d * 8 + 8]
        Ps = summ.ap[:, d * 8:d * 8 + 4]
        P.dve(I_copy(Hs, Hc(order[0])), reads=smr, writes=[summ.r()])
        P.dve(I_copy(Ps, Pc(order[0])), reads=smr, writes=[summ.r()])
        for c in order[1:]:
            P.dve(I_tt(Hs, Hs, Pc(c), ALU.mult), reads=smr + [summ.r()], writes=[summ.r()])
            P.dve(I_tt(Hs, Hs, Hc(c), ALU.add), reads=smr + [summ.r()], writes=[summ.r()])
            P.dve(I_tt(Ps, Ps, Pc(c), ALU.mult), reads=smr + [summ.r()], writes=[summ.r()])
    P.dma("sp", I_dma(cc_src[:, :], summ.ap), reads=[summ.r()], writes=[dres("ccs").r()])
    P.add("pool", lambda e: e.collective_compute("AllGather", ALU.bypass, replica_groups=groups, ins=[cc_src[:, :]], outs=[cc_dst[:, :]]),
          reads=[dres("ccs").r()], writes=[dres("ccd").r()], kind="cc")
    P.dma("sp", I_dma(gath.ap, cc_dst.rearrange("(r p) n -> p r n", p=128)), reads=[dres("ccd").r()], writes=[gath.r()])
    diagA = AR.alloc("diagA", [128, 124, 128], BF16)
    for k in range(31):
        for j in range(4):
            P.dve(I_ts(diagA.ap[:, k * 4 + j, :], identb.ap, V("adw_w", k * 4 + j), None, ALU.mult),
                  reads=[identb.r(), vec.r()], writes=[diagA.r(k * 4 + j)])
    mA = AR.alloc("mA", [128, 4, TOK + CTX], BF16, at=TOP - 18688 - 2048 - 18944 - 18432)
    cf = AR.alloc("cf", [128, 4, 512], F32)
    cb = AR.alloc("cb", [128, 4, 512], BF16)
    csq = AR.alloc("csq", [128, 4, 512], BF16)
    mean = AR.alloc("mean", [128, 512], F32)
    m2 = AR.alloc("m2", [128, 512], F32)
    rstdA = AR.alloc("rstdA", [128, 512], F32)
    tln = Rot(AR, "tln", [128, 512], F32, 1)
    for ci, (c0, c1) in enumerate(OC):
        w = c1 - c0
        W = slice(0, w)
        for j in range(4):
            bank = PS.b[j]
            for k in range(31):
                o = ucol(c0) + k - 15
                P.pe(I_mm(bank.ap[:, W], diagA.ap[:, k * 4 + j, :], uA.ap[:, j, o:o + w], k == 0, k == 30),
                     reads=[diagA.r(k * 4 + j)] + allres(uA, j), writes=[bank.r()])
            P.act(I_act(cf.ap[:, j, W], bank.ap[:, W], AF.Identity, bias=V("adw_b", j)), reads=[bank.r(), vec.r()], writes=[cf.r(j)])
            P.dve(I_copy(cb.ap[:, j, W], cf.ap[:, j, W]), reads=[cf.r(j)], writes=[cb.r(j)])
            P.act(I_act(csq.ap[:, j, W], cf.ap[:, j, W], AF.Square), reads=[cf.r(j)], writes=[csq.r(j)])
        bs, bq = PS.b[4], PS.b[5]
        for j in range(4):
            P.pe(I_mm(bs.ap[:, W], onesb.ap, cb.ap[:, j, W], j == 0, j == 3), reads=[cb.r(j), onesb.r()], writes=[bs.r()])
        for j in range(4):
            P.pe(I_mm(bq.ap[:, W], onesb.ap, csq.ap[:, j, W], j == 0, j == 3), reads=[csq.r(j), onesb.r()], writes=[bq.r()])
        P.act(I_act(mean.ap[:, W], bs.ap[:, W], AF.Copy, scale=1.0 / 512), reads=[bs.r()], writes=[mean.r()])
        P.dve(I_tt(m2.ap[:, W], mean.ap[:, W], mean.ap[:, W], ALU.mult), reads=[mean.r()], writes=[m2.r()])
        P.dve(I_stt(m2.ap[:, W], bq.ap[:, W], 1.0 / 512, m2.ap[:, W], ALU.mult, ALU.subtract), reads=[bq.r(), m2.r()], writes=[m2.r()])
        P.dve(I_ts(m2.ap[:, W], m2.ap[:, W], 0.0, None, ALU.max), reads=[m2.r()], writes=[m2.r()])
        P.act(I_act(m2.ap[:, W], m2.ap[:, W], AF.Sqrt, bias=EPS), reads=[m2.r()], writes=[m2.r()])
        P.dve(I_recip(rstdA.ap[:, W], m2.ap[:, W]), reads=[m2.r()], writes=[rstdA.r()])
        for j in range(4):
            t = tln.next()
            P.dve(I_tt(t.ap[:, W], cf.ap[:, j, W], mean.ap[:, W], ALU.subtract), reads=[cf.r(j), mean.r()], writes=[t.r()])
            P.dve(I_tt(t.ap[:, W], t.ap[:, W], rstdA.ap[:, W], ALU.mult), reads=[t.r(), rstdA.r()], writes=[t.r()])
            P.act(I_act(mA.ap[:, j, mcol(c0):mcol(c0) + w], t.ap[:, W], AF.Silu, bias=V("aln_b", j), scale=V("aln_g", j)),
                  reads=[t.r(), vec.r()], writes=[mA.r((j, ci))])
        if ci < 4:
            emit_mod(0, [6 + ci])
    emit_scl(0, ["n2"])
    tln.free()
    AR.free(diagA, cf, cb, csq, mean, m2, rstdA, uA)

    hr_ = Hin.r()
    for d in range(2):
        qs = [0, 1, 2, 3] if d == 0 else [3, 2, 1, 0]
        Hc_ = TV(d)
        Hi = Hin.ap[:, d, :]
        Pq = lambda q: gath.ap[:, q, d * 8:d * 8 + 4]
        Hq = lambda q: gath.ap[:, q, d * 8 + 4:d * 8 + 8]
        P.dve(I_copy(Hc_, hend.ap[:, d, :]), reads=[hend.r()], writes=[tr_])
        P.dve(I_ts(Hi, Hc_, V("flags", 2 + qs[0]), None, ALU.mult), reads=[tr_, vec.r()], writes=[hr_])
        for n in range(3):
            q = qs[n]
            P.dve(I_tt(Hc_, Hc_, Pq(q), ALU.mult), reads=[tr_, gath.r()], writes=[tr_])
            P.dve(I_tt(Hc_, Hc_, Hq(q), ALU.add), reads=[tr_, gath.r()], writes=[tr_])
            P.dve(I_stt(Hi, Hc_, V("flags", 2 + qs[n + 1]), Hi, ALU.mult, ALU.add), reads=[tr_, hr_, vec.r()], writes=[hr_])

    wo = AR.alloc("wo", [128, 8, D], BF16, at=TOP - 18688 - 2048 - 18944)
    P.dma("pool", I_dma(wo.ap, dr["w_out"][0].rearrange("(k p) n -> p k n", p=128)), writes=[wo.r()])
    mB = AR.alloc("mB", [128, 4, TOK], BF16)
    lar = Rot(AR, "la", [128, TOK], F32, 2)
    lbr = Rot(AR, "lb", [128, TOK], F32, 2)
    yb = AR.alloc("yb", [128, TOK], F32)
    hs = AR.alloc("hs", [128, TOK], F32)
    for j in range(4):
        for d in range(2):
            la = lar.next()
            lb = lbr.next()
            abr = dres(f"ab{j}{d}")
            P.dma("sp", I_dma(la.ap, abD[d * 4 + j, 0, :, :]), reads=[abr.r(("a", c)) for c in range(4)], writes=[la.r()])
            P.dma("sp", I_dma(lb.ap, abD[d * 4 + j, 1, :, :]), reads=[abr.r(("b", c)) for c in range(4)], writes=[lb.r()])
            init = Hin.ap[:, d, j:j + 1]
            if d == 0:
                P.dve(I_scan(yb.ap, la.ap, lb.ap, init), reads=[la.r(), lb.r(), hr_], writes=[yb.r()])
            else:
                P.dve(I_scan(hs.ap[:, ::-1], la.ap[:, ::-1], lb.ap[:, ::-1], init), reads=[la.r(), lb.r(), hr_], writes=[hs.r()])
        P.pool(I_tt(yb.ap, yb.ap, hs.ap, ALU.add), reads=[yb.r(), hs.r()], writes=[yb.r()])
        P.dve(I_tt(mB.ap[:, j, :], yb.ap, gB.ap[:, j, OWN0:OWN0 + TOK], ALU.mult), reads=[yb.r()] + allres(gB, j), writes=[mB.r(j)])
    lar.free()
    lbr.free()
    AR.free(yb, hs, gB)

    def out_proj(l, wo, rhs_of, chunks, chunk_hook=None):
        for ci, (c0, c1, t) in enumerate(chunks):
            if chunk_hook is not None and ci >= 1:
                chunk_hook(ci - 1)
            for dk in range(8):
                w = c1 - c0
                bank = PS.next("op", [0, 1, 2, 3])
                for mk in range(8):
                    rap, rres = rhs_of(mk, ci)
                    P.pe(I_mm(bank.ap[:, 0:w], wo.ap[:, mk, dk * 128:(dk + 1) * 128], rap, mk == 0, mk == 7), reads=[wo.r()] + rres, writes=[bank.r()])
                P.dve(I_stt(hT.ap[:, dk, c0:c1], bank.ap[:, 0:w], scl.ap[:, l, 2, t, dk:dk + 1], hT.ap[:, dk, c0:c1], ALU.mult, ALU.add),
                      reads=[bank.r(), scl.r((l, 2, t))] + hres(dk, c0, c1), writes=hres(dk, c0, c1))
        if chunk_hook is not None:
            chunk_hook(len(chunks) - 1)

    def rhs0(mk, ci):
        c0, c1 = OC[ci]
        w = c1 - c0
        if mk < 4:
            return mA.ap[:, mk, mcol(c0):mcol(c0) + w], [mA.r((mk, ci))]
        if ci < 4:
            return mB.ap[:, mk - 4, mcol(c0):mcol(c0) + w], [mB.r(mk - 4)]
        return mBc.ap[:, mk - 4, :], [mBc.r(mk - 4)]

    def l0_outproj_then(norm_chunk):
        out_proj(0, wo, rhs0, [(c0, c1, 0 if c0 < CTX0 else 1) for (c0, c1) in OC], chunk_hook=norm_chunk)
        AR.free(wo, mA, mB, mBc)

    def emit_moe(l, chunks, mod_hook=None, expert_hook=None, chunk_done_hook=None, pre_norm=None):
        ncol = sum(c1 - c0 for c0, c1, _ in chunks)
        ntile = ncol // 128
        n2T = AR.alloc("n2T", [128, 8, ncol], BF16)
        rl = PS.b[6]
        P.pe(I_mm(rl.ap[:, 0:288], zer.ap[:, 0:128], zer.ap[:, 0:288], True, False, skip=True), reads=[zer.r()], writes=[rl.r()])
        nch = []
        tile0 = []
        n0 = 0
        for (c0, c1, t) in chunks:
            nch.append((c0, c1, n0, t))
            tile0.append(n0 // 128)
            n0 += c1 - c0
        descs = [hchunk(c0, c1, n2T, n0_, t, ci) for ci, (c0, c1, n0_, t) in enumerate(nch)]
        rt = dict(ps=rl, tile0=tile0)
        yield_ = lambda ci: emit_norm(l, 2, [descs[ci]], router=rt, nsq=1, ci0=ci)
        if pre_norm is not None:
            pre_norm(yield_)
        else:
            emit_norm(l, 2, descs, router=rt)
        wslots = [None, None]

        def load_expert(e):
            g = AR.alloc(f"wg{e % 2}", [128, 8, 512], BF16)
            u = AR.alloc(f"wu{e % 2}", [128, 8, 512], BF16)
            dn = AR.alloc(f"wd{e % 2}", [128, 4, D], BF16)
            P.dma("pool", I_dma(g.ap, dr["wg"][l, e].rearrange("(k p) n -> p k n", p=128)), writes=[g.r()])
            P.dma("pool", I_dma(u.ap, dr["wu"][l, e].rearrange("(k p) n -> p k n", p=128)), writes=[u.r()])
            P.dma("pool", I_dma(dn.ap, dr["wd"][l, e].rearrange("(k p) n -> p k n", p=128)), writes=[dn.r()])
            wslots[e % 2] = (g, u, dn)

        load_expert(0)
        load_expert(1)
        NT_ = ntile
        R = lambda name: AR.alloc(name, [128, 288], F32)
        sc, sel, tA_, tB_, gsc, keep = R("r_sc"), R("r_sel"), R("r_ta"), R("r_tb"), R("r_gs"), R("r_keep")
        NE = NT_ * 16
        NG = NT_ * 4
        P.act(I_act(sc.ap[:, 0:NE], rl.ap[:, 0:NE], AF.Sigmoid), reads=[rl.r()], writes=[sc.r()])
        P.dve(I_tt(sel.ap[:, 0:NE], sc.ap[:, 0:NE], rb18.ap[:, 0:NE], ALU.add), reads=[sc.r(), rb18.r()], writes=[sel.r()])
        X = lambda e_: sel.ap[:, 0:NE].rearrange("p (g e) -> p g e", e=4)[:, :, e_]
        pairs = [(0, 1), (0, 2), (0, 3), (1, 2), (1, 3), (2, 3)]
        ta = tA_.ap[:, 0:NG]
        tb = tB_.ap[:, 0:NG]
        gs_ = gsc.ap[:, 0:NG]
        thr = gsc.ap[:, NG:2 * NG]
        for pi_, (a, b) in enumerate(pairs):
            P.dve(I_tt(ta, X(a), X(b), ALU.add), reads=[sel.r()], writes=[tA_.r()])
            P.dve(I_tt(tb, X(a), X(b), ALU.min), reads=[sel.r()], writes=[tB_.r()])
            if pi_ == 0:
                P.dve(I_copy(gs_, ta), reads=[tA_.r()], writes=[gsc.r("g")])
                P.dve(I_copy(thr, tb), reads=[tB_.r()], writes=[gsc.r("t")])
            else:
                P.dve(I_tt(gs_, gs_, ta, ALU.max), reads=[tA_.r(), gsc.r("g")], writes=[gsc.r("g")])
                P.dve(I_tt(thr, thr, tb, ALU.max), reads=[tB_.r(), gsc.r("t")], writes=[gsc.r("t")])
        G = lambda g_: gsc.ap[:, 0:NG].rearrange("p (t g) -> p t g", g=4)[:, :, g_]
        gm = tA_.ap[:, 0:NT_]
        P.dve(I_tt(gm, G(0), G(1), ALU.max), reads=[gsc.r("g")], writes=[tA_.r()])
        P.dve(I_tt(gm, gm, G(2), ALU.max), reads=[gsc.r("g"), tA_.r()], writes=[tA_.r()])
        P.dve(I_tt(gm, gm, G(3), ALU.max), reads=[gsc.r("g"), tA_.r()], writes=[tA_.r()])
        ing = tB_.ap[:, 0:NG]
        for g_ in range(4):
            P.dve(I_tt(ing.rearrange("p (t g) -> p t g", g=4)[:, :, g_], G(g_), gm, ALU.is_equal), reads=[gsc.r("g"), tA_.r()], writes=[tB_.r()])
        K = lambda e_: keep.ap[:, 0:NE].rearrange("p (g e) -> p g e", e=4)[:, :, e_]
        for e_ in range(4):
            P.dve(I_tt(K(e_), X(e_), thr, ALU.is_ge), reads=[sel.r(), gsc.r("t")], writes=[keep.r()])
            P.dve(I_tt(K(e_), K(e_), ing, ALU.mult), reads=[keep.r(), tB_.r()], writes=[keep.r()])
        P.dve(I_tt(keep.ap[:, 0:NE], keep.ap[:, 0:NE], sc.ap[:, 0:NE], ALU.mult), reads=[keep.r(), sc.r()], writes=[keep.r()])
        den = tA_.ap[:, 64:64 + NT_]
        P.dve(I_red(den, keep.ap[:, 0:NE].rearrange("p (t e) -> p t e", e=16), ALU.add), reads=[keep.r()], writes=[tA_.r()])
        P.dve(I_recip(den, den), reads=[tA_.r()], writes=[tA_.r()])
        comb = sel
        for t_ in range(NT_):
            P.dve(I_ts(comb.ap[:, t_ * 16:(t_ + 1) * 16], keep.ap[:, t_ * 16:(t_ + 1) * 16], tA_.ap[:, 64 + t_:65 + t_], None, ALU.mult),
                  reads=[keep.r(), tA_.r()], writes=[sel.r()])
        chi = AR.alloc("chi", [128, 288], BF16)
        clo = AR.alloc("clo", [128, 288], BF16)
        P.dve(I_copy(chi.ap[:, 0:NE], comb.ap[:, 0:NE]), reads=[sel.r()], writes=[chi.r()])
        P.dve(I_tt(clo.ap[:, 0:NE], comb.ap[:, 0:NE], chi.ap[:, 0:NE], ALU.subtract), reads=[sel.r(), chi.r()], writes=[clo.r()])
        cT = [AR.alloc("cThi", [128, ncol], BF16), AR.alloc("cTlo", [128, ncol], BF16)]
        for hi_, src in enumerate((chi, clo)):
            t_ = 0
            while t_ < NT_:
                nb = min(8, NT_ - t_)
                bank = PS.next("ct", [4, 5])
                bap = bank.ap.bitcast(BF16)
                for q in range(nb):
                    P.pe(I_tr(bap[0:16, q * 128:(q + 1) * 128], src.ap[:, (t_ + q) * 16:(t_ + q + 1) * 16], identb.ap),
                         reads=[src.r(), identb.r()], writes=[bank.r()])
                P.act(I_act(cT[hi_].ap[0:16, t_ * 128:(t_ + nb) * 128], bap[0:16, 0:nb * 128], AF.Copy), reads=[bank.r()], writes=[cT[hi_].r()])
                t_ += nb
        AR.free(sc, sel, tA_, tB_, gsc, keep, chi, clo)
        if mod_hook is not None:
            mod_hook()
        sgr = Rot(AR, "sg", [128, 512], F32, 2)
        t1r = Rot(AR, "t1", [128, 512], F32, 2)
        hidr = Rot(AR, "hid", [128, 4, 512], BF16, 2)
        for e in range(16):
            g, u, dn = wslots[e % 2]
            n0s = []
            n0 = 0
            for (c0, c1, t) in chunks:
                n0s.append(n0)
                n0 += c1 - c0
            corder = list(range(len(chunks)))
            if e == 15 and len(chunks) >= 4:
                corder = [0, 3, 1, 2] + corder[4:]
            for cidx in corder:
                c0, c1, t = chunks[cidx]
                n0 = n0s[cidx]
                w = c1 - c0
                W = slice(0, w)
                cwb = PS.b[6]
                P.pe(I_mm(cwb.ap[:, W], selm.ap[:, e, :], cT[0].ap[0:16, n0:n0 + w], True, False), reads=[selm.r(), cT[0].r()], writes=[cwb.r()])
                P.pe(I_mm(cwb.ap[:, W], selm.ap[:, e, :], cT[1].ap[0:16, n0:n0 + w], False, True), reads=[selm.r(), cT[1].r()], writes=[cwb.r()])
                hid = hidr.next()
                for f in range(4):
                    pg = PS.next("gu", [0, 1, 2, 3])
                    pu = PS.next("gu", [0, 1, 2, 3])
                    for k in range(8):
                        P.pe(I_mm(pg.ap[:, W], g.ap[:, k, f * 128:(f + 1) * 128], n2T.ap[:, k, n0:n0 + w], k == 0, k == 7),
                             reads=[g.r(), n2T.r((k, chunks.index((c0, c1, t))))], writes=[pg.r()])
                    for k in range(8):
                        P.pe(I_mm(pu.ap[:, W], u.ap[:, k, f * 128:(f + 1) * 128], n2T.ap[:, k, n0:n0 + w], k == 0, k == 7),
                             reads=[u.r(), n2T.r((k, chunks.index((c0, c1, t))))], writes=[pu.r()])
                    sg = sgr.next()
                    t1 = t1r.next()
                    P.act(I_act(sg.ap[:, W], pg.ap[:, W], AF.Silu), reads=[pg.r()], writes=[sg.r()])
                    P.dve(I_tt(t1.ap[:, W], pu.ap[:, W], sg.ap[:, W], ALU.mult), reads=[pu.r(), sg.r()], writes=[t1.r()])
                    P.dve(I_tt(hid.ap[:, f, W], t1.ap[:, W], cwb.ap[:, W], ALU.mult), reads=[t1.r(), cwb.r()], writes=[hid.r(f)])
                for dk in range(8):
                    pd = PS.next("dn", [4, 5])
                    for f in range(4):
                        P.pe(I_mm(pd.ap[:, W], dn.ap[:, f, dk * 128:(dk + 1) * 128], hid.ap[:, f, W], f == 0, f == 3),
                             reads=[dn.r(), hid.r(f)], writes=[pd.r()])
                    P.dve(I_stt(hT.ap[:, dk, c0:c1], pd.ap[:, W], scl.ap[:, l, 5, t, dk:dk + 1], hT.ap[:, dk, c0:c1], ALU.mult, ALU.add),
                          reads=[pd.r(), scl.r((l, 5, t))] + hres(dk, c0, c1), writes=hres(dk, c0, c1))
                if e == 15 and chunk_done_hook is not None:
                    chunk_done_hook(cidx)
            AR.free(g, u, dn)
            if e + 2 < 16:
                load_expert(e + 2)
            if expert_hook is not None:
                expert_hook(e)
        sgr.free()
        t1r.free()
        hidr.free()
        AR.free(n2T, cT[0], cT[1])


    def hook0():
        emit_mod(0, range(10, 12))
        emit_scl(0, ["g2"])

    def ehook0(e):
        if 1 <= e <= 12:
            emit_mod(1, [e - 1])

    emit_moe(0, [(c0, c1, 0 if c0 < CTX0 else 1) for (c0, c1) in OC], mod_hook=hook0, expert_hook=ehook0, pre_norm=l0_outproj_then)
    emit_scl(1, ["n1", "g1", "n2", "g2"])
    cut("l0")

    NH = 448
    w1 = dr["w_in1"]

    def load_w_at(name, segs, src, at):
        ncols = sum(n for _, n in segs)
        wb = AR.alloc(name, [128, 8, ncols], BF16, at=at)
        o = 0
        for si, (c0, n) in enumerate(segs):
            P.dma("pool", I_dma(wb.ap[:, :, o:o + n], src[:, c0:c0 + n].rearrange("(k p) n -> p k n", p=128)), writes=[wb.r(si)])
            o += n
        return wb

    wbs = [load_w_at(f"wC{p}", [(2 * p * 128, 256), (1024 + 2 * p * 128, 256)], w1, ARENA_BYTES - 16384 - 16384 + 8192 * p) for p in range(2)]
    hxd_views = []
    for half in range(2):
        hx_src = nc.dram_tensor(f"hx_src{half}", [128, 4 * NH], F32, kind="Internal").ap()
        hx_dst = nc.dram_tensor(f"hx_dst{half}", [4 * 128, 4 * NH], F32, kind="Internal").ap()
        rdh0, rdh1 = [], []
        for k in range(4 * half, 4 * half + 4):
            rdh0 += hres(k, OWN0, OWN0 + 192)
            rdh1 += hres(k, OWN0 + TOK - 256, OWN0 + TOK)
        hxs = hx_src.rearrange("p (k n) -> p k n", k=4)
        ks = slice(4 * half, 4 * half + 4)
        P.dma("sp", I_dma(hxs[:, :, 0:192], hT.ap[:, ks, OWN0:OWN0 + 192]), reads=rdh0, writes=[dres(f"hxs{half}").r(0)])
        P.dma("sp", I_dma(hxs[:, :, 192:448], hT.ap[:, ks, OWN0 + TOK - 256:OWN0 + TOK]), reads=rdh1, writes=[dres(f"hxs{half}").r(1)])
        P.add("pool", (lambda e, a_=hx_src, b_=hx_dst: e.collective_compute("AllGather", ALU.bypass, replica_groups=groups, ins=[a_[:, :]], outs=[b_[:, :]])),
              reads=[dres(f"hxs{half}").r(0), dres(f"hxs{half}").r(1)], writes=[dres(f"hxd{half}").r()], kind="cc")
        hxd_views.append(hx_dst.rearrange("(r p) (k n) -> p r k n", r=4, k=4))

    NT1 = 2752
    nTo = AR.alloc("nTo", [128, 8, TOK], BF16, at=0)
    nTx = AR.alloc("nTx", [128, 8, 704], BF16, at=32768)
    hH = AR.alloc("hH", [128, 8, NH], F32)

    def ncol(c):
        if c < 256:
            return nTx, c
        if c < 2304:
            return nTo, c - 256
        if c < 2496:
            return nTx, 256 + (c - 2304)
        return nTx, 448 + (c - 2496)

    def nslice(k, c0, c1):
        b, o = ncol(c0)
        return b.ap[:, k, o:o + (c1 - c0)], b.r((k, c0))

    L1CH = [(256 + 512 * c, 256 + 512 * (c + 1)) for c in range(4)] + [(2496, 2752)]
    chs = []
    for (c0, c1) in L1CH:
        if c0 < 2496:
            h0 = OWN0 + (c0 - 256)
            tt = 0
        else:
            h0 = CTX0
            tt = 1
        b, o = ncol(c0)
        chs.append(dict(w=c1 - c0, src3=hT.ap[:, :, h0:h0 + (c1 - c0)], srck=(lambda k, h0=h0, w=c1 - c0: hT.ap[:, k, h0:h0 + w]),
                        rres=(lambda k, h0=h0, w=c1 - c0: hres(k, h0, h0 + w)),
                        dst=(lambda k, b=b, o=o, w=c1 - c0: b.ap[:, k, o:o + w]), dres=(lambda k, b=b, c0=c0: b.r((k, c0))), t=tt))
    emit_norm(1, 1, chs)
    def emit_halo():
        cand = Rot(AR, "cand", [128, 2, NH], F32, 2)
        for k in range(8):
            top = hH.ap[:, k, 0:256]
            bot = hH.ap[:, k, 256:448]
            for hf in range(2):
                cb_ = cand.next()
                P.dma("sp", I_dma(cb_.ap, hxd_views[k // 4][:, 2 * hf:2 * hf + 2, k % 4, :]), reads=[dres(f"hxd{k // 4}").r()], writes=[cb_.r()])
                for rr in range(2):
                    r = 2 * hf + rr
                    if r == 0:
                        P.dve(I_ts(top, cb_.ap[:, rr, 192:448], V("flags2", r), None, ALU.mult), reads=[cb_.r(), vec.r()], writes=[hH.r(k)])
                        P.dve(I_ts(bot, cb_.ap[:, rr, 0:192], V("flags2", 4 + r), None, ALU.mult), reads=[cb_.r(), vec.r()], writes=[hH.r(k)])
                    else:
                        P.dve(I_stt(top, cb_.ap[:, rr, 192:448], V("flags2", r), top, ALU.mult, ALU.add), reads=[cb_.r(), vec.r(), hH.r(k)], writes=[hH.r(k)])
                        P.dve(I_stt(bot, cb_.ap[:, rr, 0:192], V("flags2", 4 + r), bot, ALU.mult, ALU.add), reads=[cb_.r(), vec.r(), hH.r(k)], writes=[hH.r(k)])
        cand.free()
        cut("hx")
        chs = []
        for (c0, c1, s0) in [(0, 256, 0), (2304, 2496, 256)]:
            b, o = ncol(c0)
            chs.append(dict(w=c1 - c0, src3=hH.ap[:, :, s0:s0 + (c1 - c0)], srck=(lambda k, s0=s0, w=c1 - c0: hH.ap[:, k, s0:s0 + w]),
                            rres=(lambda k: [hH.r(k)]),
                            dst=(lambda k, b=b, o=o, w=c1 - c0: b.ap[:, k, o:o + w]), dres=(lambda k, b=b, c0=c0: b.r((k, c0))), t=0))
        emit_norm(1, 1, chs, wmax=256)
        AR.free(hH)

    ALLCH = L1CH[:4] + [(2496, 2752), (0, 256), (2304, 2496)]
    def proj1(wb, seg, wc0, c0, c1, group="proj", banks=(0, 1, 2, 3)):
        bank = PS.next(group, list(banks))
        w = c1 - c0
        for k in range(8):
            ap, rs = nslice(k, c0, c1)
            P.pe(I_mm(bank.ap[:, 0:w], wb.ap[:, k, wc0:wc0 + 128], ap, k == 0, k == 7), reads=[wb.r(seg), rs], writes=[bank.r()])
        return bank

    tC = AR.alloc("tC", [128, 4, 2496], BF16)
    tmpC = Rot(AR, "tmpC", [128, 512], F32, 3)
    CCH = L1CH[:4] + [(0, 256), (2304, 2496)]
    for part in range(2):
        if part == 1:
            emit_halo()
        for p in range(2):
            wb = wbs[p]
            for (c0, c1) in (CCH[:4] if part == 0 else CCH[4:]):
                w = c1 - c0
                for jj in range(2):
                    j = 2 * p + jj
                    px = proj1(wb, 0, jj * 128, c0, c1)
                    pc = proj1(wb, 1, 256 + jj * 128, c0, c1)
                    sg = tmpC.next()
                    P.act(I_act(sg.ap[:, 0:w], pc.ap[:, 0:w], AF.Copy), reads=[pc.r()], writes=[sg.r()])
                    P.dve(I_tt(tC.ap[:, j, c0:c1], px.ap[:, 0:w], sg.ap[:, 0:w], ALU.mult), reads=[px.r(), sg.r()], writes=[tC.r((j, c0))])
    AR.free(*wbs)
    for j in range(4):
        P.dve(I_ts(tC.ap[:, j, 255:256], tC.ap[:, j, 255:256], V("flags", 0), None, ALU.mult), reads=[tC.r((j, 0)), vec.r()], writes=[tC.r((j, 0))])
        P.dve(I_ts(tC.ap[:, j, 2304:2305], tC.ap[:, j, 2304:2305], V("flags", 1), None, ALU.mult), reads=[tC.r((j, 2304)), vec.r()], writes=[tC.r((j, 2304))])
    mC = AR.alloc("mC", [128, 4, TOK], BF16, at=ARENA_BYTES - 16384)
    wb = load_w("wCb", [(512, 512)], w1)
    tcall = lambda j: [tC.r((j, c0)) for (c0, _) in CCH]
    for j in range(4):
        for (c0, c1) in L1CH[:4]:
            w = c1 - c0
            pb = proj1(wb, 0, j * 128, c0, c1)
            cv = tmpC.next()
            P.dve(I_ts(cv.ap[:, 0:w], tC.ap[:, j, c0 - 1:c1 - 1], V("ccw", 0 * 4 + j), None, ALU.mult), reads=tcall(j) + [vec.r()], writes=[cv.r()])
            P.dve(I_stt(cv.ap[:, 0:w], tC.ap[:, j, c0:c1], V("ccw", 1 * 4 + j), cv.ap[:, 0:w], ALU.mult, ALU.add), reads=tcall(j) + [vec.r(), cv.r()], writes=[cv.r()])
            P.dve(I_stt(cv.ap[:, 0:w], tC.ap[:, j, c0 + 1:c1 + 1], V("ccw", 2 * 4 + j), cv.ap[:, 0:w], ALU.mult, ALU.add), reads=tcall(j) + [vec.r(), cv.r()], writes=[cv.r()])
            P.dve(I_tt(mC.ap[:, j, c0 - 256:c1 - 256], pb.ap[:, 0:w], cv.ap[:, 0:w], ALU.mult), reads=[pb.r(), cv.r()], writes=[mC.r((j, c0))])
    AR.free(wb, tC)
    tmpC.free()

    KT = AR.alloc("KT", [128, 4, NT1], BF16)
    Vt = AR.alloc("Vt", [128, 22, 512], BF16)
    wb = load_w("wK", [(2048, 512)], w1)
    for j in range(4):
        for (c0, c1) in ALLCH:
            w = c1 - c0
            pk = proj1(wb, 0, j * 128, c0, c1)
            if (j + c0 // 256) % 2 == 0:
                P.act(I_act(KT.ap[:, j, c0:c1], pk.ap[:, 0:w], AF.Copy), reads=[pk.r()], writes=[KT.r((j, c0))])
            else:
                P.dve(I_copy(KT.ap[:, j, c0:c1], pk.ap[:, 0:w]), reads=[pk.r()], writes=[KT.r((j, c0))])
    AR.free(wb)
    wb = load_w("wV", [(2560, 512)], w1)
    vtiles = [(128 * m, min(128, 2496 - 128 * m)) for m in range(20)] + [(2496, 128), (2624, 128)]

    def chunk_of(c):
        for (c0, c1) in ALLCH:
            if c0 <= c < c1:
                return c0
        raise ValueError(c)

    vorder = [ti for ti, (c0, n) in enumerate(vtiles) if 256 <= c0 < 2304 or c0 >= 2496] + \
             [ti for ti, (c0, n) in enumerate(vtiles) if not (256 <= c0 < 2304 or c0 >= 2496)]
    for ti in vorder:
        c0, n = vtiles[ti]
        bank = PS.next("proj", [0, 1, 2, 3])
        for k in range(8):
            ap, _ = nslice(k, c0, c0 + n)
            b_, _o = ncol(c0)
            P.pe(I_mm(bank.ap[0:n, 0:512], ap, wb.ap[:, k, 0:512], k == 0, k == 7), reads=[wb.r(0), b_.r((k, chunk_of(c0)))], writes=[bank.r()])
        if ti % 2 == 0:
            P.act(I_act(Vt.ap[0:n, ti, :], bank.ap[0:n, 0:512], AF.Copy), reads=[bank.r()], writes=[Vt.r(ti)])
        else:
            P.dve(I_copy(Vt.ap[0:n, ti, :], bank.ap[0:n, 0:512]), reads=[bank.r()], writes=[Vt.r(ti)])
    AR.free(wb, nTx)
    QT = AR.alloc("QT", [128, 4, TOK], BF16)
    wb = load_w("wQ", [(1536, 512)], w1)
    for j in range(4):
        for (c0, c1) in L1CH[:4]:
            w = c1 - c0
            pq = proj1(wb, 0, j * 128, c0, c1)
            if (j + c0 // 512) % 2 == 0:
                P.act(I_act(QT.ap[:, j, c0 - 256:c1 - 256], pq.ap[:, 0:w], AF.Copy), reads=[pq.r()], writes=[QT.r((j, (c0 - 256) // 128 + mm)) for mm in range(4)])
            else:
                P.dve(I_copy(QT.ap[:, j, c0 - 256:c1 - 256], pq.ap[:, 0:w]), reads=[pq.r()], writes=[QT.r((j, (c0 - 256) // 128 + mm)) for mm in range(4)])
    AR.free(wb, nTo)
    cut("l1a")

    din_ug = dr["ug"]
    din_eg = dr["eg"]
    UG = AR.alloc("UG", [128, 8, 576], F32)
    P.dma("sp", I_dma(UG.ap, din_ug.rearrange("h p n -> p h n")), writes=[UG.r()])
    Sbr = Rot(AR, "Sb", [128, 1024], F32, 2)
    Pbr = Rot(AR, "Pb", [128, 1024], BF16, 2)
    PTr = Rot(AR, "PT", [128, 1024], BF16, 2)
    EGr = Rot(AR, "EGb", [128, 768], F32, 2)
    mdt = Rot(AR, "mdt", [128, 512], BF16, 2)
    stat = Rot(AR, "stat", [128, 8, 4], F32, 2)
    special = {0: (0, 768, 0), 1: (128, 640, 1), 15: (1792, 704, 3)}
    units = [(m, h) for m in range(16) for h in range(8)]
    ctxs = {}
    ctxa = {}
    pair_st = {}

    def pair_info(m):
        if m in special:
            return special[m]
        return 128 * m, 576, (2 if m == 14 else None)

    def stage_a(u):
        m, h = units[u]
        kc0, nloc, egi = pair_info(m)
        ntot = nloc + 256
        if m not in pair_st:
            pair_st[m] = stat.next()
        st = pair_st[m]
        jh, hp = h // 2, h % 2
        prt = slice(hp * 64, hp * 64 + 64)
        bi = 2 * (u % 2)
        sb0, sb1 = PS.b[bi], PS.b[bi + 1]
        sres = [sb0.r(), sb1.r()]
        S2 = PS.t[:, bi:bi + 2, :].rearrange("p a b -> p (a b)")
        q_ap = QT.ap[prt, jh, 128 * m:128 * m + 128]
        qr = QT.r((jh, m))
        kr = [KT.r((jh, c0)) for (c0, c1) in ALLCH if c0 < kc0 + nloc and kc0 < c1]
        P.pe(I_mm(sb0.ap[:, 0:512], q_ap, KT.ap[prt, jh, kc0:kc0 + 512]), reads=[qr] + kr, writes=[sres[0]])
        P.pe(I_mm(sb1.ap[:, 0:nloc - 512], q_ap, KT.ap[prt, jh, kc0 + 512:kc0 + nloc]), reads=[qr] + kr, writes=[sres[1]])
        P.pe(I_mm(sb1.ap[:, nloc - 512:nloc - 512 + 256], q_ap, KT.ap[prt, jh, 2496:2752]), reads=[qr, KT.r((jh, 2496))], writes=[sres[1]])
        Sb = Sbr.next()
        if egi is None:
            bias_ap, bias_r = UG.ap[:, h, 0:nloc], [UG.r()]
        else:
            eb = EGr.next()
            P.dma("sp", I_dma(eb.ap[:, 0:nloc], din_eg[egi, h, :, 0:nloc]), writes=[eb.r()])
            bias_ap, bias_r = eb.ap[:, 0:nloc], [eb.r()]
        P.dve(I_stt(Sb.ap[:, 0:nloc], S2[:, 0:nloc], 0.125, bias_ap, ALU.mult, ALU.add), reads=sres + bias_r, writes=[Sb.r()])
        P.act(I_act(Sb.ap[:, nloc:ntot], S2[:, nloc:ntot], AF.Copy, scale=0.125), reads=sres, writes=[Sb.r()])
        P.dve(lambda e, o=st.ap[:, h, 0:1], i=Sb.ap[:, 0:ntot]: e.tensor_reduce(out=o, in_=i, axis=AX.X, op=ALU.max, negate=True),
              reads=[Sb.r()], writes=[st.r(h)])
        ctxa[u] = (Sb, st, ntot)

    def stage_a2(u):
        m, h = units[u]
        Sb, st, ntot = ctxa.pop(u)
        Pb = Pbr.next()
        P.act(I_act(Pb.ap[:, 0:ntot], Sb.ap[:, 0:ntot], AF.Exp, bias=st.ap[:, h, 0:1], accum_out=st.ap[:, h, 1:2]),
              reads=[Sb.r(), st.r(h)], writes=[Pb.r(), st.r(h)])
        ctxs[u] = Pb

    ctxt = {}

    def stage_t(u):
        m, h = units[u]
        kc0, nloc, egi = pair_info(m)
        Pb = ctxs.pop(u)
        chunks_ = []
        off = 0
        while off < nloc:
            cw = min(128, nloc - off)
            chunks_.append((off, cw, (kc0 + off) // 128))
            off += cw
        chunks_.append((nloc, 128, 20))
        chunks_.append((nloc + 128, 128, 21))
        ptb = PS.next("pt", [4, 5])
        ptap = ptb.ap.bitcast(BF16)
        for i_, (off, cw, vt) in enumerate(chunks_):
            P.pe(I_tr(ptap[0:cw, i_ * 128:(i_ + 1) * 128], Pb.ap[:, off:off + cw], identb.ap), reads=[Pb.r(), identb.r()], writes=[ptb.r()])
        PT = PTr.next()
        ncols_ = len(chunks_) * 128
        if h % 2 == 0:
            P.act(I_act(PT.ap[:, 0:ncols_], ptap[:, 0:ncols_], AF.Copy), reads=[ptb.r()], writes=[PT.r()])
        else:
            P.dve(I_copy(PT.ap[:, 0:ncols_], ptap[:, 0:ncols_]), reads=[ptb.r()], writes=[PT.r()])
        ctxt[u] = (PT, chunks_)

    def stage_pv(u):
        m, h = units[u]
        PT, chunks_ = ctxt.pop(u)
        pvb = PS.b[6 + (m % 2)]
        for i_, (off, cw, vt) in enumerate(chunks_):
            P.pe(I_mm(pvb.ap[:, h * 64:(h + 1) * 64], PT.ap[0:cw, i_ * 128:(i_ + 1) * 128], Vt.ap[0:cw, vt, h * 64:(h + 1) * 64], i_ == 0, i_ == len(chunks_) - 1),
                 reads=[PT.r(), Vt.r(vt)], writes=[pvb.r()])

    def stage_e(m):
        st = pair_st[m]
        pvb = PS.b[6 + (m % 2)]
        md = mdt.next()
        allst = [st.r(h) for h in range(8)]
        P.dve(I_recip(st.ap[:, :, 2], st.ap[:, :, 1]), reads=allst, writes=allst)
        for h in range(8):
            P.dve(I_ts(md.ap[:, h * 64:(h + 1) * 64], pvb.ap[:, h * 64:(h + 1) * 64], st.ap[:, h, 2:3], None, ALU.mult), reads=[pvb.r(), st.r(h)], writes=[md.r()])
        tb_ = PS.next("pt", [4, 5])
        tbap = tb_.ap.bitcast(BF16)
        for j in range(4):
            P.pe(I_tr(tbap[:, j * 128:(j + 1) * 128], md.ap[:, j * 128:(j + 1) * 128], identb.ap), reads=[md.r(), identb.r()], writes=[tb_.r()])
        P.act(I_act(QT.ap[:, :, 128 * m:128 * m + 128], tbap[:, 0:512].rearrange("p (j n) -> p j n", j=4), AF.Copy),
              reads=[tb_.r()], writes=[QT.r((j, m)) for j in range(4)])

    NU = len(units)
    stage_a(0)
    stage_a2(0)
    stage_a(1)
    stage_a2(1)
    stage_t(0)
    for u in range(NU):
        if u + 2 < NU:
            stage_a(u + 2)
        if u + 1 < NU:
            stage_t(u + 1)
        if u + 2 < NU:
            stage_a2(u + 2)
        stage_pv(u)
        if units[u][1] == 7:
            stage_e(units[u][0])
    for r_ in (Sbr, Pbr, PTr, EGr, mdt, stat):
        r_.free()
    AR.free(UG, KT, Vt)

    wo = AR.alloc("wo", [128, 8, D], BF16)
    P.dma("pool", I_dma(wo.ap, dr["w_out"][1].rearrange("(k p) n -> p k n", p=128)), writes=[wo.r()])
    OC1 = OC[:4]

    def rhs1(mk, ci):
        c0, c1 = OC1[ci]
        if mk < 4:
            return mC.ap[:, mk, c0 - OWN0:c1 - OWN0], [mC.r((mk, 256 + 512 * ci))]
        return QT.ap[:, mk - 4, c0 - OWN0:c1 - OWN0], [QT.r((mk - 4, 4 * ci + mm)) for mm in range(4)]

    def l1_outproj_then(norm_chunk):
        out_proj(1, wo, rhs1, [(c0, c1, 0) for (c0, c1) in OC1], chunk_hook=norm_chunk)
        AR.free(wo, mC, QT)

    finals = []
    emit_moe(1, [(c0, c1, 0) for (c0, c1) in OC1], pre_norm=l1_outproj_then)
    frs = AR.alloc("frs", [128, TOK], F32)
    fsq = Rot(AR, "fsq", [128, 8, 512], BF16, 2)
    fsd = Rot(AR, "fsd", [128, 512], F32, 2)
    for ci, (c0, c1) in enumerate(OC1):
        hr = []
        for k in range(8):
            hr += hres(k, c0, c1)
        q_ = fsq.next()
        d_ = fsd.next()
        P.act(I_act(q_.ap, hT.ap[:, :, c0:c1], AF.Square), reads=hr, writes=[q_.r()])
        bank = PS.next("nrm", [4, 5])
        for k in range(8):
            P.pe(I_mm(bank.ap, onesb.ap, q_.ap[:, k, :], k == 0, k == 7), reads=[q_.r(), onesb.r()], writes=[bank.r()])
        P.act(I_act(d_.ap, bank.ap, AF.Sqrt, bias=EPS, scale=1.0 / D), reads=[bank.r()], writes=[d_.r()])
        P.dve(I_recip(frs.ap[:, ci * 512:(ci + 1) * 512], d_.ap), reads=[d_.r()], writes=[frs.r(ci)])
    fsq.free()
    fsd.free()
    ofs = [AR.alloc(f"of{i}", [128, 8, 512], F32) for i in range(2)]
    ftm = Rot(AR, "ftm", [128, 512], F32, 3)
    yt = Rot(AR, "yt", [128, D], F32, 3)

    def f_pass2(ci):
        c0, c1 = OC1[ci]
        of = ofs[ci % 2]
        for k in range(8):
            tb = ftm.next()
            P.dve(I_tt(tb.ap, hT.ap[:, k, c0:c1], frs.ap[:, ci * 512:(ci + 1) * 512], ALU.mult), reads=hres(k, c0, c1) + [frs.r(ci)], writes=[tb.r()])
            P.act(I_act(of.ap[:, k, :], tb.ap, AF.Identity, scale=V("fing", k)), reads=[tb.r(), vec.r()], writes=[of.r(k)])

    def f_out(ci):
        c0, c1 = OC1[ci]
        of = ofs[ci % 2]
        for tt in range(4):
            y_ = yt.next()
            for g in range(2):
                bank = PS.next("xt", [0, 1, 2, 3])
                for kk in range(4):
                    k = g * 4 + kk
                    P.pe(I_tr(bank.ap[:, kk * 128:(kk + 1) * 128], of.ap[:, k, tt * 128:(tt + 1) * 128], ident.ap), reads=[of.r(k), ident.r()], writes=[bank.r()])
                if g == 0:
                    P.dve(I_copy(y_.ap[:, 0:512], bank.ap), reads=[bank.r()], writes=[y_.r()])
                else:
                    P.act(I_act(y_.ap[:, 512:1024], bank.ap, AF.Copy), reads=[bank.r()], writes=[y_.r()])
            r0 = (c0 - OWN0) + tt * 128
            finals.append(P.dma("sp", I_dma(yout[r0:r0 + 128, :], y_.ap), reads=[y_.r()], writes=[dres("y").r(r0)]))

    finals = []
    for ci in range(4):
        f_pass2(ci)
        if ci >= 1:
            f_out(ci - 1)
    f_out(3)
    P.emit({"sp": finals})


def _fm(v):
    v = np.asarray(v, np.float32).reshape(-1, 128)
    return np.ascontiguousarray(v.T)


def _bias_table(rpb, w0, nrows, m, qcore):
    qc = np.arange(64)
    kc = np.arange(64)
    col_start = np.clip(qc - 8, 0, 48)
    colvalid = (kc[None, :] >= col_start[:, None]) & (kc[None, :] < col_start[:, None] + 16)
    colidx = np.clip(kc[None, :] - qc[:, None] + 15, 0, 30)
    T = np.full((8, 2, 64, nrows, 64), -BIG, np.float32)
    for rho in range(2):
        R = 32 * qcore + 2 * m + rho
        r_start = min(max(R - 4, 0), 120)
        for wp in range(nrows):
            keyrow = 32 * qcore + (w0 + wp) - 4
            if keyrow < r_start or keyrow >= r_start + 8:
                continue
            ridx = keyrow - R + 7
            vals = rpb[:, ridx, :][:, colidx]
            T[:, rho, :, wp, :] = np.where(colvalid[None], vals, np.float32(-BIG))
    return np.ascontiguousarray(T.reshape(8, 128, nrows * 64))


def prep_inputs(inp):
    f32 = lambda a: np.ascontiguousarray(np.asarray(a, np.float32))
    x, c, ctx, c_ctx = f32(inp["x"]), f32(inp["c"]), f32(inp["ctx"]), f32(inp["c_ctx"])
    shared = {
        "ident": np.eye(128, dtype=np.float32),
        "ada_w": f32(inp["ada_w"]),
        "w_in0": f32(inp["ab_w_in"][0]),
        "w_in1": f32(inp["cd_w_in"][0]),
        "w_out": f32(inp["w_out"]),
        "gate_w": f32(inp["b_gate_w"]).reshape(16, 128, 128),
        "router_w": f32(inp["router_w"]),
        "rb18": np.ascontiguousarray(np.broadcast_to(np.tile(f32(inp["router_bias"]), 18)[None, :], (128, 288))),
        "selm": np.ascontiguousarray(np.repeat(np.eye(16, dtype=np.float32)[:, :, None], 128, axis=2).reshape(16, 16 * 128)),
        "wg": f32(inp["moe_w_gate"]),
        "wu": f32(inp["moe_w_up"]),
        "wd": f32(inp["moe_w_down"]),
    }
    common = {
        "ada_b0": _fm(inp["ada_b"][0]), "ada_b1": _fm(inp["ada_b"][1]),
        "nmg0": _fm(inp["norm_mix_g"][0]), "nmg1": _fm(inp["norm_mix_g"][1]),
        "nfg0": _fm(inp["norm_ffn_g"][0]), "nfg1": _fm(inp["norm_ffn_g"][1]),
        "fing": _fm(inp["final_g"]),
        "adw_w": np.ascontiguousarray(f32(inp["a_dw_w"][0]).reshape(31, 4, 128).transpose(2, 0, 1).reshape(128, 124)),
        "adw_b": _fm(inp["a_dw_b"][0]), "aln_g": _fm(inp["a_ln_g"][0]), "aln_b": _fm(inp["a_ln_b"][0]),
        "bcw": np.ascontiguousarray(f32(inp["b_conv_w"][0]).reshape(4, 4, 128).transpose(2, 0, 1).reshape(128, 16)),
        "bcb": _fm(inp["b_conv_b"][0]),
        "bgb": np.ascontiguousarray(f32(inp["b_gate_b"][0]).reshape(4, 4, 128).transpose(2, 0, 1).reshape(128, 16)),
        "blam": np.ascontiguousarray(f32(inp["b_lambda"][0]).reshape(2, 4, 128).transpose(2, 0, 1).reshape(128, 8)),
        "ccw": np.ascontiguousarray(f32(inp["c_conv_w"][0]).reshape(3, 4, 128).transpose(2, 0, 1).reshape(128, 12)),
    }
    rpb = f32(inp["d_rpb"][0])
    shared["ug"] = _bias_table(rpb, 8, 9, 4, 1)
    maps = []
    for core in range(NCORES):
        b, q = core // 4, core % 4
        xe = np.zeros((NL0, D), np.float32)
        lo, hi = q * TOK - HAL0, (q + 1) * TOK + HAL0
        slo, shi = max(lo, 0), min(hi, 8192)
        xe[slo - lo:shi - lo] = x[b, slo:shi]
        vecT = np.zeros((128, NV), np.float32)
        for name, n in VEC_LAYOUT:
            o = VOFF[name]
            if name == "cvec":
                vecT[:, o:o + 8] = _fm(c[b])
                vecT[:, o + 8:o + 16] = _fm(c_ctx)
            elif name == "flags":
                fl = np.zeros(8, np.float32)
                fl[0] = 1.0 if q > 0 else 0.0
                fl[1] = 1.0 if q < 3 else 0.0
                fl[2 + q] = 1.0
                vecT[:, o:o + 8] = fl[None, :]
            elif name == "flags2":
                fl = np.zeros(8, np.float32)
                if q > 0:
                    fl[q - 1] = 1.0
                if q < 3:
                    fl[4 + q + 1] = 1.0
                vecT[:, o:o + 8] = fl[None, :]
            else:
                vecT[:, o:o + n] = common[name]
        m = dict(shared)
        eg = np.full((4, 8, 128, 768), -BIG, np.float32)
        for i, (mm, w0, nr) in enumerate([(0, 0, 12), (1, 2, 10), (14, 28, 9), (15, 28, 11)]):
            eg[i, :, :, 0:nr * 64] = _bias_table(rpb, w0, nr, mm, q)
        m["eg"] = eg
        m["xe"] = xe
        m["ctxb"] = np.ascontiguousarray(ctx[b])
        m["vecT"] = vecT
        maps.append(m)
    return maps


def run_stage(stage, inp):
    nc = build(stage)
    maps = prep_inputs(inp)
    res = run_bass_kernel_spmd(nc, maps, core_ids=list(range(NCORES)))
    return res


def kernel(**inputs):
    res = run_stage("full", inputs)
    out = np.zeros((2, 8192, D), np.float32)
    for core in range(NCORES):
        b, q = core // 4, core % 4
        out[b, q * TOK:(q + 1) * TOK] = res.results[core]["y"]
    return out
```

```python
import contextlib
import numpy as np
import concourse.bass as bass
import concourse.mybir as mybir
from concourse.bass_utils import run_bass_kernel_spmd

F32 = mybir.dt.float32
BF16 = mybir.dt.bfloat16
AF = mybir.ActivationFunctionType
ALU = mybir.AluOpType
AX = mybir.AxisListType

NCORES = 8
D = 1024
KD = 8
TOK = 2048
CTX = 256
HAL0 = 16
NL0 = TOK + 2 * HAL0
NT0 = NL0 + CTX
OWN0 = HAL0
CTX0 = NL0
UW = NL0 + 15 + CTX + 15
UCTX = NL0 + 15
EPS = 1e-6
BIG = 30000.0
DEBUG_SERIAL = False


class Res:
    __slots__ = ("name", "w", "rc", "rd")

    def __init__(self, name, init=()):
        self.name = name
        self.w = None
        self.rc = {}
        self.rd = []
        for o in init:
            self.addr(o)

    def addr(self, o):
        if o.kind == "d":
            self.rd.append(o)
        else:
            p = self.rc.get(o.eng)
            if p is None or p.idx < o.idx:
                self.rc[o.eng] = o

    def readers(self):
        return list(self.rc.values()) + self.rd


class Buf:
    def __init__(self, name, ap, lo=0, hi=0, init=()):
        self.name = name
        self.ap = ap
        self.lo = lo
        self.hi = hi
        self.init = list(init)
        self.subs = {}

    def r(self, key=0):
        s = self.subs.get(key)
        if s is None:
            s = Res(f"{self.name}.{key}", self.init)
            self.subs[key] = s
        return s

    def last_ops(self):
        d = {}
        for s in self.subs.values():
            if s.w is not None:
                d[s.w.idx] = s.w
            for x in s.readers():
                d[x.idx] = x
        for x in self.init:
            d[x.idx] = x
        best = {}
        out = []
        for x in d.values():
            if x.kind == "d":
                out.append(x)
            else:
                p = best.get(x.eng)
                if p is None or p.idx < x.idx:
                    best[x.eng] = x
        return out + list(best.values())


class Op:
    __slots__ = ("eng", "fn", "deps", "kind", "need", "sigsem", "sigval", "idx", "name")


ENGS = ["pe", "dve", "act", "pool", "sp"]


class Prog:
    def __init__(self, nc):
        self.nc = nc
        self.q = {e: [] for e in ENGS}
        self.n = 0

    def add(self, eng, fn, reads=(), writes=(), kind="c", name=""):
        o = Op()
        o.eng = eng
        o.fn = fn
        o.kind = kind
        o.need = False
        o.sigsem = None
        o.sigval = 0
        o.idx = self.n
        o.name = name
        self.n += 1
        deps = {}
        for r in reads:
            if r.w is not None:
                deps[r.w.idx] = r.w
        for w in writes:
            if w.w is not None:
                deps[w.w.idx] = w.w
            for x in w.readers():
                deps[x.idx] = x
        best = {}
        o.deps = []
        for d in deps.values():
            if d is o:
                continue
            if d.kind == "d":
                d.need = True
                o.deps.append(d)
                continue
            if d.eng == "pe" and eng == "pe" and kind == "c":
                continue
            p = best.get(d.eng)
            if p is None or p.idx < d.idx:
                best[d.eng] = d
        for d in best.values():
            d.need = True
            o.deps.append(d)
        for r in reads:
            r.addr(o)
        for w in writes:
            w.w = o
            w.rc = {}
            w.rd = []
        self.q[eng].append(o)
        return o

    def pe(self, fn, reads=(), writes=(), **k):
        return self.add("pe", fn, reads, writes, **k)

    def dve(self, fn, reads=(), writes=(), **k):
        return self.add("dve", fn, reads, writes, **k)

    def act(self, fn, reads=(), writes=(), **k):
        return self.add("act", fn, reads, writes, **k)

    def pool(self, fn, reads=(), writes=(), **k):
        return self.add("pool", fn, reads, writes, **k)

    def dma(self, q, fn, reads=(), writes=(), **k):
        return self.add(q, fn, reads, writes, kind="d", **k)

    def emit(self, final_waits):
        nc = self.nc
        NDS = 8
        with contextlib.ExitStack() as es:
            csem = {e: es.enter_context(nc.semaphore(f"c_{e}")) for e in ENGS}
            dsem = {e: [es.enter_context(nc.semaphore(f"d_{e}{i}")) for i in range(NDS)] for e in ENGS}
            for e in ENGS:
                cc = 0
                dcnt = [0] * NDS
                di = 0
                for o in self.q[e]:
                    if o.kind == "d":
                        s = di % NDS
                        di += 1
                        dcnt[s] += 16
                        o.sigsem = ("d", e, s)
                        o.sigval = dcnt[s]
                    else:
                        if o.need or o.kind == "cc":
                            cc += 1
                            o.sigsem = ("c", e, 0)
                            o.sigval = cc
            block = es.enter_context(nc.Block())

            def semof(key):
                return csem[key[1]] if key[0] == "c" else dsem[key[1]][key[2]]

            def run(ename, eng):
                waited = {}
                for o in self.q[ename]:
                    dl = list(o.deps)
                    if o.kind == "d":
                        prev = o.sigval - 16
                        if prev > 0:
                            key = o.sigsem
                            if waited.get(key, 0) < prev:
                                eng.wait_ge(semof(key), prev)
                                waited[key] = prev
                    for d in dl:
                        key = d.sigsem
                        if waited.get(key, 0) < d.sigval:
                            eng.wait_ge(semof(key), d.sigval)
                            waited[key] = d.sigval
                    ins = o.fn(eng)
                    if o.sigsem is not None:
                        ins.then_inc(semof(o.sigsem), 16 if o.kind == "d" else 1)
                for o in final_waits.get(ename, []):
                    eng.wait_ge(semof(o.sigsem), o.sigval)
                last = {}
                for o in self.q[ename]:
                    if o.kind == "d":
                        last[o.sigsem] = o.sigval
                for key, val in last.items():
                    if waited.get(key, 0) < val:
                        eng.wait_ge(semof(key), val)

            @block.tensor
            def _(eng):
                run("pe", eng)

            @block.vector
            def _(eng):
                run("dve", eng)

            @block.scalar
            def _(eng):
                run("act", eng)

            @block.gpsimd
            def _(eng):
                run("pool", eng)

            @block.sync
            def _(eng):
                run("sp", eng)


class Arena:
    def __init__(self, nc, nbytes, name="arena"):
        self.nbytes = nbytes
        self.t = nc.alloc_sbuf_tensor(name, [128, nbytes // 4], F32)
        self.live = []
        self.dead = []

    def alloc(self, name, shape, dtype, at=None):
        esz = 4 if dtype == F32 else 2
        n = 1
        for s in shape[1:]:
            n *= s
        nb = (n * esz + 63) // 64 * 64
        if at is None:
            pts = sorted([(lo, hi) for lo, hi, _ in self.live])
            cur = 0
            at = None
            for lo, hi in pts:
                if lo - cur >= nb:
                    at = cur
                    break
                cur = max(cur, hi)
            if at is None:
                if self.nbytes - cur >= nb:
                    at = cur
                else:
                    raise RuntimeError(f"arena full allocating {name} {nb}B; live={[(b.name, lo, hi) for lo, hi, b in self.live]}")
        lo, hi = at, at + nb
        assert hi <= self.nbytes, (name, lo, hi)
        for l2, h2, b2 in self.live:
            assert not (l2 < hi and lo < h2), f"arena overlap {name} [{lo},{hi}) with {b2.name} [{l2},{h2})"
        init = []
        keep = []
        for dl, dh, db in self.dead:
            if dl < hi and lo < dh:
                init.extend(db.last_ops())
                keep.append((dl, dh, db))
            else:
                keep.append((dl, dh, db))
        self.dead = keep
        ap = self.t[:, lo // 4: hi // 4]
        if dtype != F32:
            ap = ap.bitcast(dtype)
        ap = ap[:, 0:n]
        if len(shape) == 3:
            ap = ap.rearrange("p (a b) -> p a b", a=shape[1])
        elif len(shape) == 4:
            ap = ap.rearrange("p (a b c) -> p a b c", a=shape[1], b=shape[2])
        if shape[0] != 128:
            ap = ap[0:shape[0]]
        b = Buf(name, ap, lo, hi, init)
        self.live.append((lo, hi, b))
        return b

    def free(self, *bufs):
        for b in bufs:
            for i, (lo, hi, x) in enumerate(self.live):
                if x is b:
                    self.live.pop(i)
                    self.dead.append((lo, hi, b))
                    break
            else:
                raise RuntimeError(f"free of non-live {b.name}")
        if len(self.dead) > 400:
            self.dead = self.dead[-400:]


def I_mm(out, lhsT, rhs, start=True, stop=True, skip=False):
    if skip:
        return lambda e: e.matmul(out, lhsT, rhs, start=start, stop=stop, skip_group_check=True)
    return lambda e: e.matmul(out, lhsT, rhs, start=start, stop=stop)


def I_tr(out, in_, ident):
    return lambda e: e.transpose(out, in_, ident)


def I_act(out, in_, func, bias=None, scale=None, accum_out=None):
    def f(e):
        kw = {}
        if bias is not None:
            kw["bias"] = bias
        if scale is not None:
            kw["scale"] = scale
        if accum_out is not None:
            kw["accum_out"] = accum_out
        return e.activation(out=out, in_=in_, func=func, **kw)
    return f


def I_tt(out, in0, in1, op):
    return lambda e: e.tensor_tensor(out=out, in0=in0, in1=in1, op=op)


def I_ts(out, in0, s1, s2, op0, op1=None):
    if op1 is None:
        return lambda e: e.tensor_scalar(out=out, in0=in0, scalar1=s1, scalar2=None, op0=op0)
    return lambda e: e.tensor_scalar(out=out, in0=in0, scalar1=s1, scalar2=s2, op0=op0, op1=op1)


def I_stt(out, in0, scalar, in1, op0, op1):
    return lambda e: e.scalar_tensor_tensor(out=out, in0=in0, scalar=scalar, in1=in1, op0=op0, op1=op1)


def I_copy(out, in_):
    return lambda e: e.tensor_copy(out=out, in_=in_)


def I_dma(out, in_):
    return lambda e: e.dma_start(out=out, in_=in_)


def I_scan(out, d0, d1, init, op0=ALU.mult, op1=ALU.add):
    return lambda e: e.tensor_tensor_scan(out=out, data0=d0, data1=d1, initial=init, op0=op0, op1=op1)


def I_recip(out, in_):
    return lambda e: e.reciprocal(out=out, in_=in_)


def I_red(out, in_, op, axis=AX.X):
    return lambda e: e.tensor_reduce(out=out, in_=in_, axis=axis, op=op)


def I_memset(ap, v):
    return lambda e: e.memset(ap, v)


class PSum:
    def __init__(self, nc):
        self.t = nc.alloc_psum_tensor("ps", [128, 8, 512], F32)
        self.b = [Buf(f"ps{i}", self.t[:, i, :]) for i in range(8)]
        self.rr = {}

    def next(self, group, banks):
        i = self.rr.get(group, 0)
        self.rr[group] = i + 1
        return self.b[banks[i % len(banks)]]


VEC_LAYOUT = [
    ("cvec", 16), ("ada_b0", 48), ("ada_b1", 48), ("nmg0", 8), ("nmg1", 8), ("nfg0", 8), ("nfg1", 8), ("fing", 8),
    ("adw_w", 124), ("adw_b", 4), ("aln_g", 4), ("aln_b", 4), ("bcw", 16), ("bcb", 4), ("bgb", 16), ("blam", 8),
    ("ccw", 12), ("flags", 8), ("flags2", 8),
]
VOFF = {}
_o = 0
for _n, _c in VEC_LAYOUT:
    VOFF[_n] = _o
    _o += _c
NV = _o

ARENA_BYTES = 121 * 1024


class Rot:
    def __init__(self, AR, name, shape, dtype, n=2):
        self.bufs = [AR.alloc(f"{name}{i}", shape, dtype) for i in range(n)]
        self.i = 0
        self.AR = AR

    def next(self):
        b = self.bufs[self.i % len(self.bufs)]
        self.i += 1
        return b

    def free(self):
        self.AR.free(*self.bufs)


class _Done(Exception):
    pass


def build(stage="full"):
    nc = bass.Bass("TRN2", target_bir_lowering=False)
    try:
        _build(nc, stage)
    except _Done:
        pass
    return nc


def _build(nc, stage):
    dr = {}

    def din(name, shape):
        dr[name] = nc.dram_tensor(name, list(shape), F32, kind="ExternalInput").ap()

    din("xe", [NL0, D])
    din("ctxb", [CTX, D])
    din("vecT", [128, NV])
    din("ident", [128, 128])
    din("ada_w", [2, D, 6 * D])
    din("w_in0", [D, 2048])
    din("w_in1", [D, 3072])
    din("w_out", [2, D, D])
    din("gate_w", [16, 128, 128])
    din("router_w", [D, 16])
    din("rb18", [128, 288])
    din("selm", [16, 16 * 128])
    din("wg", [2, 16, D, 512])
    din("wu", [2, 16, D, 512])
    din("wd", [2, 16, 512, D])
    din("ug", [8, 128, 576])
    din("eg", [4, 8, 128, 768])
    yout = nc.dram_tensor("y", [TOK, D], F32, kind="ExternalOutput").ap()
    dbg = None
    if stage != "full":
        dbg = nc.dram_tensor("dbg", [128, KD, NT0], F32, kind="ExternalOutput").ap()
    abD = nc.dram_tensor("abD", [8, 2, 128, TOK], F32, kind="Internal").ap()
    cc_src = nc.dram_tensor("cc_src", [128, 16], F32, kind="Internal").ap()
    cc_dst = nc.dram_tensor("cc_dst", [4 * 128, 16], F32, kind="Internal").ap()
    groups = [[0, 1, 2, 3], [4, 5, 6, 7]]

    P = Prog(nc)
    AR = Arena(nc, ARENA_BYTES)
    PS = PSum(nc)

    def sb(name, shape, dt=F32):
        return Buf(name, nc.alloc_sbuf_tensor("sb_" + name, list(shape), dt)[:])

    hT = sb("hT", [128, KD, NT0])
    ident = sb("identf", [128, 128])
    identb = sb("identb", [128, 128], BF16)
    onesb = sb("onesb", [128, 128], BF16)
    vec = sb("vec", [128, NV])
    modT = [sb(f"mod{l}", [128, 48, 2]) for l in range(2)]
    scl = sb("scl", [128, 2, 6, 2, 8])
    condT = sb("condT", [128, 8, 2], BF16)
    lruc = sb("lruc", [128, 2, 8])
    lrut = sb("lrut", [128, 8, 8])
    rw = sb("rw", [128, 8, 16])
    rb18 = sb("rb18", [128, 288])
    selm = sb("selm", [16, 16, 128], BF16)
    zer = sb("zer", [128, 288])
    Sm = sb("Sm", [128, 4, 2, 5, 2])
    RS = sb("RS", [128, 4, 2, 4])
    hend = sb("hend", [128, 2, 4])
    summ = sb("summ", [128, 16])
    gath = sb("gath", [128, 4, 16])
    Hin = sb("Hin", [128, 2, 4])
    tiny = sb("tiny", [128, 8, 4])

    DB = {}

    def dres(name):
        if name not in DB:
            DB[name] = Buf(name, None)
        return DB[name]

    def hres(k, c0, c1):
        out = []
        if c1 > CTX0:
            out.append(hT.r((k, "x")))
        if c0 < CTX0:
            for c in range(4):
                lo = 0 if c == 0 else OWN0 + 512 * c
                hi = NL0 if c == 3 else OWN0 + 512 * (c + 1)
                if c0 < hi and lo < min(c1, CTX0):
                    out.append(hT.r((k, c)))
        return out

    V = lambda name, i=0, n=1: vec.ap[:, VOFF[name] + i: VOFF[name] + i + n]

    def cut(name, dumps=None):
        if stage != name:
            return
        fin = []
        rd = []
        for k in range(8):
            rd += hres(k, 0, NT0)
        if dumps:
            for (dst, src, reads) in dumps:
                fin.append(P.dma("sp", I_dma(dst, src), reads=reads, writes=[dres("dbgx").r()]))
        else:
            fin.append(P.dma("sp", I_dma(dbg[:, :, :], hT.ap), reads=rd, writes=[dres("dbg").r()]))
        P.emit({"sp": fin})
        raise _Done()

    P.dma("sp", I_dma(ident.ap, dr["ident"][:, :]), writes=[ident.r()])
    P.dma("sp", I_dma(vec.ap, dr["vecT"][:, :]), writes=[vec.r()])
    P.dma("sp", I_dma(rw.ap, dr["router_w"].rearrange("(k p) n -> p k n", p=128)), writes=[rw.r()])
    P.dma("sp", I_dma(rb18.ap, dr["rb18"][:, :]), writes=[rb18.r()])
    P.dma("pool", I_dma(selm.ap, dr["selm"].rearrange("p (e n) -> p e n", e=16)), writes=[selm.r()])
    P.dve(I_copy(identb.ap, ident.ap), reads=[ident.r()], writes=[identb.r()])
    P.dve(I_memset(onesb.ap, 1.0), writes=[onesb.r()])
    P.dve(I_memset(zer.ap, 0.0), writes=[zer.r()])
    P.act(I_act(condT.ap.rearrange("p k t -> p t k"), V("cvec", 0, 16).rearrange("p (t k) -> p t k", t=2), AF.Silu),
          reads=[vec.r()], writes=[condT.r()])

    cut("c0")
    psmod = PS.b[7]

    def emit_mod(l, pieces):
        for pc in pieces:
            wb = AR.alloc("adaw", [128, 8, 512], BF16)
            P.dma("pool", I_dma(wb.ap, dr["ada_w"][l, :, pc * 512:(pc + 1) * 512].rearrange("(k p) n -> p k n", p=128)),
                  writes=[wb.r()])
            pr = psmod.r()
            base = 320 + (l * 48 + pc * 4) * 2
            for o4 in range(4):
                pap = psmod.ap[:, base + o4 * 2:base + o4 * 2 + 2]
                for k in range(8):
                    P.pe(I_mm(pap, wb.ap[:, k, o4 * 128:(o4 + 1) * 128], condT.ap[:, k, :], k == 0, k == 7),
                         reads=[wb.r(), condT.r()], writes=[pr])
            pp = psmod.ap[:, base:base + 8].rearrange("p (o t) -> p o t", t=2)
            for t in range(2):
                P.dve(I_tt(modT[l].ap[:, pc * 4:pc * 4 + 4, t], pp[:, :, t], V(f"ada_b{l}", pc * 4, 4), ALU.add),
                      reads=[pr, vec.r()], writes=[modT[l].r(pc * 4 + o) for o in range(4)])
            AR.free(wb)

    def emit_scl(l, kinds):
        for t in range(2):
            m = modT[l].ap
            if "n1" in kinds:
                P.dve(I_stt(scl.ap[:, l, 0, t, :], m[:, 8:16, t], 1.0, V(f"nmg{l}", 0, 8), ALU.add, ALU.mult),
                      reads=[modT[l].r(o) for o in range(8, 16)] + [vec.r()], writes=[scl.r((l, 0, t))])
                P.dve(I_copy(scl.ap[:, l, 1, t, :], m[:, 0:8, t]), reads=[modT[l].r(o) for o in range(0, 8)], writes=[scl.r((l, 1, t))])
            if "g1" in kinds:
                P.dve(I_copy(scl.ap[:, l, 2, t, :], m[:, 16:24, t]), reads=[modT[l].r(o) for o in range(16, 24)], writes=[scl.r((l, 2, t))])
            if "n2" in kinds:
                P.dve(I_stt(scl.ap[:, l, 3, t, :], m[:, 32:40, t], 1.0, V(f"nfg{l}", 0, 8), ALU.add, ALU.mult),
                      reads=[modT[l].r(o) for o in range(32, 40)] + [vec.r()], writes=[scl.r((l, 3, t))])
                P.dve(I_copy(scl.ap[:, l, 4, t, :], m[:, 24:32, t]), reads=[modT[l].r(o) for o in range(24, 32)], writes=[scl.r((l, 4, t))])
            if "g2" in kinds:
                P.dve(I_copy(scl.ap[:, l, 5, t, :], m[:, 40:48, t]), reads=[modT[l].r(o) for o in range(40, 48)], writes=[scl.r((l, 5, t))])

    cut("c0b")
    xs = Rot(AR, "xs", [128, D], F32, 4)
    tiles = [("xe", t * 128, min(128, NL0 - t * 128), t * 128) for t in range(17)] + \
            [("ctxb", t * 128, 128, CTX0 + t * 128) for t in range(2)]
    for ti, (src, r0, rows, col) in enumerate(tiles):
        b = xs.next()
        P.dma("sp", I_dma(b.ap[0:rows, :], dr[src][r0:r0 + rows, :]), writes=[b.r()])
        for g in range(2):
            bank = PS.next("xt", [4, 5, 6])
            for kk in range(4):
                k = g * 4 + kk
                P.pe(I_tr(bank.ap[:, kk * 128:kk * 128 + rows], b.ap[0:rows, k * 128:(k + 1) * 128], ident.ap[0:rows, 0:rows]),
                     reads=[b.r(), ident.r()], writes=[bank.r()])
            srcap = bank.ap.rearrange("p (a b) -> p a b", a=4)[:, :, 0:rows]
            dst = hT.ap[:, 4 * g:4 * g + 4, col:col + rows]
            wr = []
            for k in range(4 * g, 4 * g + 4):
                wr += hres(k, col, col + rows)
            if (ti + g) % 2 == 0:
                P.dve(I_copy(dst, srcap), reads=[bank.r()], writes=wr)
            else:
                P.act(I_act(dst, srcap, AF.Copy), reads=[bank.r()], writes=wr)
            if ti == 0 and g == 0:
                cut("x0")
            if ti == 0 and g == 1:
                cut("x1")
            if ti == 1 and g == 1:
                cut("x2")
    xs.free()
    emit_mod(0, range(0, 4))
    cut("s0")
    emit_scl(0, ["n1"])

    cut("c1")

    def hchunk(c0, c1, nT, n0, t, ci):
        return dict(w=c1 - c0, src3=hT.ap[:, :, c0:c1], srck=lambda k: hT.ap[:, k, c0:c1], rres=lambda k: hres(k, c0, c1),
                    dst=lambda k: nT.ap[:, k, n0:n0 + (c1 - c0)], dres=lambda k: nT.r((k, ci)), t=t)

    def emit_norm(l, kind, chunks, router=None, gvec=None, wmax=512):
        gsk, shk = (0, 1) if kind == 1 else (3, 4)
        ncols_all = sum(ch["w"] for ch in chunks)
        sq = Rot(AR, "nsq", [128, 8, wmax], BF16, 2)
        sd = Rot(AR, "nsd", [128, wmax], F32, 2)
        rstd = AR.alloc("nrstd", [128, ncols_all], F32)
        offs = []
        o_ = 0
        for ci, ch in enumerate(chunks):
            w = ch["w"]
            offs.append(o_)
            hr = []
            for k in range(8):
                hr += ch["rres"](k)
            sq_ = sq.next()
            sd_ = sd.next()
            P.act(I_act(sq_.ap[:, :, 0:w], ch["src3"], AF.Square), reads=hr, writes=[sq_.r()])
            bank = PS.next("nrm", [4, 5])
            for k in range(8):
                P.pe(I_mm(bank.ap[:, 0:w], onesb.ap, sq_.ap[:, k, 0:w], k == 0, k == 7), reads=[sq_.r(), onesb.r()], writes=[bank.r()])
            P.act(I_act(sd_.ap[:, 0:w], bank.ap[:, 0:w], AF.Sqrt, bias=EPS, scale=1.0 / D), reads=[bank.r()], writes=[sd_.r()])
            P.dve(I_recip(rstd.ap[:, o_:o_ + w], sd_.ap[:, 0:w]), reads=[sd_.r()], writes=[rstd.r(ci)])
            o_ += w
        sq.free()
        sd.free()
        tmp = Rot(AR, "ntmp", [128, wmax], F32, 3)
        n2f = Rot(AR, "n2f", [128, wmax], F32, 3) if router is not None else None
        cnt = 0
        for ci, ch in enumerate(chunks):
            w = ch["w"]
            t = ch["t"]
            rs_ap = rstd.ap[:, offs[ci]:offs[ci] + w]
            for k in range(8):
                tb = tmp.next()
                P.dve(I_tt(tb.ap[:, 0:w], ch["srck"](k), rs_ap, ALU.mult), reads=ch["rres"](k) + [rstd.r(ci)], writes=[tb.r()])
                if kind == 3:
                    P.act(I_act(ch["dst"](k), tb.ap[:, 0:w], AF.Identity, scale=gvec(k)), reads=[tb.r(), vec.r()], writes=[ch["dres"](k)])
                    continue
                gs = scl.ap[:, l, gsk, t, k:k + 1]
                sh = scl.ap[:, l, shk, t, k:k + 1]
                sr = [scl.r((l, gsk, t)), scl.r((l, shk, t))]
                if router is None:
                    P.act(I_act(ch["dst"](k), tb.ap[:, 0:w], AF.Identity, bias=sh, scale=gs), reads=[tb.r()] + sr, writes=[ch["dres"](k)])
                else:
                    fb = n2f.next()
                    P.act(I_act(fb.ap[:, 0:w], tb.ap[:, 0:w], AF.Identity, bias=sh, scale=gs), reads=[tb.r()] + sr, writes=[fb.r()])
                    cnt += 1
                    if cnt % 3 == 0:
                        P.act(I_act(ch["dst"](k), fb.ap[:, 0:w], AF.Copy), reads=[fb.r()], writes=[ch["dres"](k)])
                    elif cnt % 3 == 1:
                        P.dve(I_copy(ch["dst"](k), fb.ap[:, 0:w]), reads=[fb.r()], writes=[ch["dres"](k)])
                    else:
                        P.pool(I_copy(ch["dst"](k), fb.ap[:, 0:w]), reads=[fb.r()], writes=[ch["dres"](k)])
                    tile0 = router["tile0"][ci]
                    for tt in range(w // 128):
                        P.pe(I_mm(router["ps"].ap[:, (tile0 + tt) * 16:(tile0 + tt) * 16 + 16], fb.ap[:, tt * 128:(tt + 1) * 128], rw.ap[:, k, :], False, k == 7, skip=True),
                             reads=[fb.r(), rw.r()], writes=[router["ps"].r()])
        AR.free(rstd)
        tmp.free()
        if n2f is not None:
            n2f.free()

    CH0 = [(i * 416, (i + 1) * 416) for i in range(5)] + [(CTX0, NT0)]
    OC = [(OWN0 + 512 * c, OWN0 + 512 * (c + 1)) for c in range(4)] + [(CTX0, NT0)]

    def ucol(c0):
        return c0 if c0 < CTX0 else UCTX

    def mcol(c0):
        return c0 - OWN0 if c0 < CTX0 else TOK

    nT = AR.alloc("nT", [128, 8, NT0], BF16)
    emit_norm(0, 1, [hchunk(c0, c1, nT, c0, 0 if c0 < CTX0 else 1, ci) for ci, (c0, c1) in enumerate(CH0)])

    TOP = ARENA_BYTES
    gB = AR.alloc("gB", [128, 4, NT0], BF16, at=TOP - 18688)
    uA = AR.alloc("uA", [128, 4, UW], BF16, at=TOP - 18688 - 2048 - 18944)
    uB = AR.alloc("uB", [128, 4, UW], BF16, at=TOP - 18688 - 2048 - 2 * 18944)
    for ub in (uA, uB):
        P.pool(I_memset(ub.ap[:, :, NL0:NL0 + 15], 0.0), writes=[ub.r((j, 5)) for j in range(4)])
        P.pool(I_memset(ub.ap[:, :, UCTX + CTX:UW], 0.0), writes=[ub.r((j, 5)) for j in range(4)])
    w0 = dr["w_in0"]

    def load_w(name, segs, src):
        ncols = sum(n for _, n in segs)
        wb = AR.alloc(name, [128, 8, ncols], BF16)
        o = 0
        for si, (c0, n) in enumerate(segs):
            P.dma("pool", I_dma(wb.ap[:, :, o:o + n], src[:, c0:c0 + n].rearrange("(k p) n -> p k n", p=128)), writes=[wb.r(si)])
            o += n
        return wb

    tA = Rot(AR, "tA", [128, 416], F32, 3)

    def proj(wb, seg, wc0, ci, c0, c1):
        bank = PS.next("proj", [0, 1, 2, 3])
        w = c1 - c0
        for k in range(8):
            P.pe(I_mm(bank.ap[:, 0:w], wb.ap[:, k, wc0:wc0 + 128], nT.ap[:, k, c0:c1], k == 0, k == 7),
                 reads=[wb.r(seg), nT.r((k, ci))], writes=[bank.r()])
        return bank

    for p in range(2):
        wb = load_w("wA", [(2 * p * 128, 256), (512 + 2 * p * 128, 256)], w0)
        for ci, (c0, c1) in enumerate(CH0):
            w = c1 - c0
            for jj in range(2):
                j = 2 * p + jj
                pv = proj(wb, 0, jj * 128, ci, c0, c1)
                pg = proj(wb, 1, 256 + jj * 128, ci, c0, c1)
                sg = tA.next()
                P.act(I_act(sg.ap[:, 0:w], pg.ap[:, 0:w], AF.Sigmoid), reads=[pg.r()], writes=[sg.r()])
                P.dve(I_tt(uA.ap[:, j, ucol(c0):ucol(c0) + w], pv.ap[:, 0:w], sg.ap[:, 0:w], ALU.mult),
                      reads=[pv.r(), sg.r()], writes=[uA.r((j, ci))])
        AR.free(wb)
    wb = load_w("wBu", [(1024, 512)], w0)
    for j in range(4):
        for ci, (c0, c1) in enumerate(CH0):
            w = c1 - c0
            pu = proj(wb, 0, j * 128, ci, c0, c1)
            P.act(I_act(uB.ap[:, j, ucol(c0):ucol(c0) + w], pu.ap[:, 0:w], AF.Copy), reads=[pu.r()], writes=[uB.r((j, ci))])
    AR.free(wb)
    for ub in (uA, uB):
        for j in range(4):
            P.dve(I_ts(ub.ap[:, j, 0:HAL0], ub.ap[:, j, 0:HAL0], V("flags", 0), None, ALU.mult), reads=[ub.r((j, 0)), vec.r()], writes=[ub.r((j, 0))])
            P.dve(I_ts(ub.ap[:, j, NL0 - HAL0:NL0], ub.ap[:, j, NL0 - HAL0:NL0], V("flags", 1), None, ALU.mult), reads=[ub.r((j, 4)), vec.r()], writes=[ub.r((j, 4))])
    wb = load_w("wBg", [(1536, 512)], w0)
    for j in range(4):
        for ci, (c0, c1) in enumerate(CH0):
            w = c1 - c0
            pg = proj(wb, 0, j * 128, ci, c0, c1)
            x2 = tA.next()
            P.act(I_act(x2.ap[:, 0:w], pg.ap[:, 0:w], AF.Square), reads=[pg.r()], writes=[x2.r()])
            P.dve(I_ts(x2.ap[:, 0:w], x2.ap[:, 0:w], 0.044715, 1.0, ALU.mult, ALU.add), reads=[x2.r()], writes=[x2.r()])
            t2 = tA.next()
            P.dve(I_tt(t2.ap[:, 0:w], pg.ap[:, 0:w], x2.ap[:, 0:w], ALU.mult), reads=[pg.r(), x2.r()], writes=[t2.r()])
            P.act(I_act(t2.ap[:, 0:w], t2.ap[:, 0:w], AF.Sigmoid, scale=1.5957691216), reads=[t2.r()], writes=[t2.r()])
            P.dve(I_tt(gB.ap[:, j, c0:c1], pg.ap[:, 0:w], t2.ap[:, 0:w], ALU.mult), reads=[pg.r(), t2.r()], writes=[gB.r((j, ci))])
    AR.free(wb)
    tA.free()
    AR.free(nT)

    def allres(buf, j, n=6):
        return [buf.r((j, ci)) for ci in range(n)]

    emit_mod(0, range(4, 6))
    emit_scl(0, ["g1"])
    diagB = AR.alloc("diagB", [128, 16, 128], BF16)
    for k in range(4):
        for j in range(4):
            P.dve(I_ts(diagB.ap[:, k * 4 + j, :], identb.ap, V("bcw", k * 4 + j), None, ALU.mult),
                  reads=[identb.r(), vec.r()], writes=[diagB.r(k * 4 + j)])
    gw = AR.alloc("gw", [128, 16, 128], BF16)
    P.dma("pool", I_dma(gw.ap, dr["gate_w"].rearrange("g k j -> k g j")), writes=[gw.r()])
    L = lambda i: lrut.ap[:, i, :]
    lr = lrut.r()
    P.dve(I_ts(L(0), V("blam", 0, 8), -1.0, None, ALU.mult), reads=[vec.r()], writes=[lr])
    P.dve(I_tt(L(1), L(0), V("blam", 0, 8), ALU.max), reads=[lr, vec.r()], writes=[lr])
    P.act(I_act(L(2), L(1), AF.Exp, scale=-1.0), reads=[lr], writes=[lr])
    P.dve(I_ts(L(3), L(2), 2.0, None, ALU.add), reads=[lr], writes=[lr])
    P.dve(I_recip(L(3), L(3)), reads=[lr], writes=[lr])
    P.dve(I_tt(L(3), L(3), L(2), ALU.mult), reads=[lr], writes=[lr])
    P.dve(I_tt(L(4), L(3), L(3), ALU.mult), reads=[lr], writes=[lr])
    P.dve(I_ts(L(5), L(4), 1.0 / 13, 1.0 / 11, ALU.mult, ALU.add), reads=[lr], writes=[lr])
    for cf in (1.0 / 9, 1.0 / 7, 1.0 / 5, 1.0 / 3, 1.0):
        P.dve(I_tt(L(5), L(5), L(4), ALU.mult), reads=[lr], writes=[lr])
        P.dve(I_ts(L(5), L(5), cf, None, ALU.add), reads=[lr], writes=[lr])
    P.dve(I_tt(L(5), L(5), L(3), ALU.mult), reads=[lr], writes=[lr])
    P.dve(I_ts(L(6), L(0), 0.0, None, ALU.max), reads=[lr], writes=[lr])
    P.dve(I_stt(L(6), L(5), 2.0, L(6), ALU.mult, ALU.add), reads=[lr], writes=[lr])
    P.dve(I_ts(lruc.ap[:, 0, :], L(6), -8.0, None, ALU.mult), reads=[lr], writes=[lruc.r()])
    P.dve(I_ts(lruc.ap[:, 1, :], L(6), -16.0, None, ALU.mult), reads=[lr], writes=[lruc.r()])

    mBc = AR.alloc("mBc", [128, 4, CTX], BF16, at=TOP - 18688 - 2048)
    hbg = sb("hbg", [128, 16])
    lrc2 = sb("lrc2", [128, 2, 8])
    P.dve(I_ts(hbg.ap, V("bgb", 0, 16), 0.5, None, ALU.mult), reads=[vec.r()], writes=[hbg.r()])
    P.dve(I_ts(lrc2.ap[:, 0, :], lruc.ap[:, 0, :], 0.5, None, ALU.mult), reads=[lruc.r()], writes=[lrc2.r()])
    P.dve(I_ts(lrc2.ap[:, 1, :], lruc.ap[:, 0, :], 256.0, None, ALU.mult), reads=[lruc.r()], writes=[lrc2.r()])
    vFs = [AR.alloc(f"vF{i}", [128, 512], F32) for i in range(5)]
    vbs = [AR.alloc(f"vb{i}", [128, 512], BF16) for i in range(5)]
    aR = Rot(AR, "lra", [128, 512], F32, 5)
    sR = Rot(AR, "lrs", [128, 512], F32, 5)
    ivR = Rot(AR, "lriv", [128, 512], F32, 5)
    hlR = Rot(AR, "lrhl", [128, 512], F32, 2)
    hcf = AR.alloc("hcf", [128, CTX], F32)
    hcr = AR.alloc("hcr", [128, CTX], F32)
    for j in range(4):
        for ci, (c0, c1) in enumerate(OC):
            w = c1 - c0
            bank = PS.next("cv", [0, 1])
            for k in range(4):
                o = ucol(c0) + k - 2
                P.pe(I_mm(bank.ap[:, 0:w], diagB.ap[:, k * 4 + j, :], uB.ap[:, j, o:o + w], k == 0, k == 3),
                     reads=[diagB.r(k * 4 + j)] + allres(uB, j), writes=[bank.r()])
            P.act(I_act(vFs[ci].ap[:, 0:w], bank.ap[:, 0:w], AF.Identity, bias=V("bcb", j)), reads=[bank.r(), vec.r()], writes=[vFs[ci].r()])
            P.pool(I_copy(vbs[ci].ap[:, 0:w], vFs[ci].ap[:, 0:w]), reads=[vFs[ci].r()], writes=[vbs[ci].r()])
        for d in range(2):
            c1h = lrc2.ap[:, 0, d * 4 + j:d * 4 + j + 1]
            c1f = lruc.ap[:, 0, d * 4 + j:d * 4 + j + 1]
            bufs = []
            for ci, (c0, c1) in enumerate(OC):
                w = c1 - c0
                W = slice(0, w)
                pr = PS.next("gt", [2, 3, 4, 5])
                pi = PS.next("gt", [2, 3, 4, 5])
                P.pe(I_mm(pr.ap[:, W], gw.ap[:, (d * 2 + 0) * 4 + j, :], vbs[ci].ap[:, W]), reads=[gw.r(), vbs[ci].r()], writes=[pr.r()])
                P.pe(I_mm(pi.ap[:, W], gw.ap[:, (d * 2 + 1) * 4 + j, :], vbs[ci].ap[:, W]), reads=[gw.r(), vbs[ci].r()], writes=[pi.r()])
                a_, s_, iv = aR.next(), sR.next(), ivR.next()
                bufs.append((a_, s_, iv))
                gi = (d * 2 + 0) * 4 + j
                if ci < 4:
                    P.act(I_act(a_.ap[:, W], pr.ap[:, W], AF.Tanh, bias=hbg.ap[:, gi:gi + 1], scale=0.5, accum_out=RS.ap[:, j, d, ci:ci + 1]),
                          reads=[pr.r(), hbg.r()], writes=[a_.r(), RS.r((j, d))])
                else:
                    P.act(I_act(a_.ap[:, W], pr.ap[:, W], AF.Tanh, bias=hbg.ap[:, gi:gi + 1], scale=0.5), reads=[pr.r(), hbg.r()], writes=[a_.r()])
                gi = (d * 2 + 1) * 4 + j
                P.act(I_act(iv.ap[:, W], pi.ap[:, W], AF.Tanh, bias=hbg.ap[:, gi:gi + 1], scale=0.5), reads=[pi.r(), hbg.r()], writes=[iv.r()])
                P.act(I_act(a_.ap[:, W], a_.ap[:, W], AF.Exp, bias=c1h, scale=c1h), reads=[a_.r(), lrc2.r()], writes=[a_.r()])
                P.pool(I_tt(s_.ap[:, W], a_.ap[:, W], a_.ap[:, W], ALU.mult), reads=[a_.r()], writes=[s_.r()])
                P.dve(I_stt(iv.ap[:, W], iv.ap[:, W], 1.0, vFs[ci].ap[:, W], ALU.add, ALU.mult), reads=[iv.r(), vFs[ci].r()], writes=[iv.r()])
            for ci, (c0, c1) in enumerate(OC):
                w = c1 - c0
                W = slice(0, w)
                a_, s_, iv = bufs[ci]
                P.act(I_act(s_.ap[:, W], s_.ap[:, W], AF.Sqrt, bias=0.25, scale=-0.25), reads=[s_.r()], writes=[s_.r()])
                b_ = iv
                P.dve(I_tt(b_.ap[:, W], iv.ap[:, W], s_.ap[:, W], ALU.mult), reads=[iv.r(), s_.r()], writes=[iv.r()])
                if ci < 4:
                    hl = hlR.next()
                    if d == 0:
                        P.dve(I_scan(hl.ap[:, W], a_.ap[:, W], b_.ap[:, W], 0.0), reads=[a_.r(), b_.r()], writes=[hl.r()])
                        end = hl.ap[:, w - 1:w]
                    else:
                        P.dve(I_scan(hl.ap[:, W][:, ::-1], a_.ap[:, W][:, ::-1], b_.ap[:, W][:, ::-1], 0.0), reads=[a_.r(), b_.r()], writes=[hl.r()])
                        end = hl.ap[:, 0:1]
                    P.dve(I_copy(Sm.ap[:, j, d, ci, 1:2], end), reads=[hl.r()], writes=[Sm.r((j, d, "h"))])
                    P.dma("sp", I_dma(abD[d * 4 + j, 0, :, ci * 512:(ci + 1) * 512], a_.ap[:, W]), reads=[a_.r()], writes=[dres(f"ab{j}{d}").r(("a", ci))])
                    P.dma("sp", I_dma(abD[d * 4 + j, 1, :, ci * 512:(ci + 1) * 512], b_.ap[:, W]), reads=[b_.r()], writes=[dres(f"ab{j}{d}").r(("b", ci))])
                else:
                    hc = hcf if d == 0 else hcr
                    if d == 0:
                        P.dve(I_scan(hc.ap, a_.ap[:, W], b_.ap[:, W], 0.0), reads=[a_.r(), b_.r()], writes=[hc.r()])
                        P.dve(I_copy(hend.ap[:, 0, j:j + 1], hc.ap[:, CTX - 1:CTX]), reads=[hc.r()], writes=[hend.r()])
                    else:
                        P.dve(I_scan(hc.ap[:, ::-1], a_.ap[:, W][:, ::-1], b_.ap[:, W][:, ::-1], 0.0), reads=[a_.r(), b_.r()], writes=[hc.r()])
                        P.dve(I_copy(hend.ap[:, 1, j:j + 1], hc.ap[:, 0:1]), reads=[hc.r()], writes=[hend.r()])
            P.act(I_act(Sm.ap[:, j, d, 0:4, 0], RS.ap[:, j, d, :], AF.Exp, bias=lrc2.ap[:, 1, d * 4 + j:d * 4 + j + 1], scale=c1h),
                  reads=[RS.r((j, d)), lrc2.r()], writes=[Sm.r((j, d, "p"))])
        P.dve(I_tt(hcf.ap, hcf.ap, hcr.ap, ALU.add), reads=[hcf.r(), hcr.r()], writes=[hcf.r()])
        P.dve(I_tt(mBc.ap[:, j, :], hcf.ap, gB.ap[:, j, CTX0:NT0], ALU.mult), reads=[hcf.r()] + allres(gB, j), writes=[mBc.r(j)])
    aR.free()
    sR.free()
    ivR.free()
    hlR.free()
    AR.free(*vFs)
    AR.free(*vbs)
    AR.free(hcf, hcr, diagB, gw, uB)

    smr = [Sm.r((j, d, x)) for j in range(4) for d in range(2) for x in ("p", "h")]
    tr_ = tiny.r()
    TV = lambda i: tiny.ap[:, i, :]
    for d in range(2):
        order = [0, 1, 2, 3] if d == 0 else [3, 2, 1, 0]
        Pc = lambda c: Sm.ap[:, :, d, c, 0]
        Hc = lambda c: Sm.ap[:, :, d, c, 1]
        Hs = summ.ap[:, d * 8 + 4:d * 8 + 8]
        Ps = summ.ap[:, d * 8:d * 8 + 4]
        P.dve(I_copy(Hs, Hc(order[0])), reads=smr, writes=[summ.r()])
        P.dve(I_copy(Ps, Pc(order[0])), reads=smr, writes=[summ.r()])
        for c in order[1:]:
            P.dve(I_tt(Hs, Hs, Pc(c), ALU.mult), reads=smr + [summ.r()], writes=[summ.r()])
            P.dve(I_tt(Hs, Hs, Hc(c), ALU.add), reads=smr + [summ.r()], writes=[summ.r()])
            P.dve(I_tt(Ps, Ps, Pc(c), ALU.mult), reads=smr + [summ.r()], writes=[summ.r()])
    P.dma("sp", I_dma(cc_src[:, :], summ.ap), reads=[summ.r()], writes=[dres("ccs").r()])
    P.add("pool", lambda e: e.collective_compute("AllGather", ALU.bypass, replica_groups=groups, ins=[cc_src[:, :]], outs=[cc_dst[:, :]]),
          reads=[dres("ccs").r()], writes=[dres("ccd").r()], kind="cc")
    P.dma("sp", I_dma(gath.ap, cc_dst.rearrange("(r p) n -> p r n", p=128)), reads=[dres("ccd").r()], writes=[gath.r()])
    diagA = AR.alloc("diagA", [128, 124, 128], BF16)
    for k in range(31):
        for j in range(4):
            P.dve(I_ts(diagA.ap[:, k * 4 + j, :], identb.ap, V("adw_w", k * 4 + j), None, ALU.mult),
                  reads=[identb.r(), vec.r()], writes=[diagA.r(k * 4 + j)])
    mA = AR.alloc("mA", [128, 4, TOK + CTX], BF16, at=TOP - 18688 - 2048 - 18944 - 18432)
    cf = AR.alloc("cf", [128, 4, 512], F32)
    cb = AR.alloc("cb", [128, 4, 512], BF16)
    csq = AR.alloc("csq", [128, 4, 512], BF16)
    mean = AR.alloc("mean", [128, 512], F32)
    m2 = AR.alloc("m2", [128, 512], F32)
    rstdA = AR.alloc("rstdA", [128, 512], F32)
    tln = Rot(AR, "tln", [128, 512], F32, 1)
    for ci, (c0, c1) in enumerate(OC):
        w = c1 - c0
        W = slice(0, w)
        for j in range(4):
            bank = PS.b[j]
            for k in range(31):
                o = ucol(c0) + k - 15
                P.pe(I_mm(bank.ap[:, W], diagA.ap[:, k * 4 + j, :], uA.ap[:, j, o:o + w], k == 0, k == 30),
                     reads=[diagA.r(k * 4 + j)] + allres(uA, j), writes=[bank.r()])
            P.act(I_act(cf.ap[:, j, W], bank.ap[:, W], AF.Identity, bias=V("adw_b", j)), reads=[bank.r(), vec.r()], writes=[cf.r(j)])
            P.dve(I_copy(cb.ap[:, j, W], cf.ap[:, j, W]), reads=[cf.r(j)], writes=[cb.r(j)])
            P.act(I_act(csq.ap[:, j, W], cf.ap[:, j, W], AF.Square), reads=[cf.r(j)], writes=[csq.r(j)])
        bs, bq = PS.b[4], PS.b[5]
        for j in range(4):
            P.pe(I_mm(bs.ap[:, W], onesb.ap, cb.ap[:, j, W], j == 0, j == 3), reads=[cb.r(j), onesb.r()], writes=[bs.r()])
        for j in range(4):
            P.pe(I_mm(bq.ap[:, W], onesb.ap, csq.ap[:, j, W], j == 0, j == 3), reads=[csq.r(j), onesb.r()], writes=[bq.r()])
        P.act(I_act(mean.ap[:, W], bs.ap[:, W], AF.Copy, scale=1.0 / 512), reads=[bs.r()], writes=[mean.r()])
        P.dve(I_tt(m2.ap[:, W], mean.ap[:, W], mean.ap[:, W], ALU.mult), reads=[mean.r()], writes=[m2.r()])
        P.dve(I_stt(m2.ap[:, W], bq.ap[:, W], 1.0 / 512, m2.ap[:, W], ALU.mult, ALU.subtract), reads=[bq.r(), m2.r()], writes=[m2.r()])
        P.dve(I_ts(m2.ap[:, W], m2.ap[:, W], 0.0, None, ALU.max), reads=[m2.r()], writes=[m2.r()])
        P.act(I_act(m2.ap[:, W], m2.ap[:, W], AF.Sqrt, bias=EPS), reads=[m2.r()], writes=[m2.r()])
        P.dve(I_recip(rstdA.ap[:, W], m2.ap[:, W]), reads=[m2.r()], writes=[rstdA.r()])
        for j in range(4):
            t = tln.next()
            P.dve(I_tt(t.ap[:, W], cf.ap[:, j, W], mean.ap[:, W], ALU.subtract), reads=[cf.r(j), mean.r()], writes=[t.r()])
            P.dve(I_tt(t.ap[:, W], t.ap[:, W], rstdA.ap[:, W], ALU.mult), reads=[t.r(), rstdA.r()], writes=[t.r()])
            P.act(I_act(mA.ap[:, j, mcol(c0):mcol(c0) + w], t.ap[:, W], AF.Silu, bias=V("aln_b", j), scale=V("aln_g", j)),
                  reads=[t.r(), vec.r()], writes=[mA.r((j, ci))])
        if ci < 4:
            emit_mod(0, [6 + ci])
    emit_scl(0, ["n2"])
    tln.free()
    AR.free(diagA, cf, cb, csq, mean, m2, rstdA, uA)

    hr_ = Hin.r()
    for d in range(2):
        qs = [0, 1, 2, 3] if d == 0 else [3, 2, 1, 0]
        Hc_ = TV(d)
        Hi = Hin.ap[:, d, :]
        Pq = lambda q: gath.ap[:, q, d * 8:d * 8 + 4]
        Hq = lambda q: gath.ap[:, q, d * 8 + 4:d * 8 + 8]
        P.dve(I_copy(Hc_, hend.ap[:, d, :]), reads=[hend.r()], writes=[tr_])
        P.dve(I_ts(Hi, Hc_, V("flags", 2 + qs[0]), None, ALU.mult), reads=[tr_, vec.r()], writes=[hr_])
        for n in range(3):
            q = qs[n]
            P.dve(I_tt(Hc_, Hc_, Pq(q), ALU.mult), reads=[tr_, gath.r()], writes=[tr_])
            P.dve(I_tt(Hc_, Hc_, Hq(q), ALU.add), reads=[tr_, gath.r()], writes=[tr_])
            P.dve(I_stt(Hi, Hc_, V("flags", 2 + qs[n + 1]), Hi, ALU.mult, ALU.add), reads=[tr_, hr_, vec.r()], writes=[hr_])

    wo = AR.alloc("wo", [128, 8, D], BF16)
    P.dma("pool", I_dma(wo.ap, dr["w_out"][0].rearrange("(k p) n -> p k n", p=128)), writes=[wo.r()])
    mB = AR.alloc("mB", [128, 4, TOK], BF16)
    lar = Rot(AR, "la", [128, TOK], F32, 2)
    lbr = Rot(AR, "lb", [128, TOK], F32, 2)
    yb = AR.alloc("yb", [128, TOK], F32)
    hs = AR.alloc("hs", [128, TOK], F32)
    for j in range(4):
        for d in range(2):
            la = lar.next()
            lb = lbr.next()
            abr = dres(f"ab{j}{d}")
            P.dma("sp", I_dma(la.ap, abD[d * 4 + j, 0, :, :]), reads=[abr.r(("a", c)) for c in range(4)], writes=[la.r()])
            P.dma("sp", I_dma(lb.ap, abD[d * 4 + j, 1, :, :]), reads=[abr.r(("b", c)) for c in range(4)], writes=[lb.r()])
            init = Hin.ap[:, d, j:j + 1]
            if d == 0:
                P.dve(I_scan(yb.ap, la.ap, lb.ap, init), reads=[la.r(), lb.r(), hr_], writes=[yb.r()])
            else:
                P.dve(I_scan(hs.ap[:, ::-1], la.ap[:, ::-1], lb.ap[:, ::-1], init), reads=[la.r(), lb.r(), hr_], writes=[hs.r()])
        P.pool(I_tt(yb.ap, yb.ap, hs.ap, ALU.add), reads=[yb.r(), hs.r()], writes=[yb.r()])
        P.dve(I_tt(mB.ap[:, j, :], yb.ap, gB.ap[:, j, OWN0:OWN0 + TOK], ALU.mult), reads=[yb.r()] + allres(gB, j), writes=[mB.r(j)])
    lar.free()
    lbr.free()
    AR.free(yb, hs, gB)

    def out_proj(l, wo, rhs_of, chunks):
        for ci, (c0, c1, t) in enumerate(chunks):
            for dk in range(8):
                w = c1 - c0
                bank = PS.next("op", [0, 1, 2, 3])
                for mk in range(8):
                    rap, rres = rhs_of(mk, ci)
                    P.pe(I_mm(bank.ap[:, 0:w], wo.ap[:, mk, dk * 128:(dk + 1) * 128], rap, mk == 0, mk == 7), reads=[wo.r()] + rres, writes=[bank.r()])
                P.dve(I_stt(hT.ap[:, dk, c0:c1], bank.ap[:, 0:w], scl.ap[:, l, 2, t, dk:dk + 1], hT.ap[:, dk, c0:c1], ALU.mult, ALU.add),
                      reads=[bank.r(), scl.r((l, 2, t))] + hres(dk, c0, c1), writes=hres(dk, c0, c1))

    def rhs0(mk, ci):
        c0, c1 = OC[ci]
        w = c1 - c0
        if mk < 4:
            return mA.ap[:, mk, mcol(c0):mcol(c0) + w], [mA.r((mk, ci))]
        if ci < 4:
            return mB.ap[:, mk - 4, mcol(c0):mcol(c0) + w], [mB.r(mk - 4)]
        return mBc.ap[:, mk - 4, :], [mBc.r(mk - 4)]

    out_proj(0, wo, rhs0, [(c0, c1, 0 if c0 < CTX0 else 1) for (c0, c1) in OC])
    AR.free(wo, mA, mB, mBc)

    def emit_moe(l, chunks, mod_hook=None, expert_hook=None, chunk_done_hook=None):
        ncol = sum(c1 - c0 for c0, c1, _ in chunks)
        ntile = ncol // 128
        n2T = AR.alloc("n2T", [128, 8, ncol], BF16)
        rl = PS.b[6]
        P.pe(I_mm(rl.ap[:, 0:288], zer.ap[:, 0:128], zer.ap[:, 0:288], True, False, skip=True), reads=[zer.r()], writes=[rl.r()])
        nch = []
        tile0 = []
        n0 = 0
        for (c0, c1, t) in chunks:
            nch.append((c0, c1, n0, t))
            tile0.append(n0 // 128)
            n0 += c1 - c0
        emit_norm(l, 2, [hchunk(c0, c1, n2T, n0_, t, ci) for ci, (c0, c1, n0_, t) in enumerate(nch)], router=dict(ps=rl, tile0=tile0))
        wslots = [None, None]

        def load_expert(e):
            g = AR.alloc(f"wg{e % 2}", [128, 8, 512], BF16)
            u = AR.alloc(f"wu{e % 2}", [128, 8, 512], BF16)
            dn = AR.alloc(f"wd{e % 2}", [128, 4, D], BF16)
            P.dma("pool", I_dma(g.ap, dr["wg"][l, e].rearrange("(k p) n -> p k n", p=128)), writes=[g.r()])
            P.dma("pool", I_dma(u.ap, dr["wu"][l, e].rearrange("(k p) n -> p k n", p=128)), writes=[u.r()])
            P.dma("pool", I_dma(dn.ap, dr["wd"][l, e].rearrange("(k p) n -> p k n", p=128)), writes=[dn.r()])
            wslots[e % 2] = (g, u, dn)

        load_expert(0)
        load_expert(1)
        NT_ = ntile
        R = lambda name: AR.alloc(name, [128, 288], F32)
        sc, sel, tA_, tB_, gsc, keep = R("r_sc"), R("r_sel"), R("r_ta"), R("r_tb"), R("r_gs"), R("r_keep")
        NE = NT_ * 16
        NG = NT_ * 4
        P.act(I_act(sc.ap[:, 0:NE], rl.ap[:, 0:NE], AF.Sigmoid), reads=[rl.r()], writes=[sc.r()])
        P.dve(I_tt(sel.ap[:, 0:NE], sc.ap[:, 0:NE], rb18.ap[:, 0:NE], ALU.add), reads=[sc.r(), rb18.r()], writes=[sel.r()])
        X = lambda e_: sel.ap[:, 0:NE].rearrange("p (g e) -> p g e", e=4)[:, :, e_]
        pairs = [(0, 1), (0, 2), (0, 3), (1, 2), (1, 3), (2, 3)]
        ta = tA_.ap[:, 0:NG]
        tb = tB_.ap[:, 0:NG]
        gs_ = gsc.ap[:, 0:NG]
        thr = gsc.ap[:, NG:2 * NG]
        for pi_, (a, b) in enumerate(pairs):
            P.dve(I_tt(ta, X(a), X(b), ALU.add), reads=[sel.r()], writes=[tA_.r()])
            P.dve(I_tt(tb, X(a), X(b), ALU.min), reads=[sel.r()], writes=[tB_.r()])
            if pi_ == 0:
                P.dve(I_copy(gs_, ta), reads=[tA_.r()], writes=[gsc.r("g")])
                P.dve(I_copy(thr, tb), reads=[tB_.r()], writes=[gsc.r("t")])
            else:
                P.dve(I_tt(gs_, gs_, ta, ALU.max), reads=[tA_.r(), gsc.r("g")], writes=[gsc.r("g")])
                P.dve(I_tt(thr, thr, tb, ALU.max), reads=[tB_.r(), gsc.r("t")], writes=[gsc.r("t")])
        G = lambda g_: gsc.ap[:, 0:NG].rearrange("p (t g) -> p t g", g=4)[:, :, g_]
        gm = tA_.ap[:, 0:NT_]
        P.dve(I_tt(gm, G(0), G(1), ALU.max), reads=[gsc.r("g")], writes=[tA_.r()])
        P.dve(I_tt(gm, gm, G(2), ALU.max), reads=[gsc.r("g"), tA_.r()], writes=[tA_.r()])
        P.dve(I_tt(gm, gm, G(3), ALU.max), reads=[gsc.r("g"), tA_.r()], writes=[tA_.r()])
        ing = tB_.ap[:, 0:NG]
        for g_ in range(4):
            P.dve(I_tt(ing.rearrange("p (t g) -> p t g", g=4)[:, :, g_], G(g_), gm, ALU.is_equal), reads=[gsc.r("g"), tA_.r()], writes=[tB_.r()])
        K = lambda e_: keep.ap[:, 0:NE].rearrange("p (g e) -> p g e", e=4)[:, :, e_]
        for e_ in range(4):
            P.dve(I_tt(K(e_), X(e_), thr, ALU.is_ge), reads=[sel.r(), gsc.r("t")], writes=[keep.r()])
            P.dve(I_tt(K(e_), K(e_), ing, ALU.mult), reads=[keep.r(), tB_.r()], writes=[keep.r()])
        P.dve(I_tt(keep.ap[:, 0:NE], keep.ap[:, 0:NE], sc.ap[:, 0:NE], ALU.mult), reads=[keep.r(), sc.r()], writes=[keep.r()])
        den = tA_.ap[:, 64:64 + NT_]
        P.dve(I_red(den, keep.ap[:, 0:NE].rearrange("p (t e) -> p t e", e=16), ALU.add), reads=[keep.r()], writes=[tA_.r()])
        P.dve(I_recip(den, den), reads=[tA_.r()], writes=[tA_.r()])
        comb = sel
        for t_ in range(NT_):
            P.dve(I_ts(comb.ap[:, t_ * 16:(t_ + 1) * 16], keep.ap[:, t_ * 16:(t_ + 1) * 16], tA_.ap[:, 64 + t_:65 + t_], None, ALU.mult),
                  reads=[keep.r(), tA_.r()], writes=[sel.r()])
        chi = AR.alloc("chi", [128, 288], BF16)
        clo = AR.alloc("clo", [128, 288], BF16)
        P.dve(I_copy(chi.ap[:, 0:NE], comb.ap[:, 0:NE]), reads=[sel.r()], writes=[chi.r()])
        P.dve(I_tt(clo.ap[:, 0:NE], comb.ap[:, 0:NE], chi.ap[:, 0:NE], ALU.subtract), reads=[sel.r(), chi.r()], writes=[clo.r()])
        cT = [AR.alloc("cThi", [128, ncol], BF16), AR.alloc("cTlo", [128, ncol], BF16)]
        for hi_, src in enumerate((chi, clo)):
            t_ = 0
            while t_ < NT_:
                nb = min(8, NT_ - t_)
                bank = PS.next("ct", [4, 5])
                bap = bank.ap.bitcast(BF16)
                for q in range(nb):
                    P.pe(I_tr(bap[0:16, q * 128:(q + 1) * 128], src.ap[:, (t_ + q) * 16:(t_ + q + 1) * 16], identb.ap),
                         reads=[src.r(), identb.r()], writes=[bank.r()])
                P.act(I_act(cT[hi_].ap[0:16, t_ * 128:(t_ + nb) * 128], bap[0:16, 0:nb * 128], AF.Copy), reads=[bank.r()], writes=[cT[hi_].r()])
                t_ += nb
        AR.free(sc, sel, tA_, tB_, gsc, keep, chi, clo)
        if mod_hook is not None:
            mod_hook()
        sgr = Rot(AR, "sg", [128, 512], F32, 2)
        t1r = Rot(AR, "t1", [128, 512], F32, 2)
        hidr = Rot(AR, "hid", [128, 4, 512], BF16, 2)
        for e in range(16):
            g, u, dn = wslots[e % 2]
            n0s = []
            n0 = 0
            for (c0, c1, t) in chunks:
                n0s.append(n0)
                n0 += c1 - c0
            corder = list(range(len(chunks)))
            if e == 15 and len(chunks) >= 4:
                corder = [0, 3, 1, 2] + corder[4:]
            for cidx in corder:
                c0, c1, t = chunks[cidx]
                n0 = n0s[cidx]
                w = c1 - c0
                W = slice(0, w)
                cwb = PS.b[6]
                P.pe(I_mm(cwb.ap[:, W], selm.ap[:, e, :], cT[0].ap[0:16, n0:n0 + w], True, False), reads=[selm.r(), cT[0].r()], writes=[cwb.r()])
                P.pe(I_mm(cwb.ap[:, W], selm.ap[:, e, :], cT[1].ap[0:16, n0:n0 + w], False, True), reads=[selm.r(), cT[1].r()], writes=[cwb.r()])
                hid = hidr.next()
                for f in range(4):
                    pg = PS.next("gu", [0, 1, 2, 3])
                    pu = PS.next("gu", [0, 1, 2, 3])
                    for k in range(8):
                        P.pe(I_mm(pg.ap[:, W], g.ap[:, k, f * 128:(f + 1) * 128], n2T.ap[:, k, n0:n0 + w], k == 0, k == 7),
                             reads=[g.r(), n2T.r((k, chunks.index((c0, c1, t))))], writes=[pg.r()])
                    for k in range(8):
                        P.pe(I_mm(pu.ap[:, W], u.ap[:, k, f * 128:(f + 1) * 128], n2T.ap[:, k, n0:n0 + w], k == 0, k == 7),
                             reads=[u.r(), n2T.r((k, chunks.index((c0, c1, t))))], writes=[pu.r()])
                    sg = sgr.next()
                    t1 = t1r.next()
                    P.act(I_act(sg.ap[:, W], pg.ap[:, W], AF.Silu), reads=[pg.r()], writes=[sg.r()])
                    P.dve(I_tt(t1.ap[:, W], pu.ap[:, W], sg.ap[:, W], ALU.mult), reads=[pu.r(), sg.r()], writes=[t1.r()])
                    P.dve(I_tt(hid.ap[:, f, W], t1.ap[:, W], cwb.ap[:, W], ALU.mult), reads=[t1.r(), cwb.r()], writes=[hid.r(f)])
                for dk in range(8):
                    pd = PS.next("dn", [4, 5])
                    for f in range(4):
                        P.pe(I_mm(pd.ap[:, W], dn.ap[:, f, dk * 128:(dk + 1) * 128], hid.ap[:, f, W], f == 0, f == 3),
                             reads=[dn.r(), hid.r(f)], writes=[pd.r()])
                    P.dve(I_stt(hT.ap[:, dk, c0:c1], pd.ap[:, W], scl.ap[:, l, 5, t, dk:dk + 1], hT.ap[:, dk, c0:c1], ALU.mult, ALU.add),
                          reads=[pd.r(), scl.r((l, 5, t))] + hres(dk, c0, c1), writes=hres(dk, c0, c1))
                if e == 15 and chunk_done_hook is not None:
                    chunk_done_hook(cidx)
            AR.free(g, u, dn)
            if e + 2 < 16:
                load_expert(e + 2)
            if expert_hook is not None:
                expert_hook(e)
        sgr.free()
        t1r.free()
        hidr.free()
        AR.free(n2T, cT[0], cT[1])


    def hook0():
        emit_mod(0, range(10, 12))
        emit_scl(0, ["g2"])

    def ehook0(e):
        if 1 <= e <= 12:
            emit_mod(1, [e - 1])

    emit_moe(0, [(c0, c1, 0 if c0 < CTX0 else 1) for (c0, c1) in OC], mod_hook=hook0, expert_hook=ehook0)
    emit_scl(1, ["n1", "g1", "n2", "g2"])
    cut("l0")

    NH = 448
    w1 = dr["w_in1"]

    def load_w_at(name, segs, src, at):
        ncols = sum(n for _, n in segs)
        wb = AR.alloc(name, [128, 8, ncols], BF16, at=at)
        o = 0
        for si, (c0, n) in enumerate(segs):
            P.dma("pool", I_dma(wb.ap[:, :, o:o + n], src[:, c0:c0 + n].rearrange("(k p) n -> p k n", p=128)), writes=[wb.r(si)])
            o += n
        return wb

    wbs = [load_w_at(f"wC{p}", [(2 * p * 128, 256), (1024 + 2 * p * 128, 256)], w1, ARENA_BYTES - 16384 - 16384 + 8192 * p) for p in range(2)]
    hxd_views = []
    for half in range(2):
        hx_src = nc.dram_tensor(f"hx_src{half}", [128, 4 * NH], F32, kind="Internal").ap()
        hx_dst = nc.dram_tensor(f"hx_dst{half}", [4 * 128, 4 * NH], F32, kind="Internal").ap()
        rdh0, rdh1 = [], []
        for k in range(4 * half, 4 * half + 4):
            rdh0 += hres(k, OWN0, OWN0 + 192)
            rdh1 += hres(k, OWN0 + TOK - 256, OWN0 + TOK)
        hxs = hx_src.rearrange("p (k n) -> p k n", k=4)
        ks = slice(4 * half, 4 * half + 4)
        P.dma("sp", I_dma(hxs[:, :, 0:192], hT.ap[:, ks, OWN0:OWN0 + 192]), reads=rdh0, writes=[dres(f"hxs{half}").r(0)])
        P.dma("sp", I_dma(hxs[:, :, 192:448], hT.ap[:, ks, OWN0 + TOK - 256:OWN0 + TOK]), reads=rdh1, writes=[dres(f"hxs{half}").r(1)])
        P.add("pool", (lambda e, a_=hx_src, b_=hx_dst: e.collective_compute("AllGather", ALU.bypass, replica_groups=groups, ins=[a_[:, :]], outs=[b_[:, :]])),
              reads=[dres(f"hxs{half}").r(0), dres(f"hxs{half}").r(1)], writes=[dres(f"hxd{half}").r()], kind="cc")
        hxd_views.append(hx_dst.rearrange("(r p) (k n) -> p r k n", r=4, k=4))

    NT1 = 2752
    nTo = AR.alloc("nTo", [128, 8, TOK], BF16, at=0)
    nTx = AR.alloc("nTx", [128, 8, 704], BF16, at=32768)
    hH = AR.alloc("hH", [128, 8, NH], F32)

    def ncol(c):
        if c < 256:
            return nTx, c
        if c < 2304:
            return nTo, c - 256
        if c < 2496:
            return nTx, 256 + (c - 2304)
        return nTx, 448 + (c - 2496)

    def nslice(k, c0, c1):
        b, o = ncol(c0)
        return b.ap[:, k, o:o + (c1 - c0)], b.r((k, c0))

    L1CH = [(256 + 512 * c, 256 + 512 * (c + 1)) for c in range(4)] + [(2496, 2752)]
    chs = []
    for (c0, c1) in L1CH:
        if c0 < 2496:
            h0 = OWN0 + (c0 - 256)
            tt = 0
        else:
            h0 = CTX0
            tt = 1
        b, o = ncol(c0)
        chs.append(dict(w=c1 - c0, src3=hT.ap[:, :, h0:h0 + (c1 - c0)], srck=(lambda k, h0=h0, w=c1 - c0: hT.ap[:, k, h0:h0 + w]),
                        rres=(lambda k, h0=h0, w=c1 - c0: hres(k, h0, h0 + w)),
                        dst=(lambda k, b=b, o=o, w=c1 - c0: b.ap[:, k, o:o + w]), dres=(lambda k, b=b, c0=c0: b.r((k, c0))), t=tt))
    emit_norm(1, 1, chs)
    def emit_halo():
        cand = Rot(AR, "cand", [128, 2, NH], F32, 2)
        for k in range(8):
            top = hH.ap[:, k, 0:256]
            bot = hH.ap[:, k, 256:448]
            for hf in range(2):
                cb_ = cand.next()
                P.dma("sp", I_dma(cb_.ap, hxd_views[k // 4][:, 2 * hf:2 * hf + 2, k % 4, :]), reads=[dres(f"hxd{k // 4}").r()], writes=[cb_.r()])
                for rr in range(2):
                    r = 2 * hf + rr
                    if r == 0:
                        P.dve(I_ts(top, cb_.ap[:, rr, 192:448], V("flags2", r), None, ALU.mult), reads=[cb_.r(), vec.r()], writes=[hH.r(k)])
                        P.dve(I_ts(bot, cb_.ap[:, rr, 0:192], V("flags2", 4 + r), None, ALU.mult), reads=[cb_.r(), vec.r()], writes=[hH.r(k)])
                    else:
                        P.dve(I_stt(top, cb_.ap[:, rr, 192:448], V("flags2", r), top, ALU.mult, ALU.add), reads=[cb_.r(), vec.r(), hH.r(k)], writes=[hH.r(k)])
                        P.dve(I_stt(bot, cb_.ap[:, rr, 0:192], V("flags2", 4 + r), bot, ALU.mult, ALU.add), reads=[cb_.r(), vec.r(), hH.r(k)], writes=[hH.r(k)])
        cand.free()
        cut("hx")
        chs = []
        for (c0, c1, s0) in [(0, 256, 0), (2304, 2496, 256)]:
            b, o = ncol(c0)
            chs.append(dict(w=c1 - c0, src3=hH.ap[:, :, s0:s0 + (c1 - c0)], srck=(lambda k, s0=s0, w=c1 - c0: hH.ap[:, k, s0:s0 + w]),
                            rres=(lambda k: [hH.r(k)]),
                            dst=(lambda k, b=b, o=o, w=c1 - c0: b.ap[:, k, o:o + w]), dres=(lambda k, b=b, c0=c0: b.r((k, c0))), t=0))
        emit_norm(1, 1, chs, wmax=256)
        AR.free(hH)

    ALLCH = L1CH[:4] + [(2496, 2752), (0, 256), (2304, 2496)]
    def proj1(wb, seg, wc0, c0, c1, group="proj", banks=(0, 1, 2, 3)):
        bank = PS.next(group, list(banks))
        w = c1 - c0
        for k in range(8):
            ap, rs = nslice(k, c0, c1)
            P.pe(I_mm(bank.ap[:, 0:w], wb.ap[:, k, wc0:wc0 + 128], ap, k == 0, k == 7), reads=[wb.r(seg), rs], writes=[bank.r()])
        return bank

    tC = AR.alloc("tC", [128, 4, 2496], BF16)
    tmpC = Rot(AR, "tmpC", [128, 512], F32, 3)
    CCH = L1CH[:4] + [(0, 256), (2304, 2496)]
    for part in range(2):
        if part == 1:
            emit_halo()
        for p in range(2):
            wb = wbs[p]
            for (c0, c1) in (CCH[:4] if part == 0 else CCH[4:]):
                w = c1 - c0
                for jj in range(2):
                    j = 2 * p + jj
                    px = proj1(wb, 0, jj * 128, c0, c1)
                    pc = proj1(wb, 1, 256 + jj * 128, c0, c1)
                    sg = tmpC.next()
                    P.act(I_act(sg.ap[:, 0:w], pc.ap[:, 0:w], AF.Copy), reads=[pc.r()], writes=[sg.r()])
                    P.dve(I_tt(tC.ap[:, j, c0:c1], px.ap[:, 0:w], sg.ap[:, 0:w], ALU.mult), reads=[px.r(), sg.r()], writes=[tC.r((j, c0))])
    AR.free(*wbs)
    for j in range(4):
        P.dve(I_ts(tC.ap[:, j, 255:256], tC.ap[:, j, 255:256], V("flags", 0), None, ALU.mult), reads=[tC.r((j, 0)), vec.r()], writes=[tC.r((j, 0))])
        P.dve(I_ts(tC.ap[:, j, 2304:2305], tC.ap[:, j, 2304:2305], V("flags", 1), None, ALU.mult), reads=[tC.r((j, 2304)), vec.r()], writes=[tC.r((j, 2304))])
    mC = AR.alloc("mC", [128, 4, TOK], BF16, at=ARENA_BYTES - 16384)
    wb = load_w("wCb", [(512, 512)], w1)
    tcall = lambda j: [tC.r((j, c0)) for (c0, _) in CCH]
    for j in range(4):
        for (c0, c1) in L1CH[:4]:
            w = c1 - c0
            pb = proj1(wb, 0, j * 128, c0, c1)
            cv = tmpC.next()
            P.dve(I_ts(cv.ap[:, 0:w], tC.ap[:, j, c0 - 1:c1 - 1], V("ccw", 0 * 4 + j), None, ALU.mult), reads=tcall(j) + [vec.r()], writes=[cv.r()])
            P.dve(I_stt(cv.ap[:, 0:w], tC.ap[:, j, c0:c1], V("ccw", 1 * 4 + j), cv.ap[:, 0:w], ALU.mult, ALU.add), reads=tcall(j) + [vec.r(), cv.r()], writes=[cv.r()])
            P.dve(I_stt(cv.ap[:, 0:w], tC.ap[:, j, c0 + 1:c1 + 1], V("ccw", 2 * 4 + j), cv.ap[:, 0:w], ALU.mult, ALU.add), reads=tcall(j) + [vec.r(), cv.r()], writes=[cv.r()])
            P.dve(I_tt(mC.ap[:, j, c0 - 256:c1 - 256], pb.ap[:, 0:w], cv.ap[:, 0:w], ALU.mult), reads=[pb.r(), cv.r()], writes=[mC.r((j, c0))])
    AR.free(wb, tC)
    tmpC.free()

    KT = AR.alloc("KT", [128, 4, NT1], BF16)
    Vt = AR.alloc("Vt", [128, 22, 512], BF16)
    wb = load_w("wK", [(2048, 512)], w1)
    for j in range(4):
        for (c0, c1) in ALLCH:
            w = c1 - c0
            pk = proj1(wb, 0, j * 128, c0, c1)
            if (j + c0 // 256) % 2 == 0:
                P.act(I_act(KT.ap[:, j, c0:c1], pk.ap[:, 0:w], AF.Copy), reads=[pk.r()], writes=[KT.r((j, c0))])
            else:
                P.dve(I_copy(KT.ap[:, j, c0:c1], pk.ap[:, 0:w]), reads=[pk.r()], writes=[KT.r((j, c0))])
    AR.free(wb)
    wb = load_w("wV", [(2560, 512)], w1)
    vtiles = [(128 * m, min(128, 2496 - 128 * m)) for m in range(20)] + [(2496, 128), (2624, 128)]

    def chunk_of(c):
        for (c0, c1) in ALLCH:
            if c0 <= c < c1:
                return c0
        raise ValueError(c)

    vorder = [ti for ti, (c0, n) in enumerate(vtiles) if 256 <= c0 < 2304 or c0 >= 2496] + \
             [ti for ti, (c0, n) in enumerate(vtiles) if not (256 <= c0 < 2304 or c0 >= 2496)]
    for ti in vorder:
        c0, n = vtiles[ti]
        bank = PS.next("proj", [0, 1, 2, 3])
        for k in range(8):
            ap, _ = nslice(k, c0, c0 + n)
            b_, _o = ncol(c0)
            P.pe(I_mm(bank.ap[0:n, 0:512], ap, wb.ap[:, k, 0:512], k == 0, k == 7), reads=[wb.r(0), b_.r((k, chunk_of(c0)))], writes=[bank.r()])
        if ti % 2 == 0:
            P.act(I_act(Vt.ap[0:n, ti, :], bank.ap[0:n, 0:512], AF.Copy), reads=[bank.r()], writes=[Vt.r(ti)])
        else:
            P.dve(I_copy(Vt.ap[0:n, ti, :], bank.ap[0:n, 0:512]), reads=[bank.r()], writes=[Vt.r(ti)])
    AR.free(wb, nTx)
    QT = AR.alloc("QT", [128, 4, TOK], BF16)
    wb = load_w("wQ", [(1536, 512)], w1)
    for j in range(4):
        for (c0, c1) in L1CH[:4]:
            w = c1 - c0
            pq = proj1(wb, 0, j * 128, c0, c1)
            if (j + c0 // 512) % 2 == 0:
                P.act(I_act(QT.ap[:, j, c0 - 256:c1 - 256], pq.ap[:, 0:w], AF.Copy), reads=[pq.r()], writes=[QT.r((j, (c0 - 256) // 128 + mm)) for mm in range(4)])
            else:
                P.dve(I_copy(QT.ap[:, j, c0 - 256:c1 - 256], pq.ap[:, 0:w]), reads=[pq.r()], writes=[QT.r((j, (c0 - 256) // 128 + mm)) for mm in range(4)])
    AR.free(wb, nTo)
    cut("l1a")

    din_ug = dr["ug"]
    din_eg = dr["eg"]
    UG = AR.alloc("UG", [128, 8, 576], F32)
    P.dma("sp", I_dma(UG.ap, din_ug.rearrange("h p n -> p h n")), writes=[UG.r()])
    Sbr = Rot(AR, "Sb", [128, 1024], F32, 2)
    Pbr = Rot(AR, "Pb", [128, 1024], BF16, 2)
    PTr = Rot(AR, "PT", [128, 1024], BF16, 2)
    EGr = Rot(AR, "EGb", [128, 768], F32, 2)
    mdt = Rot(AR, "mdt", [128, 512], BF16, 2)
    stat = Rot(AR, "stat", [128, 8, 4], F32, 2)
    special = {0: (0, 768, 0), 1: (128, 640, 1), 15: (1792, 704, 3)}
    units = [(m, h) for m in range(16) for h in range(8)]
    ctxs = {}
    ctxa = {}
    pair_st = {}

    def pair_info(m):
        if m in special:
            return special[m]
        return 128 * m, 576, (2 if m == 14 else None)

    def stage_a(u):
        m, h = units[u]
        kc0, nloc, egi = pair_info(m)
        ntot = nloc + 256
        if m not in pair_st:
            pair_st[m] = stat.next()
        st = pair_st[m]
        jh, hp = h // 2, h % 2
        prt = slice(hp * 64, hp * 64 + 64)
        bi = 2 * (u % 2)
        sb0, sb1 = PS.b[bi], PS.b[bi + 1]
        sres = [sb0.r(), sb1.r()]
        S2 = PS.t[:, bi:bi + 2, :].rearrange("p a b -> p (a b)")
        q_ap = QT.ap[prt, jh, 128 * m:128 * m + 128]
        qr = QT.r((jh, m))
        kr = [KT.r((jh, c0)) for (c0, c1) in ALLCH if c0 < kc0 + nloc and kc0 < c1]
        P.pe(I_mm(sb0.ap[:, 0:512], q_ap, KT.ap[prt, jh, kc0:kc0 + 512]), reads=[qr] + kr, writes=[sres[0]])
        P.pe(I_mm(sb1.ap[:, 0:nloc - 512], q_ap, KT.ap[prt, jh, kc0 + 512:kc0 + nloc]), reads=[qr] + kr, writes=[sres[1]])
        P.pe(I_mm(sb1.ap[:, nloc - 512:nloc - 512 + 256], q_ap, KT.ap[prt, jh, 2496:2752]), reads=[qr, KT.r((jh, 2496))], writes=[sres[1]])
        Sb = Sbr.next()
        if egi is None:
            bias_ap, bias_r = UG.ap[:, h, 0:nloc], [UG.r()]
        else:
            eb = EGr.next()
            P.dma("sp", I_dma(eb.ap[:, 0:nloc], din_eg[egi, h, :, 0:nloc]), writes=[eb.r()])
            bias_ap, bias_r = eb.ap[:, 0:nloc], [eb.r()]
        P.dve(I_stt(Sb.ap[:, 0:nloc], S2[:, 0:nloc], 0.125, bias_ap, ALU.mult, ALU.add), reads=sres + bias_r, writes=[Sb.r()])
        P.act(I_act(Sb.ap[:, nloc:ntot], S2[:, nloc:ntot], AF.Copy, scale=0.125), reads=sres, writes=[Sb.r()])
        P.dve(lambda e, o=st.ap[:, h, 0:1], i=Sb.ap[:, 0:ntot]: e.tensor_reduce(out=o, in_=i, axis=AX.X, op=ALU.max, negate=True),
              reads=[Sb.r()], writes=[st.r(h)])
        ctxa[u] = (Sb, st, ntot)

    def stage_a2(u):
        m, h = units[u]
        Sb, st, ntot = ctxa.pop(u)
        Pb = Pbr.next()
        P.act(I_act(Pb.ap[:, 0:ntot], Sb.ap[:, 0:ntot], AF.Exp, bias=st.ap[:, h, 0:1], accum_out=st.ap[:, h, 1:2]),
              reads=[Sb.r(), st.r(h)], writes=[Pb.r(), st.r(h)])
        ctxs[u] = Pb

    ctxt = {}

    def stage_t(u):
        m, h = units[u]
        kc0, nloc, egi = pair_info(m)
        Pb = ctxs.pop(u)
        chunks_ = []
        off = 0
        while off < nloc:
            cw = min(128, nloc - off)
            chunks_.append((off, cw, (kc0 + off) // 128))
            off += cw
        chunks_.append((nloc, 128, 20))
        chunks_.append((nloc + 128, 128, 21))
        ptb = PS.next("pt", [4, 5])
        ptap = ptb.ap.bitcast(BF16)
        for i_, (off, cw, vt) in enumerate(chunks_):
            P.pe(I_tr(ptap[0:cw, i_ * 128:(i_ + 1) * 128], Pb.ap[:, off:off + cw], identb.ap), reads=[Pb.r(), identb.r()], writes=[ptb.r()])
        PT = PTr.next()
        ncols_ = len(chunks_) * 128
        if h % 2 == 0:
            P.act(I_act(PT.ap[:, 0:ncols_], ptap[:, 0:ncols_], AF.Copy), reads=[ptb.r()], writes=[PT.r()])
        else:
            P.dve(I_copy(PT.ap[:, 0:ncols_], ptap[:, 0:ncols_]), reads=[ptb.r()], writes=[PT.r()])
        ctxt[u] = (PT, chunks_)

    def stage_pv(u):
        m, h = units[u]
        PT, chunks_ = ctxt.pop(u)
        pvb = PS.b[6 + (m % 2)]
        for i_, (off, cw, vt) in enumerate(chunks_):
            P.pe(I_mm(pvb.ap[:, h * 64:(h + 1) * 64], PT.ap[0:cw, i_ * 128:(i_ + 1) * 128], Vt.ap[0:cw, vt, h * 64:(h + 1) * 64], i_ == 0, i_ == len(chunks_) - 1),
                 reads=[PT.r(), Vt.r(vt)], writes=[pvb.r()])

    def stage_e(m):
        st = pair_st[m]
        pvb = PS.b[6 + (m % 2)]
        md = mdt.next()
        allst = [st.r(h) for h in range(8)]
        P.dve(I_recip(st.ap[:, :, 2], st.ap[:, :, 1]), reads=allst, writes=allst)
        for h in range(8):
            P.dve(I_ts(md.ap[:, h * 64:(h + 1) * 64], pvb.ap[:, h * 64:(h + 1) * 64], st.ap[:, h, 2:3], None, ALU.mult), reads=[pvb.r(), st.r(h)], writes=[md.r()])
        tb_ = PS.next("pt", [4, 5])
        tbap = tb_.ap.bitcast(BF16)
        for j in range(4):
            P.pe(I_tr(tbap[:, j * 128:(j + 1) * 128], md.ap[:, j * 128:(j + 1) * 128], identb.ap), reads=[md.r(), identb.r()], writes=[tb_.r()])
        P.act(I_act(QT.ap[:, :, 128 * m:128 * m + 128], tbap[:, 0:512].rearrange("p (j n) -> p j n", j=4), AF.Copy),
              reads=[tb_.r()], writes=[QT.r((j, m)) for j in range(4)])

    NU = len(units)
    stage_a(0)
    stage_a2(0)
    stage_a(1)
    stage_a2(1)
    stage_t(0)
    for u in range(NU):
        if u + 2 < NU:
            stage_a(u + 2)
        if u + 1 < NU:
            stage_t(u + 1)
        if u + 2 < NU:
            stage_a2(u + 2)
        stage_pv(u)
        if units[u][1] == 7:
            stage_e(units[u][0])
    for r_ in (Sbr, Pbr, PTr, EGr, mdt, stat):
        r_.free()
    AR.free(UG, KT, Vt)

    wo = AR.alloc("wo", [128, 8, D], BF16)
    P.dma("pool", I_dma(wo.ap, dr["w_out"][1].rearrange("(k p) n -> p k n", p=128)), writes=[wo.r()])
    OC1 = OC[:4]

    def rhs1(mk, ci):
        c0, c1 = OC1[ci]
        if mk < 4:
            return mC.ap[:, mk, c0 - OWN0:c1 - OWN0], [mC.r((mk, 256 + 512 * ci))]
        return QT.ap[:, mk - 4, c0 - OWN0:c1 - OWN0], [QT.r((mk - 4, 4 * ci + mm)) for mm in range(4)]

    out_proj(1, wo, rhs1, [(c0, c1, 0) for (c0, c1) in OC1])
    AR.free(wo, mC, QT)
    cut("l1m")
    finals = []

    emit_moe(1, [(c0, c1, 0) for (c0, c1) in OC1])
    frs = AR.alloc("frs", [128, TOK], F32)
    fsq = Rot(AR, "fsq", [128, 8, 512], BF16, 2)
    fsd = Rot(AR, "fsd", [128, 512], F32, 2)
    for ci, (c0, c1) in enumerate(OC1):
        hr = []
        for k in range(8):
            hr += hres(k, c0, c1)
        q_ = fsq.next()
        d_ = fsd.next()
        P.act(I_act(q_.ap, hT.ap[:, :, c0:c1], AF.Square), reads=hr, writes=[q_.r()])
        bank = PS.next("nrm", [4, 5])
        for k in range(8):
            P.pe(I_mm(bank.ap, onesb.ap, q_.ap[:, k, :], k == 0, k == 7), reads=[q_.r(), onesb.r()], writes=[bank.r()])
        P.act(I_act(d_.ap, bank.ap, AF.Sqrt, bias=EPS, scale=1.0 / D), reads=[bank.r()], writes=[d_.r()])
        P.dve(I_recip(frs.ap[:, ci * 512:(ci + 1) * 512], d_.ap), reads=[d_.r()], writes=[frs.r(ci)])
    fsq.free()
    fsd.free()
    ofs = [AR.alloc(f"of{i}", [128, 8, 512], F32) for i in range(2)]
    ftm = Rot(AR, "ftm", [128, 512], F32, 3)
    yt = Rot(AR, "yt", [128, D], F32, 3)

    def f_pass2(ci):
        c0, c1 = OC1[ci]
        of = ofs[ci % 2]
        for k in range(8):
            tb = ftm.next()
            P.dve(I_tt(tb.ap, hT.ap[:, k, c0:c1], frs.ap[:, ci * 512:(ci + 1) * 512], ALU.mult), reads=hres(k, c0, c1) + [frs.r(ci)], writes=[tb.r()])
            P.act(I_act(of.ap[:, k, :], tb.ap, AF.Identity, scale=V("fing", k)), reads=[tb.r(), vec.r()], writes=[of.r(k)])

    def f_out(ci):
        c0, c1 = OC1[ci]
        of = ofs[ci % 2]
        for tt in range(4):
            y_ = yt.next()
            for g in range(2):
                bank = PS.next("xt", [0, 1, 2, 3])
                for kk in range(4):
                    k = g * 4 + kk
                    P.pe(I_tr(bank.ap[:, kk * 128:(kk + 1) * 128], of.ap[:, k, tt * 128:(tt + 1) * 128], ident.ap), reads=[of.r(k), ident.r()], writes=[bank.r()])
                if g == 0:
                    P.dve(I_copy(y_.ap[:, 0:512], bank.ap), reads=[bank.r()], writes=[y_.r()])
                else:
                    P.act(I_act(y_.ap[:, 512:1024], bank.ap, AF.Copy), reads=[bank.r()], writes=[y_.r()])
            r0 = (c0 - OWN0) + tt * 128
            finals.append(P.dma("sp", I_dma(yout[r0:r0 + 128, :], y_.ap), reads=[y_.r()], writes=[dres("y").r(r0)]))

    finals = []
    for ci in range(4):
        f_pass2(ci)
        if ci >= 1:
            f_out(ci - 1)
    f_out(3)
    P.emit({"sp": finals})


def _fm(v):
    v = np.asarray(v, np.float32).reshape(-1, 128)
    return np.ascontiguousarray(v.T)


def _bias_table(rpb, w0, nrows, m, qcore):
    qc = np.arange(64)
    kc = np.arange(64)
    col_start = np.clip(qc - 8, 0, 48)
    colvalid = (kc[None, :] >= col_start[:, None]) & (kc[None, :] < col_start[:, None] + 16)
    colidx = np.clip(kc[None, :] - qc[:, None] + 15, 0, 30)
    T = np.full((8, 2, 64, nrows, 64), -BIG, np.float32)
    for rho in range(2):
        R = 32 * qcore + 2 * m + rho
        r_start = min(max(R - 4, 0), 120)
        for wp in range(nrows):
            keyrow = 32 * qcore + (w0 + wp) - 4
            if keyrow < r_start or keyrow >= r_start + 8:
                continue
            ridx = keyrow - R + 7
            vals = rpb[:, ridx, :][:, colidx]
            T[:, rho, :, wp, :] = np.where(colvalid[None], vals, np.float32(-BIG))
    return np.ascontiguousarray(T.reshape(8, 128, nrows * 64))


def prep_inputs(inp):
    f32 = lambda a: np.ascontiguousarray(np.asarray(a, np.float32))
    x, c, ctx, c_ctx = f32(inp["x"]), f32(inp["c"]), f32(inp["ctx"]), f32(inp["c_ctx"])
    shared = {
        "ident": np.eye(128, dtype=np.float32),
        "ada_w": f32(inp["ada_w"]),
        "w_in0": f32(inp["ab_w_in"][0]),
        "w_in1": f32(inp["cd_w_in"][0]),
        "w_out": f32(inp["w_out"]),
        "gate_w": f32(inp["b_gate_w"]).reshape(16, 128, 128),
        "router_w": f32(inp["router_w"]),
        "rb18": np.ascontiguousarray(np.broadcast_to(np.tile(f32(inp["router_bias"]), 18)[None, :], (128, 288))),
        "selm": np.ascontiguousarray(np.repeat(np.eye(16, dtype=np.float32)[:, :, None], 128, axis=2).reshape(16, 16 * 128)),
        "wg": f32(inp["moe_w_gate"]),
        "wu": f32(inp["moe_w_up"]),
        "wd": f32(inp["moe_w_down"]),
    }
    common = {
        "ada_b0": _fm(inp["ada_b"][0]), "ada_b1": _fm(inp["ada_b"][1]),
        "nmg0": _fm(inp["norm_mix_g"][0]), "nmg1": _fm(inp["norm_mix_g"][1]),
        "nfg0": _fm(inp["norm_ffn_g"][0]), "nfg1": _fm(inp["norm_ffn_g"][1]),
        "fing": _fm(inp["final_g"]),
        "adw_w": np.ascontiguousarray(f32(inp["a_dw_w"][0]).reshape(31, 4, 128).transpose(2, 0, 1).reshape(128, 124)),
        "adw_b": _fm(inp["a_dw_b"][0]), "aln_g": _fm(inp["a_ln_g"][0]), "aln_b": _fm(inp["a_ln_b"][0]),
        "bcw": np.ascontiguousarray(f32(inp["b_conv_w"][0]).reshape(4, 4, 128).transpose(2, 0, 1).reshape(128, 16)),
        "bcb": _fm(inp["b_conv_b"][0]),
        "bgb": np.ascontiguousarray(f32(inp["b_gate_b"][0]).reshape(4, 4, 128).transpose(2, 0, 1).reshape(128, 16)),
        "blam": np.ascontiguousarray(f32(inp["b_lambda"][0]).reshape(2, 4, 128).transpose(2, 0, 1).reshape(128, 8)),
        "ccw": np.ascontiguousarray(f32(inp["c_conv_w"][0]).reshape(3, 4, 128).transpose(2, 0, 1).reshape(128, 12)),
    }
    rpb = f32(inp["d_rpb"][0])
    shared["ug"] = _bias_table(rpb, 8, 9, 4, 1)
    maps = []
    for core in range(NCORES):
        b, q = core // 4, core % 4
        xe = np.zeros((NL0, D), np.float32)
        lo, hi = q * TOK - HAL0, (q + 1) * TOK + HAL0
        slo, shi = max(lo, 0), min(hi, 8192)
        xe[slo - lo:shi - lo] = x[b, slo:shi]
        vecT = np.zeros((128, NV), np.float32)
        for name, n in VEC_LAYOUT:
            o = VOFF[name]
            if name == "cvec":
                vecT[:, o:o + 8] = _fm(c[b])
                vecT[:, o + 8:o + 16] = _fm(c_ctx)
            elif name == "flags":
                fl = np.zeros(8, np.float32)
                fl[0] = 1.0 if q > 0 else 0.0
                fl[1] = 1.0 if q < 3 else 0.0
                fl[2 + q] = 1.0
                vecT[:, o:o + 8] = fl[None, :]
            elif name == "flags2":
                fl = np.zeros(8, np.float32)
                if q > 0:
                    fl[q - 1] = 1.0
                if q < 3:
                    fl[4 + q + 1] = 1.0
                vecT[:, o:o + 8] = fl[None, :]
            else:
                vecT[:, o:o + n] = common[name]
        m = dict(shared)
        eg = np.full((4, 8, 128, 768), -BIG, np.float32)
        for i, (mm, w0, nr) in enumerate([(0, 0, 12), (1, 2, 10), (14, 28, 9), (15, 28, 11)]):
            eg[i, :, :, 0:nr * 64] = _bias_table(rpb, w0, nr, mm, q)
        m["eg"] = eg
        m["xe"] = xe
        m["ctxb"] = np.ascontiguousarray(ctx[b])
        m["vecT"] = vecT
        maps.append(m)
    return maps


def run_stage(stage, inp):
    nc = build(stage)
    maps = prep_inputs(inp)
    res = run_bass_kernel_spmd(nc, maps, core_ids=list(range(NCORES)))
    return res


def kernel(**inputs):
    res = run_stage("full", inputs)
    out = np.zeros((2, 8192, D), np.float32)
    for core in range(NCORES):
        b, q = core // 4, core % 4
        out[b, q * TOK:(q + 1) * TOK] = res.results[core]["y"]
    return out
```
